# Optimizing a Trainium2 kernel written in Bass

```python
import math
import jax, jax.numpy as jnp
from jax import lax
import numpy as np

D_MODEL = 1024
BATCH = 8
SEQ = 4096
DEPTH = 4

N_MIXERS = 2
RET_QK_DIM = 256
RET_HEADS = D_MODEL // RET_QK_DIM
RET_V_DIM = 2 * RET_QK_DIM
RET_QK = RET_HEADS * RET_QK_DIM
RET_V = RET_HEADS * RET_V_DIM
RET_CHUNK = 128
ROPE_BASE = 10000.0
DILATED_PAIRS = ((128, 1), (512, 4), (2048, 16))
ATT_GROUPS = 3
ATT_HEAD_DIM = 64
ATT_HEADS = D_MODEL // ATT_HEAD_DIM
ATT_GROUP_W = ATT_HEADS * ATT_HEAD_DIM
BAND_BLOCK = 128
NUM_BUCKETS = 32
MAX_DISTANCE = 2048
D_FF = ((8 * D_MODEL + 3 * 256 - 1) // (3 * 256)) * 256
DEEPNORM_ALPHA = (2 * DEPTH) ** 0.25
DEEPNORM_BETA = (8 * DEPTH) ** -0.25
LN_EPS = 1e-5
GN_EPS = 1e-5

kernel_name = "hybrid_retention_dilated_attn_deepnorm"


def _layer_norm(x, g, b):
    xf = x.astype(jnp.float32)
    mu = jnp.mean(xf, axis=-1, keepdims=True)
    var = jnp.mean(jnp.square(xf - mu), axis=-1, keepdims=True)
    y = (xf - mu) * lax.rsqrt(var + LN_EPS) * g.astype(jnp.float32) + b.astype(jnp.float32)
    return y.astype(x.dtype)


def _rope(x):
    B, T, H, d = x.shape
    inv = 1.0 / (ROPE_BASE ** jnp.linspace(0.0, 1.0, d // 2, dtype=jnp.float32))
    ang = jnp.arange(T, dtype=jnp.float32)[:, None] * inv[None, :]
    c = jnp.cos(ang)[None, :, None, :]
    s = jnp.sin(ang)[None, :, None, :]
    xp = x.reshape(B, T, H, d // 2, 2)
    a, b = xp[..., 0], xp[..., 1]
    return jnp.stack([a * c - b * s, a * s + b * c], axis=-1).reshape(B, T, H, d)


def retention(x, w_in, w_out):
    B, T, _ = x.shape
    H, dk, dv, C = RET_HEADS, RET_QK_DIM, RET_V_DIM, RET_CHUNK
    N = T // C
    proj = x @ w_in
    q, k, v, gate = jnp.split(proj, [RET_QK, 2 * RET_QK, 2 * RET_QK + RET_V], axis=-1)
    q = _rope(q.reshape(B, T, H, dk).astype(jnp.float32))
    k = _rope(k.reshape(B, T, H, dk).astype(jnp.float32)) * (dk ** -0.5)
    v = v.reshape(B, T, H, dv).astype(jnp.float32)

    def to_chunks(t):
        return t.reshape(B, N, C, H, t.shape[-1]).transpose(1, 0, 3, 2, 4)

    qc, kc, vc = to_chunks(q), to_chunks(k), to_chunks(v)
    log_gamma = jnp.log(1.0 - 2.0 ** (-5.0 - jnp.arange(H, dtype=jnp.float32)))
    idx = jnp.arange(C, dtype=jnp.float32)
    diff = idx[:, None] - idx[None, :]
    intra = jnp.where(diff[None] >= 0,
                      jnp.exp(jnp.maximum(diff, 0.0)[None] * log_gamma[:, None, None]), 0.0)
    xi = jnp.exp((idx[None, :] + 1.0) * log_gamma[:, None])[..., None]
    zeta = jnp.exp((C - 1.0 - idx[None, :]) * log_gamma[:, None])[..., None]
    gamma_c = jnp.exp(C * log_gamma)[:, None, None]

    def body(state, inp):
        qi, ki, vi = inp
        s = jnp.einsum('bhid,bhjd->bhij', qi, ki) * intra
        inner = jnp.einsum('bhij,bhje->bhie', s, vi)
        cross = jnp.einsum('bhid,bhde->bhie', qi * xi, state)
        state = gamma_c * state + jnp.einsum('bhjd,bhje->bhde', ki * zeta, vi)
        return state, inner + cross

    state0 = jnp.zeros((B, H, dk, dv), jnp.float32)
    _, y = lax.scan(body, state0, (qc, kc, vc))
    y = y.transpose(1, 0, 3, 2, 4).reshape(B, T, H, dv)
    mu = jnp.mean(y, axis=-1, keepdims=True)
    var = jnp.mean(jnp.square(y - mu), axis=-1, keepdims=True)
    y = ((y - mu) * lax.rsqrt(var + GN_EPS)).reshape(B, T, RET_V).astype(x.dtype)
    return (jax.nn.silu(gate) * y) @ w_out


def _t5_bucket(dist):
    max_exact = NUM_BUCKETS // 2
    d_f = jnp.maximum(dist, 1).astype(jnp.float32)
    large = max_exact + (jnp.log(d_f / max_exact) / math.log(MAX_DISTANCE / max_exact)
                         * (NUM_BUCKETS - max_exact)).astype(jnp.int32)
    large = jnp.minimum(large, NUM_BUCKETS - 1)
    return jnp.where(dist < max_exact, dist, large)


def _dilated_group(q, k, v, bias_table, window, dil):
    B, H, T, hd = q.shape
    blk = BAND_BLOCK
    steps = window // dil
    L = T // dil
    nb = -(-L // blk)
    Lp = nb * blk

    def to_blocks(t):
        t = t.reshape(B, H, L, dil, hd).transpose(0, 1, 3, 2, 4)
        t = jnp.pad(t, ((0, 0), (0, 0), (0, 0), (0, Lp - L), (0, 0)))
        return t.reshape(B, H, dil, nb, blk, hd)

    def band(t):
        prev = jnp.pad(t, ((0, 0), (0, 0), (0, 0), (1, 0), (0, 0), (0, 0)))[:, :, :, :-1]
        return jnp.concatenate([prev, t], axis=4)

    qb = to_blocks(q)
    kw = band(to_blocks(k))
    vw = band(to_blocks(v))
    qi = jnp.arange(blk)[:, None]
    kj = jnp.arange(2 * blk)[None, :]
    delta = qi + blk - kj
    band_ok = (delta >= 0) & (delta <= steps)
    key_ok = (jnp.arange(nb)[:, None, None] * blk - blk + kj[None]) >= 0
    mask = band_ok[None] & key_ok
    bucket = _t5_bucket(jnp.maximum(delta, 0) * dil)
    bias = bias_table[:, bucket].astype(jnp.float32)

    s = jnp.einsum('bhrnqd,bhrnkd->bhrnqk', qb, kw).astype(jnp.float32) * (hd ** -0.5)
    s = s + bias[None, :, None, None]
    s = jnp.where(mask[None, None, None], s, -jnp.inf)
    m = jnp.max(s, axis=-1, keepdims=True)
    e = jnp.exp(s - m)
    den = jnp.sum(e, axis=-1, keepdims=True)
    p = e / den
    lse = (m + jnp.log(den))[..., 0]
    o = jnp.einsum('bhrnqk,bhrnkd->bhrnqd', p.astype(v.dtype), vw)
    o = o.reshape(B, H, dil, Lp, hd)[:, :, :, :L].transpose(0, 1, 3, 2, 4).reshape(B, H, T, hd)
    lse = lse.reshape(B, H, dil, Lp)[..., :L].transpose(0, 1, 3, 2).reshape(B, H, T)
    return o, lse


def dilated_attention(x, w_in, w_out, rel_bias):
    B, T, _ = x.shape
    proj = (x @ w_in).reshape(B, T, 3, ATT_GROUPS, ATT_HEADS, ATT_HEAD_DIM)
    proj = proj.transpose(2, 3, 0, 4, 1, 5)
    outs, lses = [], []
    for g, (window, dil) in enumerate(DILATED_PAIRS):
        o, lse = _dilated_group(proj[0, g], proj[1, g], proj[2, g],
                                rel_bias[g * ATT_HEADS:(g + 1) * ATT_HEADS], window, dil)
        outs.append(o)
        lses.append(lse)
    w = jax.nn.softmax(jnp.stack(lses, axis=0), axis=0)
    o = jnp.sum(w[..., None] * jnp.stack(outs, axis=0).astype(jnp.float32), axis=0)
    o = o.transpose(0, 2, 1, 3).reshape(B, T, ATT_GROUP_W).astype(x.dtype)
    return o @ w_out


def swiglu_ffn(x, w_up, w_down):
    gate, up = jnp.split(x @ w_up, 2, axis=-1)
    return (jax.nn.silu(gate) * up) @ w_down


def setup_inputs(seed: int = 0) -> dict:
    key = jax.random.key(seed)
    ks = jax.random.split(key, 10)
    n_ret = (DEPTH + 1) // 2
    n_att = DEPTH // 2
    f32 = jnp.float32
    x = jax.random.normal(ks[0], (BATCH, SEQ, D_MODEL), f32)
    ret_w_in = jax.random.normal(ks[1], (n_ret, D_MODEL, 2 * RET_QK + 2 * RET_V), f32) * D_MODEL ** -0.5
    ret_w_out = jax.random.normal(ks[2], (n_ret, RET_V, D_MODEL), f32) * (RET_V ** -0.5 * DEEPNORM_BETA)
    attn_w_in = jax.random.normal(ks[3], (n_att, D_MODEL, 3 * ATT_GROUPS * ATT_GROUP_W), f32) * D_MODEL ** -0.5
    attn_w_out = jax.random.normal(ks[4], (n_att, ATT_GROUP_W, D_MODEL), f32) * (ATT_GROUP_W ** -0.5 * DEEPNORM_BETA)
    rel_bias = jax.random.normal(ks[5], (ATT_GROUPS * ATT_HEADS, NUM_BUCKETS), f32) * 0.2
    ffn_w_up = jax.random.normal(ks[6], (DEPTH, D_MODEL, 2 * D_FF), f32) * D_MODEL ** -0.5
    ffn_w_down = jax.random.normal(ks[7], (DEPTH, D_FF, D_MODEL), f32) * (D_FF ** -0.5 * DEEPNORM_BETA)
    ln_gain = 1.0 + 0.02 * jax.random.normal(ks[8], (DEPTH, 2, D_MODEL), f32)
    ln_bias = 0.02 * jax.random.normal(ks[9], (DEPTH, 2, D_MODEL), f32)
    return {"x": x, "ret_w_in": ret_w_in, "ret_w_out": ret_w_out,
            "attn_w_in": attn_w_in, "attn_w_out": attn_w_out, "rel_bias": rel_bias,
            "ffn_w_up": ffn_w_up, "ffn_w_down": ffn_w_down,
            "ln_gain": ln_gain, "ln_bias": ln_bias}


def reference(x, ret_w_in, ret_w_out, attn_w_in, attn_w_out, rel_bias,
              ffn_w_up, ffn_w_down, ln_gain, ln_bias):
    for i in range(DEPTH):
        j = i // N_MIXERS
        if i % N_MIXERS == 0:
            mix = retention(x, ret_w_in[j], ret_w_out[j])
        else:
            mix = dilated_attention(x, attn_w_in[j], attn_w_out[j], rel_bias)
        x = _layer_norm(DEEPNORM_ALPHA * x + mix, ln_gain[i, 0], ln_bias[i, 0])
        x = _layer_norm(DEEPNORM_ALPHA * x + swiglu_ffn(x, ffn_w_up[i], ffn_w_down[i]),
                        ln_gain[i, 1], ln_bias[i, 1])
    return x
```

```python
import math
from contextlib import ExitStack

import numpy as np
import concourse.bass as bass
import concourse.mybir as mybir
from concourse.bass_utils import run_bass_kernel_spmd

F32 = mybir.dt.float32
BF16 = mybir.dt.bfloat16
AF = mybir.ActivationFunctionType
ALU = mybir.AluOpType

D = 1024
T = 4096
NT = T // 128
DEPTH = 4
DFF = 2816
NFC = DFF // 128
ALPHA = float((2 * DEPTH) ** 0.25)
LN_EPS = 1e-5
GN_EPS = 1e-5
NCORES = 8
SAME_ENGINE_SYNC = True
NEG = -1.0e5
DILS = (1, 4, 16)
DBG = 0


class Tk:
    __slots__ = ("w", "r", "gen")

    def __init__(self):
        self.w = []
        self.r = []
        self.gen = []


class SemC:
    def __init__(self, sem):
        self.sem = sem
        self.count = 0


class Op:
    __slots__ = ("eng", "fn", "deps", "need_inc", "semc", "val", "isdma", "idx")


def _add_latest(lst, op):
    if not op.isdma:
        for i, q in enumerate(lst):
            if (not q.isdma) and q.eng == op.eng:
                lst[i] = op
                return
    lst.append(op)


class Prog:
    CE = ("pe", "act", "dve", "pool")

    def __init__(self, nc, es):
        self.nc = nc
        self.es = es
        self.csem = {e: SemC(es.enter_context(nc.semaphore("c_" + e))) for e in self.CE}
        self.lists = {e: [] for e in ("pe", "act", "dve", "pool", "sp")}
        self.n = 0
        self.tiles = []
        self.nsem = 0
        self.sempool = []
        self.nsw = 0
        self.swpool = []

    def dsem_sw(self):
        if self.nsw == len(self.swpool):
            self.swpool.append(SemC(self.es.enter_context(self.nc.semaphore("sw%d" % self.nsw))))
        c = self.swpool[self.nsw]
        self.nsw += 1
        return c

    def tk(self):
        t = Tk()
        self.tiles.append(t)
        return t

    def tks(self, n):
        return [self.tk() for _ in range(n)]

    def dsem(self):
        if self.nsem == len(self.sempool):
            self.sempool.append(SemC(self.es.enter_context(self.nc.semaphore("d%d" % self.nsem))))
        c = self.sempool[self.nsem]
        self.nsem += 1
        return c

    def rec(self, eng, fn, reads=(), writes=(), pw=(), semc=None):
        op = Op()
        op.eng = eng
        op.fn = fn
        op.need_inc = False
        op.isdma = semc is not None
        op.idx = self.n
        op.semc = None
        op.val = 0
        self.n += 1
        deps = {}

        def add(p):
            if p.isdma:
                deps[id(p)] = p
            else:
                q = deps.get(p.eng)
                if q is None or q.idx < p.idx:
                    deps[p.eng] = p

        for t in reads:
            for p in t.w:
                add(p)
        for t in writes:
            for p in t.r:
                add(p)
            for p in t.w:
                add(p)
        for t in pw:
            if t.r:
                t.gen = t.r + t.w
            for p in t.gen:
                add(p)
        for t in reads:
            _add_latest(t.r, op)
        for t in writes:
            t.w = [op]
            t.r = []
            t.gen = [op]
        for t in pw:
            if t.r:
                t.w = [op]
                t.r = []
            else:
                _add_latest(t.w, op)
        dl = []
        for p in deps.values():
            if (not p.isdma) and (not op.isdma) and p.eng == eng and (eng == "pe" or not SAME_ENGINE_SYNC):
                continue
            p.need_inc = True
            dl.append(p)
        op.deps = dl
        if semc is not None:
            semc.count += 16
            op.semc = semc
            op.val = semc.count
        self.lists[eng].append(op)
        return op

    def flush(self):
        for e in self.CE:
            c = self.csem[e]
            for op in self.lists[e]:
                if (not op.isdma) and op.need_inc:
                    c.count += 1
                    op.semc = c
                    op.val = c.count
        lists = self.lists
        with self.nc.Block() as block:
            for e, deco in (("pe", block.tensor), ("act", block.scalar), ("dve", block.vector),
                            ("pool", block.gpsimd), ("sp", block.sync)):
                ops = lists[e]

                def body(eng, ops=ops):
                    waited = {}
                    final = {}
                    for op in ops:
                        need = {}
                        for p in op.deps:
                            k = id(p.semc)
                            if waited.get(k, 0) >= p.val:
                                continue
                            if k not in need or need[k][1] < p.val:
                                need[k] = (p.semc, p.val)
                        for k, (c, v) in need.items():
                            eng.wait_ge(c.sem, v)
                            waited[k] = v
                        ins = op.fn(eng)
                        if op.isdma:
                            ins.then_inc(op.semc.sem, 16)
                            final[id(op.semc)] = (op.semc, op.val)
                        elif op.need_inc:
                            ins.then_inc(op.semc.sem, 1)
                    for k, (c, v) in final.items():
                        if waited.get(k, 0) < v:
                            eng.wait_ge(c.sem, v)

                deco(body)
        self.lists = {e: [] for e in ("pe", "act", "dve", "pool", "sp")}
        self.nsem = 0
        self.nsw = 0
        for t in self.tiles:
            t.w = []
            t.r = []
            t.gen = []
        self.tiles = []

    def mm(self, out, lhsT, rhs, start, stop, reads, writes=(), pw=()):
        return self.rec("pe", lambda e: e.matmul(out, lhsT, rhs, start=start, stop=stop), reads, writes, pw)

    def tr(self, out, in_, ident, reads, writes=(), pw=()):
        return self.rec("pe", lambda e: e.transpose(out, in_, ident), reads, writes, pw)

    def act(self, out, in_, func, reads, writes=(), pw=(), bias=None, scale=None):
        kw = {}
        if bias is not None:
            kw["bias"] = bias
        if scale is not None:
            kw["scale"] = scale
        return self.rec("act", lambda e: e.activation(out=out, in_=in_, func=func, **kw), reads, writes, pw)

    def tt(self, eng, out, in0, in1, op, reads, writes=(), pw=()):
        return self.rec(eng, lambda e: e.tensor_tensor(out=out, in0=in0, in1=in1, op=op), reads, writes, pw)

    def ts(self, eng, out, in0, s1, s2, op0, op1, reads, writes=(), pw=()):
        return self.rec(eng, lambda e: e.tensor_scalar(out=out, in0=in0, scalar1=s1, scalar2=s2, op0=op0, op1=op1),
                        reads, writes, pw)

    def stt(self, out, in0, scalar, in1, op0, op1, reads, writes=(), pw=()):
        return self.rec("dve", lambda e: e.scalar_tensor_tensor(out=out, in0=in0, scalar=scalar, in1=in1,
                                                                op0=op0, op1=op1), reads, writes, pw)

    def cp(self, eng, out, in_, reads, writes=(), pw=()):
        if eng == "act":
            return self.rec("act", lambda e: e.copy(out=out, in_=in_), reads, writes, pw)
        return self.rec(eng, lambda e: e.tensor_copy(out=out, in_=in_), reads, writes, pw)

    def dma(self, q, out, in_, semc, reads, writes=(), pw=(), **kw):
        return self.rec(q, lambda e: e.dma_start(out=out, in_=in_, **kw), reads, writes, pw, semc=semc)


_UNIQ = [0]


def sbuf(nc, name, shape, dtype):
    _UNIQ[0] += 1
    return nc.sbuf_tensor("%s_%d" % (name, _UNIQ[0]), shape, dtype)


class Ring:
    def __init__(self, P, es, name, n, shape, dtype, dma=True, sw=False):
        self.n = n
        self.t = es.enter_context(sbuf(P.nc, name, [128, n] + list(shape), dtype))
        self.P = P
        self.sems = [(P.dsem_sw() if sw else P.dsem()) for _ in range(n)] if dma else None
        self.i = 0
        self.renew()

    def renew(self):
        self.tks = self.P.tks(self.n)

    def next(self):
        i = self.i % self.n
        self.i += 1
        return i


class Psum:
    cnt = 0

    def __init__(self, P, es, nf=6, nt=2):
        nc = P.nc
        self.P = P
        self.nf = nf
        self.nt = nt
        Psum.cnt += 1
        self.ps = es.enter_context(nc.psum_tensor("psf%d" % Psum.cnt, [128, nf * 512], F32))
        self.pt = es.enter_context(nc.psum_tensor("pst%d" % Psum.cnt, [128, nt * 1024], BF16))
        self.i = 0
        self.j = 0
        self.tks = P.tks(nf)
        self.ttks = P.tks(nt)

    def fixed(self, b):
        return self.ps[:, b * 512:(b + 1) * 512], self.tks[b]

    def bank(self):
        b = self.i % self.nf
        self.i += 1
        return self.fixed(b)

    def bank2(self):
        if self.i % 2:
            self.i += 1
        b = self.i % 6
        self.i += 2
        return self.ps[:, b * 512:(b + 2) * 512], [self.tks[b], self.tks[b + 1]]

    def tbank(self):
        b = self.j % self.nt
        self.j += 1
        return self.pt[:, b * 1024:b * 1024 + 512], self.ttks[b]

    def tbank_full(self):
        b = self.j % self.nt
        self.j += 1
        return self.pt[:, b * 1024:(b + 1) * 1024], self.ttks[b]


class Ctx:
    pass


def layer_norm_rows(P, C, src, src_tk, gain, bias, gb_tk, out, out_tk):
    st = C.ln_stats
    i = st.next()
    stt_, stk = st.t[:, i], st.tks[i]
    P.rec("dve", lambda e: e.bn_stats(out=stt_[:, 0:6], in_=src[:, 0:512]), [src_tk], [stk])
    P.rec("dve", lambda e: e.bn_stats(out=stt_[:, 6:12], in_=src[:, 512:1024]), [src_tk], (), [stk])
    P.rec("dve", lambda e: e.bn_aggr(out=stt_[:, 12:14], in_=stt_[:, 0:12]), [stk], (), [stk])
    P.act(stt_[:, 14:15], stt_[:, 13:14], AF.Sqrt, [stk], (), [stk], bias=C.eps_ln[:, 0:1], scale=1.0)
    P.rec("dve", lambda e: e.reciprocal(out=stt_[:, 14:15], in_=stt_[:, 14:15]), [stk], (), [stk])
    P.ts("dve", stt_[:, 15:16], stt_[:, 12:13], stt_[:, 14:15], -1.0, ALU.mult, ALU.mult, [stk], (), [stk])
    P.act(out, src, AF.Identity, [src_tk, stk], [out_tk], bias=stt_[:, 15:16], scale=stt_[:, 14:15])
    P.tt("pool", out, out, gain, ALU.mult, [out_tk, gb_tk], [out_tk])
    P.tt("pool", out, out, bias, ALU.add, [out_tk, gb_tk], [out_tk])


def load_gain_bias(P, C, lng, lnb, layer, which, semc):
    for k, src in enumerate((lng, lnb)):
        off = (layer * 2 + which) * D
        ap = bass.AP(src.tensor, off, [[0, 128], [1, D]])
        P.dma("sp", C.gb[:, k, :], ap, semc, [], (), [C.gb_tk])


def convert_weights(P, C, src, dst, rows, cols, tklist, chunk_rows=512):
    r = 0
    while r < rows:
        n = min(chunk_rows, rows - r)
        tk = P.tk()
        s = C.conv_sems[C.conv_i % len(C.conv_sems)]
        C.conv_i += 1
        P.dma("pool", dst[r:r + n, :], src[r:r + n, :], s, [], [tk])
        tklist.append((r, r + n, tk))
        r += n


def conv_tks(tklist, r0, r1):
    return [tk for (a, b, tk) in tklist if a < r1 and b > r0]


def ffn_pass(P, C, es0, layer, x1_d, xo_d, wup_bf, wdn_bf, wup_tk, wdn_tk, lng, lnb):
    nc = P.nc
    with ExitStack() as es:
        C.ps = Psum(P, es)
        C.ln_stats.renew()
        wd = es.enter_context(sbuf(nc, "wd", [128, NFC, D], BF16))
        wd_tk = P.tk()
        wd_sem = P.dsem()
        xg = Ring(P, es, "f_xg", 2, [4, D], F32)
        xb = es.enter_context(sbuf(nc, "f_xb", [128, 4, D], BF16))
        xb_tk = P.tk()
        xT = es.enter_context(sbuf(nc, "f_xT", [128, 8, 512], BF16))
        xT_tk = P.tk()
        hT = es.enter_context(sbuf(nc, "f_hT", [128, NFC, 512], BF16))
        hT_tk = P.tk()
        wu = Ring(P, es, "f_wu", 4, [8, 256], BF16)
        sg = Ring(P, es, "f_sg", 3, [512], F32, dma=False)
        rr = Ring(P, es, "f_r", 2, [D], F32, dma=False)
        ot = Ring(P, es, "f_o", 3, [D], F32, sw=True)
        C.gb = es.enter_context(sbuf(nc, "f_gb", [128, 2, D], F32))
        C.gb_tk = P.tk()
        gsem = P.dsem()
        load_gain_bias(P, C, lng, lnb, layer, 1, gsem)
        for half in range(2):
            P.dma("sp", wd[:, half * 11:(half + 1) * 11, :],
                  wdn_bf[:, half * 11 * D:(half + 1) * 11 * D].rearrange("p (f d) -> p f d", d=D),
                  wd_sem, conv_tks(wdn_tk, 0, 128), (), [wd_tk])
        x1v = x1_d.rearrange("(n p) d -> p n d", p=128)
        xov = xo_d.rearrange("(n p) d -> p n d", p=128)

        def load_x(g):
            i = xg.next()
            P.dma("sp", xg.t[:, i], x1v[:, g * 4:(g + 1) * 4, :], xg.sems[i], [C.x1_tk[g]], [xg.tks[i]])
            return i

        nxt = load_x(0)
        wq = []

        def load_wu(j):
            i = wu.next()
            P.dma("sp", wu.t[:, i], wup_bf[j * 128:(j + 1) * 128, :].rearrange("p (k c) -> p k c", c=256),
                  wu.sems[i], conv_tks(wup_tk, j * 128, (j + 1) * 128), [wu.tks[i]])
            return i

        for g in range(8):
            xi = nxt
            xgt, xg_tk = xg.t[:, xi], xg.tks[xi]
            for a in range(4):
                P.cp("pool" if a % 2 else "dve", xb[:, a, :], xgt[:, a, :], [xg_tk], (), [xb_tk])
            for kc in range(8):
                pt, ptk = C.ps.tbank()
                for a in range(4):
                    P.tr(pt[:, a * 128:(a + 1) * 128], xb[:, a, kc * 128:(kc + 1) * 128], C.ident,
                         [xb_tk], [ptk] if a == 0 else (), () if a == 0 else [ptk])
                P.cp("act" if kc % 2 else "dve", xT[:, kc, :], pt, [ptk], (), [xT_tk])
            if g + 1 < 8:
                nxt = load_x(g + 1)
            if not wq:
                wq = [load_wu(0), load_wu(1), load_wu(2)]
            for j in range(NFC):
                if j + 3 < NFC:
                    wq.append(load_wu(j + 3))
                elif g + 1 < 8:
                    wq.append(load_wu(j + 3 - NFC))
                wi = wq.pop(0)
                wt, wtk = wu.t[:, wi], wu.tks[wi]
                pg, pgk = C.ps.bank()
                pu, puk = C.ps.bank()
                for kc in range(8):
                    P.mm(pg, wt[:, kc, 0:128], xT[:, kc, :], kc == 0, kc == 7, [wtk, xT_tk],
                         [pgk] if kc == 0 else (), () if kc == 0 else [pgk])
                for kc in range(8):
                    P.mm(pu, wt[:, kc, 128:256], xT[:, kc, :], kc == 0, kc == 7, [wtk, xT_tk],
                         [puk] if kc == 0 else (), () if kc == 0 else [puk])
                si = sg.next()
                P.act(sg.t[:, si], pg, AF.Silu, [pgk], [sg.tks[si]])
                P.tt("dve", hT[:, j, :], sg.t[:, si], pu, ALU.mult, [sg.tks[si], puk], (), [hT_tk])
            for a in range(4):
                po, pok = C.ps.bank2()
                for half in range(2):
                    for fc in range(NFC):
                        P.mm(po[:, half * 512:(half + 1) * 512], hT[:, fc, a * 128:(a + 1) * 128],
                             wd[:, fc, half * 512:(half + 1) * 512], fc == 0, fc == NFC - 1, [hT_tk, wd_tk],
                             [pok[half]] if fc == 0 else (), () if fc == 0 else [pok[half]])
                ri = rr.next()
                P.stt(rr.t[:, ri], xgt[:, a, :], ALPHA, po, ALU.mult, ALU.add, [xg_tk] + pok, [rr.tks[ri]])
                oi = ot.next()
                layer_norm_rows(P, C, rr.t[:, ri], rr.tks[ri], C.gb[:, 0, :], C.gb[:, 1, :], C.gb_tk,
                                ot.t[:, oi], ot.tks[oi])
                P.dma("pool", xov[:, g * 4 + a, :], ot.t[:, oi], ot.sems[oi], [ot.tks[oi]], (), [C.xo_tk[g]])
        P.flush()


def retention_pass(P, C, es0, layer, x_d, x1_d, win_bf, wout_bf, win_tk, wout_tk, lng, lnb, cd):
    nc = P.nc
    with ExitStack() as es:
        C.ps = Psum(P, es)
        C.ln_stats.renew()
        wo = es.enter_context(sbuf(nc, "r_wo", [128, 16, D], BF16))
        wo_tk = P.tk()
        wo_sem = P.dsem()
        st32 = es.enter_context(sbuf(nc, "r_st32", [128, 8, 512], F32))
        stbf = es.enter_context(sbuf(nc, "r_stbf", [128, 8, 512], BF16))
        st_tk = P.tks(8)
        stb_tk = P.tks(8)
        msk = es.enter_context(sbuf(nc, "r_msk", [128, 4, 128], F32))
        zs = es.enter_context(sbuf(nc, "r_zs", [128, 4], F32))
        cst_tk = P.tk()
        cst_sem = P.dsem()
        C.gb = es.enter_context(sbuf(nc, "r_gb", [128, 2, D], F32))
        C.gb_tk = P.tk()
        gsem = P.dsem()
        xg = Ring(P, es, "r_xg", 2, [D], F32)
        xb = es.enter_context(sbuf(nc, "r_xb", [128, 4, D], BF16))
        xb_tk = P.tk()
        xT = es.enter_context(sbuf(nc, "r_xT", [128, 8, 512], BF16))
        xT_tk = P.tk()
        gT = es.enter_context(sbuf(nc, "r_gT", [128, 16, 512], BF16))
        gT_tk = P.tk()
        wr = Ring(P, es, "r_w", 3, [8, 512], BF16)
        tq = Ring(P, es, "r_tq", 2, [2, 512], F32)
        tkk = Ring(P, es, "r_tk", 1, [2, 512], F32)
        qk = Ring(P, es, "r_qk", 2, [4, 512], BF16, dma=False)
        vs = Ring(P, es, "r_v", 2, [4, 512], BF16, dma=False)
        sgt = Ring(P, es, "r_sg", 2, [4, 512], BF16, dma=False)
        tmp = Ring(P, es, "r_tmp", 4, [512], F32, dma=False)
        sT = Ring(P, es, "r_sT", 2, [4, 128], BF16, dma=False)
        kz = Ring(P, es, "r_kz", 2, [4, 256], BF16, dma=False)
        yn = Ring(P, es, "r_yn", 2, [512], F32, dma=False)
        gg = Ring(P, es, "r_g", 5, [512], BF16, dma=False)
        gst = Ring(P, es, "r_gst", 6, [16], F32, dma=False)
        rr = Ring(P, es, "r_r", 2, [D], F32)
        ot = Ring(P, es, "r_o", 2, [D], F32, sw=True)

        load_gain_bias(P, C, lng, lnb, layer, 0, gsem)
        P.dma("sp", msk[:], cd["r_msk"], cst_sem, [], (), [cst_tk])
        P.dma("sp", zs[:], cd["r_zs"], cst_sem, [], (), [cst_tk])
        for q in range(4):
            P.dma("sp", wo[:, q * 4:(q + 1) * 4, :],
                  wout_bf[:, q * 4 * D:(q + 1) * 4 * D].rearrange("p (f d) -> p f d", d=D),
                  wo_sem, conv_tks(wout_tk, 0, 128), (), [wo_tk])
        for s in range(8):
            P.rec("dve", lambda e, s=s: e.memset(st32[:, s, :], 0.0), [], [st_tk[s]])
            P.rec("pool", lambda e, s=s: e.memset(stbf[:, s, :], 0.0), [], [stb_tk[s]])
        xv = x_d.rearrange("(n p) d -> p n d", p=128)
        x1v = x1_d.rearrange("(n p) d -> p n d", p=128)
        gam = [1.0 - 2.0 ** (-5.0 - h) for h in range(4)]
        gC = [float(np.float64(g) ** 128) for g in gam]

        wblocks = [(g, h, k) for g in range(8) for h in range(4) for k in range(3)]
        wslots = {}
        wstate = {"n": 0}

        def ensure_w(upto):
            while wstate["n"] <= min(upto, len(wblocks) - 1):
                g_, h_, k_ = wblocks[wstate["n"]]
                i = wr.next()
                r0 = (h_ * 3 + k_) * 128
                P.dma("sp", wr.t[:, i], win_bf[r0:r0 + 128, :].rearrange("p (k c) -> p k c", c=512),
                      wr.sems[i], [], [wr.tks[i]])
                wslots[wstate["n"]] = i
                wstate["n"] += 1

        def x_to_xT(g):
            for a in range(4):
                i = xg.next()
                P.dma("sp", xg.t[:, i], xv[:, g * 4 + a, :], xg.sems[i], [C.x_tk[g]], [xg.tks[i]])
                P.cp("pool" if a % 2 else "dve", xb[:, a, :], xg.t[:, i], [xg.tks[i]], (), [xb_tk])
            for kc in range(8):
                pt, ptk = C.ps.tbank()
                for a in range(4):
                    P.tr(pt[:, a * 128:(a + 1) * 128], xb[:, a, kc * 128:(kc + 1) * 128], C.ident,
                         [xb_tk], [ptk] if a == 0 else (), () if a == 0 else [ptk])
                P.cp("act" if kc % 2 else "dve", xT[:, kc, :], pt, [ptk], (), [xT_tk])

        x_to_xT(0)
        for g in range(8):
            ki = tkk.next()
            P.dma("sp", tkk.t[:, ki], cd["r_ropek"][:, :, g * 512:(g + 1) * 512], tkk.sems[ki], [], [tkk.tks[ki]])
            for pair in ((0, 1), (2, 3)):
                hctx = {}
                for h in pair:
                    qi = tq.next()
                    P.dma("sp", tq.t[:, qi], cd["r_ropeq"][h][:, :, g * 512:(g + 1) * 512], tq.sems[qi], [],
                          [tq.tks[qi]])
                    b0 = (g * 4 + h) * 3
                    ensure_w(b0)
                    wqk_i = wslots[b0]
                    qki = qk.next()
                    qkt, qk_tk = qk.t[:, qki], qk.tks[qki]
                    wt, wtk = wr.t[:, wqk_i], wr.tks[wqk_i]
                    for which in range(2):
                        pa, pak = C.ps.bank()
                        pb, pbk = C.ps.bank()
                        for part, (pp, ppk) in enumerate(((pa, pak), (pb, pbk))):
                            c0 = (which * 2 + part) * 128
                            for kc in range(8):
                                P.mm(pp, wt[:, kc, c0:c0 + 128], xT[:, kc, :], kc == 0, kc == 7, [wtk, xT_tk],
                                     [ppk] if kc == 0 else (), () if kc == 0 else [ppk])
                        if which == 0:
                            ct, st_, tbk = tq.t[:, qi, 0, :], tq.t[:, qi, 1, :], tq.tks[qi]
                        else:
                            ct, st_, tbk = tkk.t[:, ki, 0, :], tkk.t[:, ki, 1, :], tkk.tks[ki]
                        t1, t2, t3, t4 = [tmp.next() for _ in range(4)]
                        P.tt("dve", tmp.t[:, t1], pa, ct, ALU.mult, [pak, tbk], [tmp.tks[t1]])
                        P.tt("dve", tmp.t[:, t2], pb, st_, ALU.mult, [pbk, tbk], [tmp.tks[t2]])
                        P.tt("dve", tmp.t[:, t3], pa, st_, ALU.mult, [pak, tbk], [tmp.tks[t3]])
                        P.tt("dve", tmp.t[:, t4], pb, ct, ALU.mult, [pbk, tbk], [tmp.tks[t4]])
                        P.tt("pool", qkt[:, which * 2 + 0, :], tmp.t[:, t1], tmp.t[:, t2], ALU.subtract,
                             [tmp.tks[t1], tmp.tks[t2]], (), [qk_tk])
                        P.tt("pool", qkt[:, which * 2 + 1, :], tmp.t[:, t3], tmp.t[:, t4], ALU.add,
                             [tmp.tks[t3], tmp.tks[t4]], (), [qk_tk])
                    vi = vs.next()
                    sgi = sgt.next()
                    ensure_w(b0 + 1)
                    wv, wvk = wr.t[:, wslots[b0 + 1]], wr.tks[wslots[b0 + 1]]
                    for c in range(4):
                        pv, pvk = C.ps.bank()
                        for kc in range(8):
                            P.mm(pv, xT[:, kc, c * 128:(c + 1) * 128], wv[:, kc, :], kc == 0, kc == 7, [wvk, xT_tk],
                                 [pvk] if kc == 0 else (), () if kc == 0 else [pvk])
                        P.cp("act", vs.t[:, vi, c, :], pv, [pvk], (), [vs.tks[vi]])
                    ensure_w(b0 + 2)
                    wg, wgk = wr.t[:, wslots[b0 + 2]], wr.tks[wslots[b0 + 2]]
                    for c in range(4):
                        pg, pgk = C.ps.bank()
                        for kc in range(8):
                            P.mm(pg, xT[:, kc, c * 128:(c + 1) * 128], wg[:, kc, :], kc == 0, kc == 7, [wgk, xT_tk],
                                 [pgk] if kc == 0 else (), () if kc == 0 else [pgk])
                        P.act(sgt.t[:, sgi, c, :], pg, AF.Silu, [pgk], (), [sgt.tks[sgi]])
                    ensure_w(b0 + 5)
                    hctx[h] = (qkt, qk_tk, vi, sgi)
                if pair == (2, 3) and g + 1 < 8:
                    x_to_xT(g + 1)
                for h in pair:
                    qkt, qk_tk, vi, sgi = hctx[h]
                    ps_, psk = C.ps.bank()
                    first = True
                    for c in range(4):
                        cs = slice(c * 128, (c + 1) * 128)
                        for dc in range(2):
                            P.mm(ps_[:, cs], qkt[:, 2 + dc, cs], qkt[:, dc, cs], dc == 0, dc == 1, [qk_tk],
                                 [psk] if first else (), () if first else [psk])
                            first = False
                    si = sT.next()
                    mb = bass.AP(msk.tensor if hasattr(msk, "tensor") else msk, h * 128, [[512, 128], [0, 4], [1, 128]])
                    P.tt("dve", sT.t[:, si], ps_.rearrange("p (c i) -> p c i", i=128), mb, ALU.mult,
                         [psk, cst_tk], [sT.tks[si]])
                    pt, ptk = C.ps.tbank_full()
                    first = True
                    for c in range(4):
                        cs = slice(c * 128, (c + 1) * 128)
                        for dc in range(2):
                            o0 = c * 256 + dc * 128
                            P.tr(pt[:, o0:o0 + 128], qkt[:, 2 + dc, cs], C.ident, [qk_tk],
                                 [ptk] if first else (), () if first else [ptk])
                            first = False
                    kzi = kz.next()
                    P.act(kz.t[:, kzi].rearrange("p c d -> p (c d)"), pt, AF.Identity, [ptk, cst_tk], [kz.tks[kzi]],
                          scale=zs[:, h:h + 1])
                    hctx[h] = (qkt, qk_tk, vi, sgi, si, kzi)
                deferred = []

                def emit_e(item):
                    h_, c_, ggi_ = item
                    cs_ = slice(c_ * 128, (c_ + 1) * 128)
                    pt_, ptk_ = C.ps.tbank()
                    for ec in range(4):
                        P.tr(pt_[:, ec * 128:(ec + 1) * 128], gg.t[:, ggi_, ec * 128:(ec + 1) * 128], C.ident,
                             [gg.tks[ggi_]], [ptk_] if ec == 0 else (), () if ec == 0 else [ptk_])
                    P.cp("act", gT[:, h_ * 4:(h_ + 1) * 4, cs_], pt_.rearrange("p (e t) -> p e t", t=128), [ptk_], (),
                         [gT_tk])

                for c in range(4):
                    cs = slice(c * 128, (c + 1) * 128)
                    for h in pair:
                        qkt, qk_tk, vi, sgi, si, kzi = hctx[h]
                        pus = []
                        for dc in range(2):
                            pu, puk = C.ps.bank()
                            P.mm(pu, kz.t[:, kzi, c, dc * 128:(dc + 1) * 128], vs.t[:, vi, c, :], True, True,
                                 [kz.tks[kzi], vs.tks[vi]], [puk])
                            pus.append((pu, puk))
                        py, pyk = C.ps.bank()
                        P.mm(py, sT.t[:, si, c, :], vs.t[:, vi, c, :], True, False, [sT.tks[si], vs.tks[vi]], [pyk])
                        for dc in range(2):
                            P.mm(py, qkt[:, dc, cs], stbf[:, h * 2 + dc, :], False, dc == 1,
                                 [qk_tk, stb_tk[h * 2 + dc]], (), [pyk])
                        for dc in range(2):
                            s_ = h * 2 + dc
                            pu, puk = pus[dc]
                            P.stt(st32[:, s_, :], st32[:, s_, :], gC[h], pu, ALU.mult, ALU.add, [puk, st_tk[s_]],
                                  [st_tk[s_]])
                            P.cp("act", stbf[:, s_, :], st32[:, s_, :], [st_tk[s_]], [stb_tk[s_]])
                        gi = gst.next()
                        gs_, gsk = gst.t[:, gi], gst.tks[gi]
                        P.rec("dve", lambda e, gs_=gs_, py=py: e.bn_stats(out=gs_[:, 0:6], in_=py), [pyk], [gsk])
                        P.rec("dve", lambda e, gs_=gs_: e.bn_aggr(out=gs_[:, 6:8], in_=gs_[:, 0:6]), [gsk], (), [gsk])
                        P.act(gs_[:, 8:9], gs_[:, 7:8], AF.Sqrt, [gsk], (), [gsk], bias=C.eps_gn[:, 0:1], scale=1.0)
                        P.rec("dve", lambda e, gs_=gs_: e.reciprocal(out=gs_[:, 8:9], in_=gs_[:, 8:9]), [gsk], (),
                              [gsk])
                        P.ts("dve", gs_[:, 9:10], gs_[:, 6:7], gs_[:, 8:9], -1.0, ALU.mult, ALU.mult, [gsk], (),
                             [gsk])
                        yi = yn.next()
                        P.act(yn.t[:, yi], py, AF.Identity, [pyk, gsk], [yn.tks[yi]], bias=gs_[:, 9:10],
                              scale=gs_[:, 8:9])
                        ggi = gg.next()
                        P.tt("pool", gg.t[:, ggi], yn.t[:, yi], sgt.t[:, sgi, c, :], ALU.mult,
                             [yn.tks[yi], sgt.tks[sgi]], [gg.tks[ggi]])
                        deferred.append((h, c, ggi))
                        if len(deferred) > 3:
                            emit_e(deferred.pop(0))
                while deferred:
                    emit_e(deferred.pop(0))
            for a in range(4):
                po, pok = C.ps.bank2()
                for half in range(2):
                    for ec in range(16):
                        P.mm(po[:, half * 512:(half + 1) * 512], gT[:, ec, a * 128:(a + 1) * 128],
                             wo[:, ec, half * 512:(half + 1) * 512], ec == 0, ec == 15, [gT_tk, wo_tk],
                             [pok[half]] if ec == 0 else (), () if ec == 0 else [pok[half]])
                ri = rr.next()
                P.dma("sp", rr.t[:, ri], xv[:, g * 4 + a, :], rr.sems[ri], [C.x_tk[g]], [rr.tks[ri]])
                P.stt(rr.t[:, ri], rr.t[:, ri], ALPHA, po, ALU.mult, ALU.add, [rr.tks[ri]] + pok, [rr.tks[ri]])
                oi = ot.next()
                layer_norm_rows(P, C, rr.t[:, ri], rr.tks[ri], C.gb[:, 0, :], C.gb[:, 1, :], C.gb_tk,
                                ot.t[:, oi], ot.tks[oi])
                P.dma("pool", x1v[:, g * 4 + a, :], ot.t[:, oi], ot.sems[oi], [ot.tks[oi]], (), [C.x1_tk[g]])
        P.flush()


BL = 129 * 256


def bias_prep_pass(P, C, rel_bias_d, w8d, b8, cd):
    nc = P.nc
    with ExitStack() as es:
        C.ps = Psum(P, es)
        rbT = es.enter_context(sbuf(nc, "b_rbT", [32, 48], F32))
        oh = es.enter_context(sbuf(nc, "b_oh", [32, 3 * 512], F32))
        ng = es.enter_context(sbuf(nc, "b_ng", [16, 512], F32))
        w8 = es.enter_context(sbuf(nc, "b_w8", [16, 3, 512], F32))
        tk_in = P.tk()
        sem = P.dsem()
        osem = P.dsem()
        bsem = P.dsem()
        P.dma("sp", rbT[:, :], bass.AP(rel_bias_d.tensor, 0, [[1, 32], [32, 48]]), sem, [], (), [tk_in],
              allow_slow_non_contiguous=True)
        P.dma("sp", oh[:, :], cd["a_oh"], sem, [], (), [tk_in])
        P.dma("sp", ng[:, :], cd["a_negm"], sem, [], (), [tk_in])
        for g in range(3):
            pb, pbk = C.ps.bank()
            P.mm(pb[0:16, :], rbT[:, g * 16:(g + 1) * 16], oh[:, g * 512:(g + 1) * 512], True, True, [tk_in], [pbk])
            wtk = P.tk()
            P.tt("dve", w8[:, g, :], pb[0:16, :], ng[:, :], ALU.add, [pbk, tk_in], [wtk])
            dtk = P.tk()
            P.dma("sp", w8d[g * 16:(g + 1) * 16, :], w8[:, g, :], osem, [wtk], [dtk])
            for h in range(16):
                for c in range(2):
                    gh = g * 16 + h
                    P.dma("sp", b8[gh * 2 + c].rearrange("(a b) -> a b", b=256),
                          bass.AP(w8d.tensor, gh * 512 + c * 256, [[0, 129], [1, 256]]), bsem, [dtk], [P.tk()])
        P.flush()


def blocks_of(g):
    d = DILS[g]
    nb = 32 // d
    return d, nb


def attention_pass(P, C, layer, x_d, oT_d, win_bf, b8):
    nc = P.nc
    with ExitStack() as es:
        C.ps = Psum(P, es, nf=7, nt=1)
        xT = es.enter_context(sbuf(nc, "a_xT", [128, 8, T], BF16))
        xT_tk = P.tk()
        oacc = es.enter_context(sbuf(nc, "a_oacc", [128, T], F32))
        dacc = es.enter_context(sbuf(nc, "a_dacc", [128, T], F32))
        oacc_tk = P.tk()
        dacc_tk = P.tk()
        ones = es.enter_context(sbuf(nc, "a_ones", [128, 2, 128], BF16))
        ones_tk = P.tk()
        qTr = Ring(P, es, "a_qT", 1, [2, T], BF16, dma=False)
        kTr = Ring(P, es, "a_kT", 2, [T], BF16, dma=False)
        vbr = Ring(P, es, "a_vb", 1, [32, 2, 128], BF16, dma=False)
        wr = Ring(P, es, "a_w", 2, [8, 384], BF16)
        w8r = Ring(P, es, "a_w8", 2, [2, 2, 128], F32)
        ssr = Ring(P, es, "a_ss", 3, [512], F32, dma=False)
        ptr = Ring(P, es, "a_pT", 4, [512], BF16, dma=False)
        C.att_i = 0
        C.att_s = 0
        onr = Ring(P, es, "a_on", 2, [1024], BF16)
        rcr = Ring(P, es, "a_rc", 2, [1024], F32, dma=False)
        xg = Ring(P, es, "a_xg", 2, [D], F32)
        xbr = Ring(P, es, "a_xb", 2, [D], BF16, dma=False)
        xv = x_d.rearrange("(n p) d -> p n d", p=128)
        P.rec("pool", lambda e: e.memset(ones[:, :, :], 0.0), [], [ones_tk])
        P.rec("pool", lambda e: e.memset(ones[:, 0, 0:64], 1.0), [ones_tk], [ones_tk])
        P.rec("pool", lambda e: e.memset(ones[:, 1, 64:128], 1.0), [ones_tk], [ones_tk])
        P.rec("pool", lambda e: e.memset(qTr.t[:, 0], 0.0), [], [qTr.tks[0]])
        P.rec("pool", lambda e: e.memset(vbr.t[:, 0], 0.0), [], [vbr.tks[0]])
        for t in range(NT):
            i = xg.next()
            P.dma("sp", xg.t[:, i], xv[:, t, :], xg.sems[i], [C.x_tk[t // 4]], [xg.tks[i]])
            bi = xbr.next()
            P.cp("pool" if t % 2 else "dve", xbr.t[:, bi], xg.t[:, i], [xg.tks[i]], [xbr.tks[bi]])
            for half in range(2):
                pt, ptk = C.ps.tbank()
                for q in range(4):
                    kc = half * 4 + q
                    P.tr(pt[:, q * 128:(q + 1) * 128], xbr.t[:, bi, kc * 128:(kc + 1) * 128], C.ident,
                         [xbr.tks[bi]], [ptk] if q == 0 else (), () if q == 0 else [ptk])
                P.cp("act" if half else "dve", xT[:, half * 4:(half + 1) * 4, t * 128:(t + 1) * 128],
                     pt.rearrange("p (k t) -> p k t", t=128), [ptk], (), [xT_tk])

        if DBG == 2:
            P.flush()
            return
        nblk = 24
        wslot = {}
        wn = {"n": 0}

        def ensure_w(upto):
            while wn["n"] <= min(upto, nblk - 1):
                b = wn["n"]
                i = wr.next()
                P.dma("sp", wr.t[:, i], win_bf[b * 128:(b + 1) * 128, :].rearrange("p (k c) -> p k c", c=384),
                      wr.sems[i], [], [wr.tks[i]])
                wslot[b] = i
                wn["n"] += 1

        ensure_w(1)
        for hp in range(8):
            for g in range(3):
                d, nb = blocks_of(g)
                b = hp * 3 + g
                ensure_w(b)
                wt, wtk = wr.t[:, wslot[b]], wr.tks[wslot[b]]
                w8i = w8r.next()
                for hd in range(2):
                    gh = g * 16 + hp * 2 + hd
                    P.dma("sp", w8r.t[:, w8i, :, hd, :], bass.AP(b8.tensor, gh * 2 * BL + 127, [[255, 128], [BL, 2], [1, 128]]),
                          w8r.sems[w8i], [], [w8r.tks[w8i]] if hd == 0 else (), () if hd == 0 else [w8r.tks[w8i]])
                w8t, w8k = w8r.t[:, w8i], w8r.tks[w8i]
                qi, ki, vi = qTr.next(), kTr.next(), vbr.next()
                qT, qk_ = qTr.t[:, qi], qTr.tks[qi]
                kT, kk_ = kTr.t[:, ki], kTr.tks[ki]
                vb, vk_ = vbr.t[:, vi], vbr.tks[vi]
                for tg in range(8):
                    pp, ppk = C.ps.bank()
                    for kc in range(8):
                        P.mm(pp, wt[:, kc, 0:128], xT[:, kc, tg * 512:(tg + 1) * 512],
                             kc == 0, kc == 7, [wtk, xT_tk], [ppk] if kc == 0 else (), () if kc == 0 else [ppk])
                    ts2 = slice(tg * 512, (tg + 1) * 512)
                    ev = "act" if tg % 2 else "dve"
                    P.cp(ev, qT[0:64, 0, ts2], pp[0:64, :], [ppk], (), [qk_])
                    P.cp(ev, qT[64:128, 1, ts2], pp[64:128, :], [ppk], (), [qk_])
                for tg in range(8):
                    pp, ppk = C.ps.bank()
                    for kc in range(8):
                        P.mm(pp, wt[:, kc, 128:256], xT[:, kc, tg * 512:(tg + 1) * 512],
                             kc == 0, kc == 7, [wtk, xT_tk], [ppk] if kc == 0 else (), () if kc == 0 else [ppk])
                    P.cp("dve" if tg % 2 else "act", kT[:, tg * 512:(tg + 1) * 512], pp, [ppk], (), [kk_])

                def tok(r, n0, cnt=1):
                    s0 = n0 * 128 * d + r
                    return slice(s0, min(s0 + cnt * 128 * d, T), d)

                blist = [(r, n) for r in range(d) for n in range(nb)]
                for b4 in range(8 if DBG not in (31, 32) else 0):
                    pv, pvk = C.ps.bank()
                    for q in range(4):
                        r, n = blist[b4 * 4 + q]
                        for kc in range(8):
                            first = (q == 0 and kc == 0)
                            P.mm(pv[:, q * 128:(q + 1) * 128], xT[:, kc, tok(r, n)], wt[:, kc, 256:384],
                                 kc == 0, kc == 7, [wtk, xT_tk], [pvk] if first else (), () if first else [pvk])
                    pv3 = pv.rearrange("p (q c) -> p q c", c=128)
                    ev = "act" if b4 % 2 else "dve"
                    P.cp(ev, vb[:, b4 * 4:(b4 + 1) * 4, 0, 0:64], pv3[:, :, 0:64], [pvk], (), [vk_])
                    P.cp(ev, vb[:, b4 * 4:(b4 + 1) * 4, 1, 64:128], pv3[:, :, 64:128], [pvk], (), [vk_])
                ensure_w(b + 1)
                if DBG in (3, 31, 32, 33, 34, 35):
                    continue
                if DBG in (4, 6, 7, 8) and g > 0:
                    continue
                if DBG == 5 and g > 1:
                    continue
                nbat = min(4, nb)
                items = []
                for r in range(d):
                    for n0 in range(0, nb, nbat):
                        pob = 0 if (C.att_i % 2 == 0) else 2
                        C.att_i += 1
                        for nn in range(nbat):
                            items.append((r, n0, nn, pob))
                LA = 2
                pend = {}

                def stage_a(it):
                    r, n0, nn, pob = it
                    n = n0 + nn
                    nch = 2 if n > 0 else 1
                    wv_ = nch * 256
                    ps_, psk = C.ps.fixed(4 + C.att_s % 3)
                    C.att_s += 1
                    for c in range(nch):
                        P.mm(ps_[:, c * 256:(c + 1) * 256], kT[:, tok(r, n - c)], qT[:, :, tok(r, n)],
                             True, True, [qk_, kk_], [psk] if c == 0 else (), () if c == 0 else [psk])
                    si, pi = ssr.next(), ptr.next()
                    P.stt(ssr.t[:, si, 0:wv_], ps_[:, 0:wv_], 600.0,
                          w8t.rearrange("p c h i -> p (c h i)")[:, 0:wv_], ALU.min, ALU.add,
                          [psk, w8k], [ssr.tks[si]])
                    P.act(ptr.t[:, pi, 0:wv_], ssr.t[:, si, 0:wv_], AF.Exp, [ssr.tks[si]], [ptr.tks[pi]],
                          scale=0.125)
                    pend[it] = pi

                def stage_b(it):
                    r, n0, nn, pob = it
                    n = n0 + nn
                    nch = 2 if n > 0 else 1
                    bc = r * nb + n
                    pi = pend.pop(it)
                    po, pok = C.ps.fixed(pob)
                    pd, pdk = C.ps.fixed(pob + 1)
                    k = 0
                    for c in range(nch):
                        for hd in range(2):
                            pTs = ptr.t[:, pi, (c * 2 + hd) * 128:(c * 2 + hd + 1) * 128]
                            fo = (nn == 0 and k == 0)
                            P.mm(po[:, nn * 128:(nn + 1) * 128], vb[:, bc - c, hd, :], pTs, k == 0,
                                 k == 2 * nch - 1, [vk_, ptr.tks[pi]], [pok] if fo else (), () if fo else [pok])
                            k += 1
                    k = 0
                    for c in range(nch):
                        for hd in range(2):
                            pTs = ptr.t[:, pi, (c * 2 + hd) * 128:(c * 2 + hd + 1) * 128]
                            fo = (nn == 0 and k == 0)
                            P.mm(pd[:, nn * 128:(nn + 1) * 128], ones[:, hd, :], pTs, k == 0,
                                 k == 2 * nch - 1, [ones_tk, ptr.tks[pi]], [pdk] if fo else (), () if fo else [pdk])
                            k += 1
                    if nn == nbat - 1:
                        ts_ = tok(r, n0, nbat)
                        w = nbat * 128
                        if g == 0:
                            P.cp("act", oacc[:, ts_], po[:, 0:w], [pok], (), [oacc_tk])
                            P.cp("act", dacc[:, ts_], pd[:, 0:w], [pdk], (), [dacc_tk])
                        else:
                            P.tt("dve", oacc[:, ts_], oacc[:, ts_], po[:, 0:w], ALU.add, [pok, oacc_tk], (), [oacc_tk])
                            P.tt("dve", dacc[:, ts_], dacc[:, ts_], pd[:, 0:w], ALU.add, [pdk, dacc_tk], (), [dacc_tk])

                for i in range(len(items) + LA):
                    if i < len(items):
                        stage_a(items[i])
                    if i >= LA:
                        stage_b(items[i - LA])
            if DBG in (3, 6, 7, 8, 31, 32, 33, 34, 35):
                continue
            for q in range(4):
                cs = slice(q * 1024, (q + 1) * 1024)
                ri, oi = rcr.next(), onr.next()
                P.rec("dve", lambda e, ri=ri, cs=cs: e.reciprocal(out=rcr.t[:, ri], in_=dacc[:, cs]), [dacc_tk],
                      [rcr.tks[ri]])
                P.tt("pool", onr.t[:, oi], oacc[:, cs], rcr.t[:, ri], ALU.mult, [oacc_tk, rcr.tks[ri]], [onr.tks[oi]])
                P.dma("sp", oT_d[hp, :, cs], onr.t[:, oi], onr.sems[oi], [onr.tks[oi]], (), [C.oT_tk])
        P.flush()


def outproj_pass(P, C, layer, x_d, x1_d, oT_d, wout_bf, lng, lnb):
    nc = P.nc
    with ExitStack() as es:
        C.ps = Psum(P, es)
        C.ln_stats.renew()
        wo = es.enter_context(sbuf(nc, "o_wo", [128, 8, D], BF16))
        wo_tk = P.tk()
        wo_sem = P.dsem()
        C.gb = es.enter_context(sbuf(nc, "o_gb", [128, 2, D], F32))
        C.gb_tk = P.tk()
        gsem = P.dsem()
        oT = Ring(P, es, "o_oT", 2, [8, 512], BF16)
        rr = Ring(P, es, "o_r", 3, [D], F32)
        ot = Ring(P, es, "o_o", 3, [D], F32, sw=True)
        load_gain_bias(P, C, lng, lnb, layer, 0, gsem)
        for q in range(2):
            P.dma("sp", wo[:, q * 4:(q + 1) * 4, :],
                  wout_bf[:, q * 4 * D:(q + 1) * 4 * D].rearrange("p (f d) -> p f d", d=D), wo_sem, [], (), [wo_tk])
        xv = x_d.rearrange("(n p) d -> p n d", p=128)
        x1v = x1_d.rearrange("(n p) d -> p n d", p=128)
        for g in range(8):
            oi_ = oT.next()
            P.dma("sp", oT.t[:, oi_], oT_d[:, :, g * 512:(g + 1) * 512].rearrange("h p t -> p h t"), oT.sems[oi_],
                  [C.oT_tk], [oT.tks[oi_]])
            for a in range(4):
                po, pok = C.ps.bank2()
                for half in range(2):
                    for hp in range(8):
                        P.mm(po[:, half * 512:(half + 1) * 512], oT.t[:, oi_, hp, a * 128:(a + 1) * 128],
                             wo[:, hp, half * 512:(half + 1) * 512], hp == 0, hp == 7, [oT.tks[oi_], wo_tk],
                             [pok[half]] if hp == 0 else (), () if hp == 0 else [pok[half]])
                ri = rr.next()
                P.dma("sp", rr.t[:, ri], xv[:, g * 4 + a, :], rr.sems[ri], [C.x_tk[g]], [rr.tks[ri]])
                P.stt(rr.t[:, ri], rr.t[:, ri], ALPHA, po, ALU.mult, ALU.add, [rr.tks[ri]] + pok, [rr.tks[ri]])
                oi = ot.next()
                layer_norm_rows(P, C, rr.t[:, ri], rr.tks[ri], C.gb[:, 0, :], C.gb[:, 1, :], C.gb_tk,
                                ot.t[:, oi], ot.tks[oi])
                P.dma("pool", x1v[:, g * 4 + a, :], ot.t[:, oi], ot.sems[oi], [ot.tks[oi]], (), [C.x1_tk[g]])
        P.flush()


def _t5_bucket_np(dist):
    max_exact = 16
    d_f = np.maximum(dist, 1).astype(np.float32)
    large = max_exact + (np.log(d_f / np.float32(max_exact)) / np.float32(math.log(2048 / max_exact))
                         * np.float32(32 - max_exact)).astype(np.int32)
    large = np.minimum(large, 31)
    return np.where(dist < max_exact, dist, large)


def make_consts():
    c = {}
    c["ident"] = np.eye(128, dtype=np.float32)
    inv = (1.0 / (np.float32(10000.0) ** np.linspace(0.0, 1.0, 128, dtype=np.float32))).astype(np.float32)
    ang = (np.arange(T, dtype=np.float32)[:, None] * inv[None, :]).astype(np.float32)
    cos = np.cos(ang.astype(np.float64)).T
    sin = np.sin(ang.astype(np.float64)).T
    c["r_ropek"] = np.stack([cos, sin], axis=1).astype(np.float32)
    gam = np.array([1.0 - 2.0 ** (-5.0 - h) for h in range(4)], dtype=np.float64)
    il = (np.arange(T) % 128).astype(np.float64)
    rq = np.zeros((4, 128, 2, T), np.float32)
    for h in range(4):
        xi = gam[h] ** (il + 1.0)
        rq[h, :, 0, :] = cos * xi[None, :]
        rq[h, :, 1, :] = sin * xi[None, :]
    c["r_ropeq"] = rq
    j = np.arange(128)[:, None].astype(np.float64)
    i = np.arange(128)[None, :].astype(np.float64)
    msk = np.zeros((128, 4, 128), np.float32)
    zs = np.zeros((128, 4), np.float32)
    for h in range(4):
        msk[:, h, :] = np.where(i >= j, gam[h] ** (-(j + 1.0)) / 16.0, 0.0)
        zs[:, h] = gam[h] ** (127.0 - np.arange(128)) / 16.0
    c["r_msk"] = msk
    c["r_zs"] = zs
    oh = np.zeros((32, 3, 2, 256), np.float32)
    negm = np.zeros((16, 2, 256), np.float32)
    u = np.arange(255)
    for cc in range(2):
        delta = (u - 127) if cc == 0 else (u + 1)
        valid = (delta >= 0) if cc == 0 else (delta <= 128)
        negm[:, cc, :255] = np.where(valid, 0.0, NEG)[None, :]
        negm[:, cc, 255] = NEG
        for g, dil in enumerate(DILS):
            bkt = _t5_bucket_np(np.maximum(delta, 0) * dil)
            for uu in range(255):
                if valid[uu]:
                    oh[bkt[uu], g, cc, uu] = 8.0
    c["a_oh"] = oh.reshape(32, 3 * 512)
    c["a_negm"] = negm.reshape(16, 512)
    return c


def layout_weights(inp):
    w = {}
    for l in range(DEPTH):
        Wu = inp["ffn_w_up"][l]
        gt = Wu[:, :DFF].reshape(8, 128, NFC, 128)
        up = Wu[:, DFF:].reshape(8, 128, NFC, 128)
        blk = np.concatenate([gt, up], axis=3)
        w["wup%d" % l] = np.ascontiguousarray(blk.transpose(2, 1, 0, 3)).reshape(NFC * 128, 2048)
        Wd = inp["ffn_w_down"][l].reshape(NFC, 128, D)
        w["wdn%d" % l] = np.ascontiguousarray(Wd.transpose(1, 0, 2)).reshape(128 * 11, 2048)
    for j in range(2):
        Wi = inp["ret_w_in"][j]
        blocks = []
        for h in range(4):
            q = Wi[:, h * 256:(h + 1) * 256]
            k = Wi[:, 1024 + h * 256:1024 + (h + 1) * 256]
            qk = np.concatenate([q[:, 0::2], q[:, 1::2], k[:, 0::2], k[:, 1::2]], axis=1)
            v = Wi[:, 2048 + h * 512:2048 + (h + 1) * 512]
            gte = Wi[:, 4096 + h * 512:4096 + (h + 1) * 512]
            for m in (qk, v, gte):
                blocks.append(m.reshape(8, 128, 512).transpose(1, 0, 2).reshape(128, 4096))
        w["rwin%d" % j] = np.ascontiguousarray(np.concatenate(blocks, axis=0)).reshape(12 * 128 * 2, 2048)
        Wo = inp["ret_w_out"][j].reshape(16, 128, D)
        w["rwout%d" % j] = np.ascontiguousarray(Wo.transpose(1, 0, 2)).reshape(128 * 8, 2048)
        Wa = inp["attn_w_in"][j]
        blocks = []
        for hp in range(8):
            for g in range(3):
                cols = []
                for s_ in range(3):
                    c0 = ((s_ * 3 + g) * 16 + 2 * hp) * 64
                    cols.append(Wa[:, c0:c0 + 128])
                m = np.concatenate(cols, axis=1)
                blocks.append(m.reshape(8, 128, 384).transpose(1, 0, 2).reshape(128, 3072))
        w["awin%d" % j] = np.ascontiguousarray(np.concatenate(blocks, axis=0)).reshape(24 * 128 * 2, 1536)
        Wao = inp["attn_w_out"][j].reshape(8, 128, D)
        w["awout%d" % j] = np.ascontiguousarray(Wao.transpose(1, 0, 2)).reshape(128 * 4, 2048)
    return w


LAYERS_DEFAULT = (0, 1, 2, 3)


def build_program(layers=LAYERS_DEFAULT, debug_out=None):
    nc = bass.Bass("TRN2", target_bir_lowering=False)
    x_in = nc.dram_tensor("x", [T, D], F32, kind="ExternalInput").ap()
    y_out = nc.dram_tensor("y", [T, D], F32, kind="ExternalOutput").ap()
    lng = nc.dram_tensor("ln_gain", [DEPTH * 2 * D], F32, kind="ExternalInput").ap()
    lnb = nc.dram_tensor("ln_bias", [DEPTH * 2 * D], F32, kind="ExternalInput").ap()
    consts = make_consts()
    cd = {}
    for k, v in consts.items():
        cd[k] = nc.dram_tensor("c_" + k, list(v.shape), F32, kind="ExternalInput").ap()
    wsrc = {}
    wshapes = {}
    for l in layers:
        wshapes["wup%d" % l] = (NFC * 128, 2048)
        wshapes["wdn%d" % l] = (128 * 11, 2048)
        if l % 2 == 0:
            wshapes["rwin%d" % (l // 2)] = (12 * 128 * 2, 2048)
            wshapes["rwout%d" % (l // 2)] = (128 * 8, 2048)
        else:
            wshapes["awin%d" % (l // 2)] = (24 * 128 * 2, 1536)
            wshapes["awout%d" % (l // 2)] = (128 * 4, 2048)
    for k, shp in wshapes.items():
        wsrc[k] = nc.dram_tensor(k, list(shp), F32, kind="ExternalInput").ap()
    wbf = {k: nc.dram_tensor(k + "_bf", list(shp), BF16).ap() for k, shp in wshapes.items()}
    xa = nc.dram_tensor("xa", [T, D], F32).ap()
    xb_ = nc.dram_tensor("xb", [T, D], F32).ap()
    x1 = nc.dram_tensor("x1", [T, D], F32).ap()
    oT_d = nc.dram_tensor("oT_d", [8, 128, T], BF16).ap()
    w8d = nc.dram_tensor("w8d", [48, 512], F32).ap()
    b8 = nc.dram_tensor("b8", [96, BL], F32).ap()
    has_attn = any(l % 2 == 1 for l in layers)
    rel_bias_d = nc.dram_tensor("rel_bias", [48, 32], F32, kind="ExternalInput").ap() if has_attn else None

    with ExitStack() as es:
        P = Prog(nc, es)
        C = Ctx()
        C.ln_stats = Ring(P, es, "lnst", 4, [16], F32, dma=False)
        C.ident_t = es.enter_context(sbuf(nc, "ident", [128, 128], BF16))
        C.ident = C.ident_t[:, :]
        C.eps_ln = es.enter_context(sbuf(nc, "eps_ln", [128, 1], F32))
        C.eps_gn = es.enter_context(sbuf(nc, "eps_gn", [128, 1], F32))
        C.conv_sems = [SemC(es.enter_context(nc.semaphore("cv%d" % i))) for i in range(4)]
        C.conv_i = 0
        isem = SemC(es.enter_context(nc.semaphore("cv_i")))
        P.dma("pool", C.ident, cd["ident"], isem, [], [P.tk()])
        P.rec("dve", lambda e: e.memset(C.eps_ln[:], LN_EPS), [], [P.tk()])
        P.rec("dve", lambda e: e.memset(C.eps_gn[:], GN_EPS), [], [P.tk()])
        wtk = {k: [] for k in wshapes}
        pass_keys = []
        for l in layers:
            if l % 2 == 0:
                pass_keys.append(["rwin%d" % (l // 2), "rwout%d" % (l // 2)])
            else:
                pass_keys.append(["awin%d" % (l // 2)])
                pass_keys.append(["awout%d" % (l // 2)])
            pass_keys.append(["wup%d" % l, "wdn%d" % l])
        C.pass_i = 0

        def conv_for(pi):
            if pi < len(pass_keys):
                for k in pass_keys[pi]:
                    convert_weights(P, C, wsrc[k], wbf[k], wshapes[k][0], wshapes[k][1], wtk[k])

        def next_pass():
            C.pass_i += 1
            conv_for(C.pass_i)

        conv_for(0)
        P.flush()
        if has_attn:
            bias_prep_pass(P, C, rel_bias_d, w8d, b8, cd)
        if DBG == 1:
            return nc, consts, list(wshapes.keys())

        cur = x_in
        for li, l in enumerate(layers):
            last = li == len(layers) - 1
            nxt = y_out if last else (xa if cur is not xa else xb_)
            C.x_tk = P.tks(8)
            C.x1_tk = P.tks(8)
            C.xo_tk = P.tks(8)
            if l % 2 == 0:
                j = l // 2
                next_pass()
                retention_pass(P, C, es, l, cur, x1, wbf["rwin%d" % j].rearrange("(r two) c -> r (two c)", two=2),
                               wbf["rwout%d" % j].rearrange("(p e) c -> p (e c)", e=8),
                               wtk["rwin%d" % j], wtk["rwout%d" % j], lng, lnb, cd)
            else:
                j = l // 2
                C.oT_tk = P.tk()
                next_pass()
                attention_pass(P, C, l, cur, oT_d, wbf["awin%d" % j].rearrange("(r two) c -> r (two c)", two=2), b8)
                if DBG:
                    return nc, consts, list(wshapes.keys())
                C.x_tk = P.tks(8)
                C.x1_tk = P.tks(8)
                C.oT_tk = P.tk()
                next_pass()
                outproj_pass(P, C, l, cur, x1, oT_d, wbf["awout%d" % j].rearrange("(p e) c -> p (e c)", e=4),
                             lng, lnb)
            C.x_tk = P.tks(8)
            C.x1_tk = P.tks(8)
            C.xo_tk = P.tks(8)
            next_pass()
            ffn_pass(P, C, es, l, x1, nxt, wbf["wup%d" % l],
                     wbf["wdn%d" % l].rearrange("(p e) c -> p (e c)", e=11),
                     wtk["wup%d" % l], wtk["wdn%d" % l], lng, lnb)
            cur = nxt
    return nc, consts, list(wshapes.keys())


_CACHE = {}


def kernel(x, ret_w_in, ret_w_out, attn_w_in, attn_w_out, rel_bias, ffn_w_up, ffn_w_down, ln_gain, ln_bias):
    inp = dict(x=np.asarray(x), ret_w_in=np.asarray(ret_w_in), ret_w_out=np.asarray(ret_w_out),
               attn_w_in=np.asarray(attn_w_in), attn_w_out=np.asarray(attn_w_out), rel_bias=np.asarray(rel_bias),
               ffn_w_up=np.asarray(ffn_w_up), ffn_w_down=np.asarray(ffn_w_down))
    nc, consts, wkeys = build_program()
    w = layout_weights(inp)
    shared = {"ln_gain": np.ascontiguousarray(np.asarray(ln_gain, np.float32).reshape(-1)),
              "ln_bias": np.ascontiguousarray(np.asarray(ln_bias, np.float32).reshape(-1)),
              "rel_bias": np.ascontiguousarray(inp["rel_bias"])}
    for k, v in consts.items():
        shared["c_" + k] = v
    for k in wkeys:
        shared[k] = w[k]
    in_maps = []
    for b in range(NCORES):
        m = dict(shared)
        m["x"] = np.ascontiguousarray(inp["x"][b])
        in_maps.append(m)
    res = run_bass_kernel_spmd(nc, in_maps, core_ids=list(range(NCORES)))
    return np.stack([np.asarray(r["y"]) for r in res.results], axis=0).astype(np.float32)
```

```python
import math
from contextlib import ExitStack

import numpy as np
import concourse.bass as bass
import concourse.mybir as mybir
from concourse.bass_utils import run_bass_kernel_spmd

F32 = mybir.dt.float32
BF16 = mybir.dt.bfloat16
AF = mybir.ActivationFunctionType
ALU = mybir.AluOpType

D = 1024
T = 4096
NT = T // 128
DEPTH = 4
DFF = 2816
NFC = DFF // 128
ALPHA = float((2 * DEPTH) ** 0.25)
LN_EPS = 1e-5
GN_EPS = 1e-5
NCORES = 8
SAME_ENGINE_SYNC = True
NEG = -1.0e5
DILS = (1, 4, 16)
DBG = 0


class Tk:
    __slots__ = ("w", "r", "gen")

    def __init__(self):
        self.w = []
        self.r = []
        self.gen = []


class SemC:
    def __init__(self, sem):
        self.sem = sem
        self.count = 0


class Op:
    __slots__ = ("eng", "fn", "deps", "need_inc", "semc", "val", "isdma", "idx")


def _add_latest(lst, op):
    if not op.isdma:
        for i, q in enumerate(lst):
            if (not q.isdma) and q.eng == op.eng:
                lst[i] = op
                return
    lst.append(op)


class Prog:
    CE = ("pe", "act", "dve", "pool")

    def __init__(self, nc, es):
        self.nc = nc
        self.es = es
        self.csem = {e: SemC(es.enter_context(nc.semaphore("c_" + e))) for e in self.CE}
        self.lists = {e: [] for e in ("pe", "act", "dve", "pool", "sp")}
        self.n = 0
        self.tiles = []
        self.nsem = 0
        self.sempool = []
        self.nsw = 0
        self.swpool = []

    def dsem_sw(self):
        if self.nsw == len(self.swpool):
            self.swpool.append(SemC(self.es.enter_context(self.nc.semaphore("sw%d" % self.nsw))))
        c = self.swpool[self.nsw]
        self.nsw += 1
        return c

    def tk(self):
        t = Tk()
        self.tiles.append(t)
        return t

    def tks(self, n):
        return [self.tk() for _ in range(n)]

    def dsem(self):
        if self.nsem == len(self.sempool):
            self.sempool.append(SemC(self.es.enter_context(self.nc.semaphore("d%d" % self.nsem))))
        c = self.sempool[self.nsem]
        self.nsem += 1
        return c

    def rec(self, eng, fn, reads=(), writes=(), pw=(), semc=None):
        op = Op()
        op.eng = eng
        op.fn = fn
        op.need_inc = False
        op.isdma = semc is not None
        op.idx = self.n
        op.semc = None
        op.val = 0
        self.n += 1
        deps = {}

        def add(p):
            if p.isdma:
                deps[id(p)] = p
            else:
                q = deps.get(p.eng)
                if q is None or q.idx < p.idx:
                    deps[p.eng] = p

        for t in reads:
            for p in t.w:
                add(p)
        for t in writes:
            for p in t.r:
                add(p)
            for p in t.w:
                add(p)
        for t in pw:
            if t.r:
                t.gen = t.r + t.w
            for p in t.gen:
                add(p)
        for t in reads:
            _add_latest(t.r, op)
        for t in writes:
            t.w = [op]
            t.r = []
            t.gen = [op]
        for t in pw:
            if t.r:
                t.w = [op]
                t.r = []
            else:
                _add_latest(t.w, op)
        dl = []
        for p in deps.values():
            if (not p.isdma) and (not op.isdma) and p.eng == eng and (eng == "pe" or not SAME_ENGINE_SYNC):
                continue
            p.need_inc = True
            dl.append(p)
        op.deps = dl
        if semc is not None:
            semc.count += 16
            op.semc = semc
            op.val = semc.count
        self.lists[eng].append(op)
        return op

    def flush(self):
        for e in self.CE:
            c = self.csem[e]
            for op in self.lists[e]:
                if (not op.isdma) and op.need_inc:
                    c.count += 1
                    op.semc = c
                    op.val = c.count
        lists = self.lists
        with self.nc.Block() as block:
            for e, deco in (("pe", block.tensor), ("act", block.scalar), ("dve", block.vector),
                            ("pool", block.gpsimd), ("sp", block.sync)):
                ops = lists[e]

                def body(eng, ops=ops):
                    waited = {}
                    final = {}
                    for op in ops:
                        need = {}
                        for p in op.deps:
                            k = id(p.semc)
                            if waited.get(k, 0) >= p.val:
                                continue
                            if k not in need or need[k][1] < p.val:
                                need[k] = (p.semc, p.val)
                        for k, (c, v) in need.items():
                            eng.wait_ge(c.sem, v)
                            waited[k] = v
                        ins = op.fn(eng)
                        if op.isdma:
                            ins.then_inc(op.semc.sem, 16)
                            final[id(op.semc)] = (op.semc, op.val)
                        elif op.need_inc:
                            ins.then_inc(op.semc.sem, 1)
                    for k, (c, v) in final.items():
                        if waited.get(k, 0) < v:
                            eng.wait_ge(c.sem, v)

                deco(body)
        self.lists = {e: [] for e in ("pe", "act", "dve", "pool", "sp")}
        self.nsem = 0
        self.nsw = 0
        for t in self.tiles:
            t.w = []
            t.r = []
            t.gen = []
        self.tiles = []

    def mm(self, out, lhsT, rhs, start, stop, reads, writes=(), pw=()):
        return self.rec("pe", lambda e: e.matmul(out, lhsT, rhs, start=start, stop=stop), reads, writes, pw)

    def tr(self, out, in_, ident, reads, writes=(), pw=()):
        return self.rec("pe", lambda e: e.transpose(out, in_, ident), reads, writes, pw)

    def act(self, out, in_, func, reads, writes=(), pw=(), bias=None, scale=None):
        kw = {}
        if bias is not None:
            kw["bias"] = bias
        if scale is not None:
            kw["scale"] = scale
        return self.rec("act", lambda e: e.activation(out=out, in_=in_, func=func, **kw), reads, writes, pw)

    def tt(self, eng, out, in0, in1, op, reads, writes=(), pw=()):
        return self.rec(eng, lambda e: e.tensor_tensor(out=out, in0=in0, in1=in1, op=op), reads, writes, pw)

    def ts(self, eng, out, in0, s1, s2, op0, op1, reads, writes=(), pw=()):
        return self.rec(eng, lambda e: e.tensor_scalar(out=out, in0=in0, scalar1=s1, scalar2=s2, op0=op0, op1=op1),
                        reads, writes, pw)

    def stt(self, out, in0, scalar, in1, op0, op1, reads, writes=(), pw=()):
        return self.rec("dve", lambda e: e.scalar_tensor_tensor(out=out, in0=in0, scalar=scalar, in1=in1,
                                                                op0=op0, op1=op1), reads, writes, pw)

    def cp(self, eng, out, in_, reads, writes=(), pw=()):
        if eng == "act":
            return self.rec("act", lambda e: e.copy(out=out, in_=in_), reads, writes, pw)
        return self.rec(eng, lambda e: e.tensor_copy(out=out, in_=in_), reads, writes, pw)

    def dma(self, q, out, in_, semc, reads, writes=(), pw=(), **kw):
        return self.rec(q, lambda e: e.dma_start(out=out, in_=in_, **kw), reads, writes, pw, semc=semc)


_UNIQ = [0]


def sbuf(nc, name, shape, dtype):
    _UNIQ[0] += 1
    return nc.sbuf_tensor("%s_%d" % (name, _UNIQ[0]), shape, dtype)


class Ring:
    def __init__(self, P, es, name, n, shape, dtype, dma=True, sw=False):
        self.n = n
        self.t = es.enter_context(sbuf(P.nc, name, [128, n] + list(shape), dtype))
        self.P = P
        self.sems = [(P.dsem_sw() if sw else P.dsem()) for _ in range(n)] if dma else None
        self.i = 0
        self.renew()

    def renew(self):
        self.tks = self.P.tks(self.n)

    def next(self):
        i = self.i % self.n
        self.i += 1
        return i


class Psum:
    cnt = 0

    def __init__(self, P, es, nf=6, nt=2):
        nc = P.nc
        self.P = P
        self.nf = nf
        self.nt = nt
        Psum.cnt += 1
        self.ps = es.enter_context(nc.psum_tensor("psf%d" % Psum.cnt, [128, nf * 512], F32))
        self.pt = es.enter_context(nc.psum_tensor("pst%d" % Psum.cnt, [128, nt * 1024], BF16))
        self.i = 0
        self.j = 0
        self.tks = P.tks(nf)
        self.ttks = P.tks(nt)

    def fixed(self, b):
        return self.ps[:, b * 512:(b + 1) * 512], self.tks[b]

    def bank(self):
        b = self.i % self.nf
        self.i += 1
        return self.fixed(b)

    def bank2(self):
        if self.i % 2:
            self.i += 1
        b = self.i % 6
        self.i += 2
        return self.ps[:, b * 512:(b + 2) * 512], [self.tks[b], self.tks[b + 1]]

    def tbank(self):
        b = self.j % self.nt
        self.j += 1
        return self.pt[:, b * 1024:b * 1024 + 512], self.ttks[b]

    def tbank_full(self):
        b = self.j % self.nt
        self.j += 1
        return self.pt[:, b * 1024:(b + 1) * 1024], self.ttks[b]


class Ctx:
    pass


def layer_norm_rows(P, C, src, src_tk, gain, bias, gb_tk, out, out_tk):
    st = C.ln_stats
    i = st.next()
    stt_, stk = st.t[:, i], st.tks[i]
    P.rec("dve", lambda e: e.bn_stats(out=stt_[:, 0:6], in_=src[:, 0:512]), [src_tk], [stk])
    P.rec("dve", lambda e: e.bn_stats(out=stt_[:, 6:12], in_=src[:, 512:1024]), [src_tk], (), [stk])
    P.rec("dve", lambda e: e.bn_aggr(out=stt_[:, 12:14], in_=stt_[:, 0:12]), [stk], (), [stk])
    P.act(stt_[:, 14:15], stt_[:, 13:14], AF.Sqrt, [stk], (), [stk], bias=C.eps_ln[:, 0:1], scale=1.0)
    P.rec("dve", lambda e: e.reciprocal(out=stt_[:, 14:15], in_=stt_[:, 14:15]), [stk], (), [stk])
    P.ts("dve", stt_[:, 15:16], stt_[:, 12:13], stt_[:, 14:15], -1.0, ALU.mult, ALU.mult, [stk], (), [stk])
    P.act(out, src, AF.Identity, [src_tk, stk], [out_tk], bias=stt_[:, 15:16], scale=stt_[:, 14:15])
    P.tt("pool", out, out, gain, ALU.mult, [out_tk, gb_tk], [out_tk])
    P.tt("pool", out, out, bias, ALU.add, [out_tk, gb_tk], [out_tk])


def load_gain_bias(P, C, lng, lnb, layer, which, semc):
    for k, src in enumerate((lng, lnb)):
        off = (layer * 2 + which) * D
        ap = bass.AP(src.tensor, off, [[0, 128], [1, D]])
        P.dma("sp", C.gb[:, k, :], ap, semc, [], (), [C.gb_tk])


def convert_weights(P, C, src, dst, rows, cols, tklist, chunk_rows=512):
    r = 0
    while r < rows:
        n = min(chunk_rows, rows - r)
        tk = P.tk()
        s = C.conv_sems[C.conv_i % len(C.conv_sems)]
        C.conv_i += 1
        P.dma("pool", dst[r:r + n, :], src[r:r + n, :], s, [], [tk])
        tklist.append((r, r + n, tk))
        r += n


def conv_tks(tklist, r0, r1):
    return [tk for (a, b, tk) in tklist if a < r1 and b > r0]


def ffn_pass(P, C, es0, layer, x1_d, xo_d, wup_bf, wdn_bf, wup_tk, wdn_tk, lng, lnb):
    nc = P.nc
    with ExitStack() as es:
        C.ps = Psum(P, es)
        C.ln_stats.renew()
        wd = es.enter_context(sbuf(nc, "wd", [128, NFC, D], BF16))
        wd_tk = P.tk()
        wd_sem = P.dsem()
        xg = Ring(P, es, "f_xg", 2, [4, D], F32)
        xb = es.enter_context(sbuf(nc, "f_xb", [128, 4, D], BF16))
        xb_tk = P.tk()
        xT = es.enter_context(sbuf(nc, "f_xT", [128, 8, 512], BF16))
        xT_tk = P.tk()
        hT = es.enter_context(sbuf(nc, "f_hT", [128, NFC, 512], BF16))
        hT_tk = P.tk()
        wu = Ring(P, es, "f_wu", 4, [8, 256], BF16)
        sg = Ring(P, es, "f_sg", 3, [512], F32, dma=False)
        rr = Ring(P, es, "f_r", 2, [D], F32, dma=False)
        ot = Ring(P, es, "f_o", 3, [D], F32, sw=True)
        C.gb = es.enter_context(sbuf(nc, "f_gb", [128, 2, D], F32))
        C.gb_tk = P.tk()
        gsem = P.dsem()
        load_gain_bias(P, C, lng, lnb, layer, 1, gsem)
        for half in range(2):
            P.dma("sp", wd[:, half * 11:(half + 1) * 11, :],
                  wdn_bf[:, half * 11 * D:(half + 1) * 11 * D].rearrange("p (f d) -> p f d", d=D),
                  wd_sem, conv_tks(wdn_tk, 0, 128), (), [wd_tk])
        x1v = x1_d.rearrange("(n p) d -> p n d", p=128)
        xov = xo_d.rearrange("(n p) d -> p n d", p=128)

        def load_x(g):
            i = xg.next()
            P.dma("sp", xg.t[:, i], x1v[:, g * 4:(g + 1) * 4, :], xg.sems[i], [C.x1_tk[g]], [xg.tks[i]])
            return i

        nxt = load_x(0)
        wq = []

        def load_wu(j):
            i = wu.next()
            P.dma("sp", wu.t[:, i], wup_bf[j * 128:(j + 1) * 128, :].rearrange("p (k c) -> p k c", c=256),
                  wu.sems[i], conv_tks(wup_tk, j * 128, (j + 1) * 128), [wu.tks[i]])
            return i

        def x_to_xT(xi_):
            xgt_, xgk_ = xg.t[:, xi_], xg.tks[xi_]
            for a in range(4):
                P.cp("pool" if a % 2 else "dve", xb[:, a, :], xgt_[:, a, :], [xgk_], (), [xb_tk])
            for kc in range(8):
                pt, ptk = C.ps.tbank()
                for a in range(4):
                    P.tr(pt[:, a * 128:(a + 1) * 128], xb[:, a, kc * 128:(kc + 1) * 128], C.ident,
                         [xb_tk], [ptk] if a == 0 else (), () if a == 0 else [ptk])
                P.cp("act" if kc % 2 else "dve", xT[:, kc, :], pt, [ptk], (), [xT_tk])

        x_to_xT(nxt)
        for g in range(8):
            xi = nxt
            xgt, xg_tk = xg.t[:, xi], xg.tks[xi]
            if g + 1 < 8:
                nxt = load_x(g + 1)
            if not wq:
                wq = [load_wu(0), load_wu(1), load_wu(2)]
            for j in range(NFC):
                if j + 3 < NFC:
                    wq.append(load_wu(j + 3))
                elif g + 1 < 8:
                    wq.append(load_wu(j + 3 - NFC))
                wi = wq.pop(0)
                wt, wtk = wu.t[:, wi], wu.tks[wi]
                pg, pgk = C.ps.bank()
                pu, puk = C.ps.bank()
                for kc in range(8):
                    P.mm(pg, wt[:, kc, 0:128], xT[:, kc, :], kc == 0, kc == 7, [wtk, xT_tk],
                         [pgk] if kc == 0 else (), () if kc == 0 else [pgk])
                for kc in range(8):
                    P.mm(pu, wt[:, kc, 128:256], xT[:, kc, :], kc == 0, kc == 7, [wtk, xT_tk],
                         [puk] if kc == 0 else (), () if kc == 0 else [puk])
                si = sg.next()
                P.act(sg.t[:, si], pg, AF.Silu, [pgk], [sg.tks[si]])
                P.tt("dve", hT[:, j, :], sg.t[:, si], pu, ALU.mult, [sg.tks[si], puk], (), [hT_tk])
            if g + 1 < 8:
                x_to_xT(nxt)
            for a in range(4):
                po, pok = C.ps.bank2()
                for half in range(2):
                    for fc in range(NFC):
                        P.mm(po[:, half * 512:(half + 1) * 512], hT[:, fc, a * 128:(a + 1) * 128],
                             wd[:, fc, half * 512:(half + 1) * 512], fc == 0, fc == NFC - 1, [hT_tk, wd_tk],
                             [pok[half]] if fc == 0 else (), () if fc == 0 else [pok[half]])
                ri = rr.next()
                P.stt(rr.t[:, ri], xgt[:, a, :], ALPHA, po, ALU.mult, ALU.add, [xg_tk] + pok, [rr.tks[ri]])
                oi = ot.next()
                layer_norm_rows(P, C, rr.t[:, ri], rr.tks[ri], C.gb[:, 0, :], C.gb[:, 1, :], C.gb_tk,
                                ot.t[:, oi], ot.tks[oi])
                P.dma("pool", xov[:, g * 4 + a, :], ot.t[:, oi], ot.sems[oi], [ot.tks[oi]], (), [C.xo_tk[g]])
        P.flush()


def retention_pass(P, C, es0, layer, x_d, x1_d, win_bf, wout_bf, win_tk, wout_tk, lng, lnb, cd):
    nc = P.nc
    with ExitStack() as es:
        C.ps = Psum(P, es)
        C.ln_stats.renew()
        wo = es.enter_context(sbuf(nc, "r_wo", [128, 16, D], BF16))
        wo_tk = P.tk()
        wo_sem = P.dsem()
        st32 = es.enter_context(sbuf(nc, "r_st32", [128, 8, 512], F32))
        stbf = es.enter_context(sbuf(nc, "r_stbf", [128, 8, 512], BF16))
        st_tk = P.tks(8)
        stb_tk = P.tks(8)
        msk = es.enter_context(sbuf(nc, "r_msk", [128, 4, 128], F32))
        zs = es.enter_context(sbuf(nc, "r_zs", [128, 4], F32))
        cst_tk = P.tk()
        cst_sem = P.dsem()
        C.gb = es.enter_context(sbuf(nc, "r_gb", [128, 2, D], F32))
        C.gb_tk = P.tk()
        gsem = P.dsem()
        xg = Ring(P, es, "r_xg", 2, [D], F32)
        xb = es.enter_context(sbuf(nc, "r_xb", [128, 4, D], BF16))
        xb_tk = P.tk()
        xT = es.enter_context(sbuf(nc, "r_xT", [128, 8, 512], BF16))
        xT_tk = P.tk()
        gT = es.enter_context(sbuf(nc, "r_gT", [128, 16, 512], BF16))
        gT_tk = P.tk()
        wr = Ring(P, es, "r_w", 3, [8, 512], BF16)
        tq = Ring(P, es, "r_tq", 2, [2, 512], F32)
        tkk = Ring(P, es, "r_tk", 1, [2, 512], F32)
        qk = Ring(P, es, "r_qk", 2, [4, 512], BF16, dma=False)
        vs = Ring(P, es, "r_v", 2, [4, 512], BF16, dma=False)
        sgt = Ring(P, es, "r_sg", 2, [4, 512], BF16, dma=False)
        tmp = Ring(P, es, "r_tmp", 4, [512], F32, dma=False)
        sT = Ring(P, es, "r_sT", 2, [4, 128], BF16, dma=False)
        kz = Ring(P, es, "r_kz", 2, [4, 256], BF16, dma=False)
        yn = Ring(P, es, "r_yn", 2, [512], F32, dma=False)
        gg = Ring(P, es, "r_g", 5, [512], BF16, dma=False)
        gst = Ring(P, es, "r_gst", 6, [16], F32, dma=False)
        rr = Ring(P, es, "r_r", 2, [D], F32)
        ot = Ring(P, es, "r_o", 2, [D], F32, sw=True)

        load_gain_bias(P, C, lng, lnb, layer, 0, gsem)
        P.dma("sp", msk[:], cd["r_msk"], cst_sem, [], (), [cst_tk])
        P.dma("sp", zs[:], cd["r_zs"], cst_sem, [], (), [cst_tk])
        for q in range(4):
            P.dma("sp", wo[:, q * 4:(q + 1) * 4, :],
                  wout_bf[:, q * 4 * D:(q + 1) * 4 * D].rearrange("p (f d) -> p f d", d=D),
                  wo_sem, conv_tks(wout_tk, 0, 128), (), [wo_tk])
        for s in range(8):
            P.rec("dve", lambda e, s=s: e.memset(st32[:, s, :], 0.0), [], [st_tk[s]])
            P.rec("pool", lambda e, s=s: e.memset(stbf[:, s, :], 0.0), [], [stb_tk[s]])
        xv = x_d.rearrange("(n p) d -> p n d", p=128)
        x1v = x1_d.rearrange("(n p) d -> p n d", p=128)
        gam = [1.0 - 2.0 ** (-5.0 - h) for h in range(4)]
        gC = [float(np.float64(g) ** 128) for g in gam]

        wblocks = [(g, h, k) for g in range(8) for h in range(4) for k in range(3)]
        wslots = {}
        wstate = {"n": 0}

        def ensure_w(upto):
            while wstate["n"] <= min(upto, len(wblocks) - 1):
                g_, h_, k_ = wblocks[wstate["n"]]
                i = wr.next()
                r0 = (h_ * 3 + k_) * 128
                P.dma("sp", wr.t[:, i], win_bf[r0:r0 + 128, :].rearrange("p (k c) -> p k c", c=512),
                      wr.sems[i], [], [wr.tks[i]])
                wslots[wstate["n"]] = i
                wstate["n"] += 1

        def x_to_xT(g):
            for a in range(4):
                i = xg.next()
                P.dma("sp", xg.t[:, i], xv[:, g * 4 + a, :], xg.sems[i], [C.x_tk[g]], [xg.tks[i]])
                P.cp("pool" if a % 2 else "dve", xb[:, a, :], xg.t[:, i], [xg.tks[i]], (), [xb_tk])
            for kc in range(8):
                pt, ptk = C.ps.tbank()
                for a in range(4):
                    P.tr(pt[:, a * 128:(a + 1) * 128], xb[:, a, kc * 128:(kc + 1) * 128], C.ident,
                         [xb_tk], [ptk] if a == 0 else (), () if a == 0 else [ptk])
                P.cp("act" if kc % 2 else "dve", xT[:, kc, :], pt, [ptk], (), [xT_tk])

        x_to_xT(0)
        for g in range(8):
            ki = tkk.next()
            P.dma("sp", tkk.t[:, ki], cd["r_ropek"][:, :, g * 512:(g + 1) * 512], tkk.sems[ki], [], [tkk.tks[ki]])
            for pair in ((0, 1), (2, 3)):
                hctx = {}
                for h in pair:
                    qi = tq.next()
                    P.dma("sp", tq.t[:, qi], cd["r_ropeq"][h][:, :, g * 512:(g + 1) * 512], tq.sems[qi], [],
                          [tq.tks[qi]])
                    b0 = (g * 4 + h) * 3
                    ensure_w(b0)
                    wqk_i = wslots[b0]
                    qki = qk.next()
                    qkt, qk_tk = qk.t[:, qki], qk.tks[qki]
                    wt, wtk = wr.t[:, wqk_i], wr.tks[wqk_i]
                    for which in range(2):
                        pa, pak = C.ps.bank()
                        pb, pbk = C.ps.bank()
                        for part, (pp, ppk) in enumerate(((pa, pak), (pb, pbk))):
                            c0 = (which * 2 + part) * 128
                            for kc in range(8):
                                P.mm(pp, wt[:, kc, c0:c0 + 128], xT[:, kc, :], kc == 0, kc == 7, [wtk, xT_tk],
                                     [ppk] if kc == 0 else (), () if kc == 0 else [ppk])
                        if which == 0:
                            ct, st_, tbk = tq.t[:, qi, 0, :], tq.t[:, qi, 1, :], tq.tks[qi]
                        else:
                            ct, st_, tbk = tkk.t[:, ki, 0, :], tkk.t[:, ki, 1, :], tkk.tks[ki]
                        t1, t2, t3, t4 = [tmp.next() for _ in range(4)]
                        P.tt("dve", tmp.t[:, t1], pa, ct, ALU.mult, [pak, tbk], [tmp.tks[t1]])
                        P.tt("dve", tmp.t[:, t2], pb, st_, ALU.mult, [pbk, tbk], [tmp.tks[t2]])
                        P.tt("dve", tmp.t[:, t3], pa, st_, ALU.mult, [pak, tbk], [tmp.tks[t3]])
                        P.tt("dve", tmp.t[:, t4], pb, ct, ALU.mult, [pbk, tbk], [tmp.tks[t4]])
                        P.tt("pool", qkt[:, which * 2 + 0, :], tmp.t[:, t1], tmp.t[:, t2], ALU.subtract,
                             [tmp.tks[t1], tmp.tks[t2]], (), [qk_tk])
                        P.tt("pool", qkt[:, which * 2 + 1, :], tmp.t[:, t3], tmp.t[:, t4], ALU.add,
                             [tmp.tks[t3], tmp.tks[t4]], (), [qk_tk])
                    vi = vs.next()
                    sgi = sgt.next()
                    ensure_w(b0 + 1)
                    wv, wvk = wr.t[:, wslots[b0 + 1]], wr.tks[wslots[b0 + 1]]
                    for c in range(4):
                        pv, pvk = C.ps.bank()
                        for kc in range(8):
                            P.mm(pv, xT[:, kc, c * 128:(c + 1) * 128], wv[:, kc, :], kc == 0, kc == 7, [wvk, xT_tk],
                                 [pvk] if kc == 0 else (), () if kc == 0 else [pvk])
                        P.cp("act", vs.t[:, vi, c, :], pv, [pvk], (), [vs.tks[vi]])
                    ensure_w(b0 + 2)
                    wg, wgk = wr.t[:, wslots[b0 + 2]], wr.tks[wslots[b0 + 2]]
                    for c in range(4):
                        pg, pgk = C.ps.bank()
                        for kc in range(8):
                            P.mm(pg, xT[:, kc, c * 128:(c + 1) * 128], wg[:, kc, :], kc == 0, kc == 7, [wgk, xT_tk],
                                 [pgk] if kc == 0 else (), () if kc == 0 else [pgk])
                        P.act(sgt.t[:, sgi, c, :], pg, AF.Silu, [pgk], (), [sgt.tks[sgi]])
                    ensure_w(b0 + 5)
                    hctx[h] = (qkt, qk_tk, vi, sgi)
                if pair == (2, 3) and g + 1 < 8:
                    x_to_xT(g + 1)
                for h in pair:
                    qkt, qk_tk, vi, sgi = hctx[h]
                    ps_, psk = C.ps.bank()
                    first = True
                    for c in range(4):
                        cs = slice(c * 128, (c + 1) * 128)
                        for dc in range(2):
                            P.mm(ps_[:, cs], qkt[:, 2 + dc, cs], qkt[:, dc, cs], dc == 0, dc == 1, [qk_tk],
                                 [psk] if first else (), () if first else [psk])
                            first = False
                    si = sT.next()
                    mb = bass.AP(msk.tensor if hasattr(msk, "tensor") else msk, h * 128, [[512, 128], [0, 4], [1, 128]])
                    P.tt("dve", sT.t[:, si], ps_.rearrange("p (c i) -> p c i", i=128), mb, ALU.mult,
                         [psk, cst_tk], [sT.tks[si]])
                    pt, ptk = C.ps.tbank_full()
                    first = True
                    for c in range(4):
                        cs = slice(c * 128, (c + 1) * 128)
                        for dc in range(2):
                            o0 = c * 256 + dc * 128
                            P.tr(pt[:, o0:o0 + 128], qkt[:, 2 + dc, cs], C.ident, [qk_tk],
                                 [ptk] if first else (), () if first else [ptk])
                            first = False
                    kzi = kz.next()
                    P.act(kz.t[:, kzi].rearrange("p c d -> p (c d)"), pt, AF.Identity, [ptk, cst_tk], [kz.tks[kzi]],
                          scale=zs[:, h:h + 1])
                    hctx[h] = (qkt, qk_tk, vi, sgi, si, kzi)
                deferred = []

                def emit_e(item):
                    h_, c_, ggi_ = item
                    cs_ = slice(c_ * 128, (c_ + 1) * 128)
                    pt_, ptk_ = C.ps.tbank()
                    for ec in range(4):
                        P.tr(pt_[:, ec * 128:(ec + 1) * 128], gg.t[:, ggi_, ec * 128:(ec + 1) * 128], C.ident,
                             [gg.tks[ggi_]], [ptk_] if ec == 0 else (), () if ec == 0 else [ptk_])
                    P.cp("act", gT[:, h_ * 4:(h_ + 1) * 4, cs_], pt_.rearrange("p (e t) -> p e t", t=128), [ptk_], (),
                         [gT_tk])

                for c in range(4):
                    cs = slice(c * 128, (c + 1) * 128)
                    for h in pair:
                        qkt, qk_tk, vi, sgi, si, kzi = hctx[h]
                        pus = []
                        for dc in range(2):
                            pu, puk = C.ps.bank()
                            P.mm(pu, kz.t[:, kzi, c, dc * 128:(dc + 1) * 128], vs.t[:, vi, c, :], True, True,
                                 [kz.tks[kzi], vs.tks[vi]], [puk])
                            pus.append((pu, puk))
                        py, pyk = C.ps.bank()
                        P.mm(py, sT.t[:, si, c, :], vs.t[:, vi, c, :], True, False, [sT.tks[si], vs.tks[vi]], [pyk])
                        for dc in range(2):
                            P.mm(py, qkt[:, dc, cs], stbf[:, h * 2 + dc, :], False, dc == 1,
                                 [qk_tk, stb_tk[h * 2 + dc]], (), [pyk])
                        for dc in range(2):
                            s_ = h * 2 + dc
                            pu, puk = pus[dc]
                            P.stt(st32[:, s_, :], st32[:, s_, :], gC[h], pu, ALU.mult, ALU.add, [puk, st_tk[s_]],
                                  [st_tk[s_]])
                            P.cp("act", stbf[:, s_, :], st32[:, s_, :], [st_tk[s_]], [stb_tk[s_]])
                        gi = gst.next()
                        gs_, gsk = gst.t[:, gi], gst.tks[gi]
                        P.rec("dve", lambda e, gs_=gs_, py=py: e.bn_stats(out=gs_[:, 0:6], in_=py), [pyk], [gsk])
                        P.rec("dve", lambda e, gs_=gs_: e.bn_aggr(out=gs_[:, 6:8], in_=gs_[:, 0:6]), [gsk], (), [gsk])
                        P.act(gs_[:, 8:9], gs_[:, 7:8], AF.Sqrt, [gsk], (), [gsk], bias=C.eps_gn[:, 0:1], scale=1.0)
                        P.rec("dve", lambda e, gs_=gs_: e.reciprocal(out=gs_[:, 8:9], in_=gs_[:, 8:9]), [gsk], (),
                              [gsk])
                        P.ts("dve", gs_[:, 9:10], gs_[:, 6:7], gs_[:, 8:9], -1.0, ALU.mult, ALU.mult, [gsk], (),
                             [gsk])
                        yi = yn.next()
                        P.act(yn.t[:, yi], py, AF.Identity, [pyk, gsk], [yn.tks[yi]], bias=gs_[:, 9:10],
                              scale=gs_[:, 8:9])
                        ggi = gg.next()
                        P.tt("pool", gg.t[:, ggi], yn.t[:, yi], sgt.t[:, sgi, c, :], ALU.mult,
                             [yn.tks[yi], sgt.tks[sgi]], [gg.tks[ggi]])
                        deferred.append((h, c, ggi))
                        if len(deferred) > 3:
                            emit_e(deferred.pop(0))
                while deferred:
                    emit_e(deferred.pop(0))
            for a in range(4):
                po, pok = C.ps.bank2()
                for half in range(2):
                    for ec in range(16):
                        P.mm(po[:, half * 512:(half + 1) * 512], gT[:, ec, a * 128:(a + 1) * 128],
                             wo[:, ec, half * 512:(half + 1) * 512], ec == 0, ec == 15, [gT_tk, wo_tk],
                             [pok[half]] if ec == 0 else (), () if ec == 0 else [pok[half]])
                ri = rr.next()
                P.dma("sp", rr.t[:, ri], xv[:, g * 4 + a, :], rr.sems[ri], [C.x_tk[g]], [rr.tks[ri]])
                P.stt(rr.t[:, ri], rr.t[:, ri], ALPHA, po, ALU.mult, ALU.add, [rr.tks[ri]] + pok, [rr.tks[ri]])
                oi = ot.next()
                layer_norm_rows(P, C, rr.t[:, ri], rr.tks[ri], C.gb[:, 0, :], C.gb[:, 1, :], C.gb_tk,
                                ot.t[:, oi], ot.tks[oi])
                P.dma("pool", x1v[:, g * 4 + a, :], ot.t[:, oi], ot.sems[oi], [ot.tks[oi]], (), [C.x1_tk[g]])
        P.flush()


BL = 129 * 256


def bias_prep_pass(P, C, rel_bias_d, w8d, b8, cd):
    nc = P.nc
    with ExitStack() as es:
        C.ps = Psum(P, es)
        rbT = es.enter_context(sbuf(nc, "b_rbT", [32, 48], F32))
        oh = es.enter_context(sbuf(nc, "b_oh", [32, 3 * 512], F32))
        ng = es.enter_context(sbuf(nc, "b_ng", [16, 512], F32))
        w8 = es.enter_context(sbuf(nc, "b_w8", [16, 3, 512], F32))
        tk_in = P.tk()
        sem = P.dsem()
        osem = P.dsem()
        bsem = P.dsem()
        P.dma("sp", rbT[:, :], bass.AP(rel_bias_d.tensor, 0, [[1, 32], [32, 48]]), sem, [], (), [tk_in],
              allow_slow_non_contiguous=True)
        P.dma("sp", oh[:, :], cd["a_oh"], sem, [], (), [tk_in])
        P.dma("sp", ng[:, :], cd["a_negm"], sem, [], (), [tk_in])
        for g in range(3):
            pb, pbk = C.ps.bank()
            P.mm(pb[0:16, :], rbT[:, g * 16:(g + 1) * 16], oh[:, g * 512:(g + 1) * 512], True, True, [tk_in], [pbk])
            wtk = P.tk()
            P.tt("dve", w8[:, g, :], pb[0:16, :], ng[:, :], ALU.add, [pbk, tk_in], [wtk])
            dtk = P.tk()
            P.dma("sp", w8d[g * 16:(g + 1) * 16, :], w8[:, g, :], osem, [wtk], [dtk])
            for h in range(16):
                for c in range(2):
                    gh = g * 16 + h
                    P.dma("sp", b8[gh * 2 + c].rearrange("(a b) -> a b", b=256),
                          bass.AP(w8d.tensor, gh * 512 + c * 256, [[0, 129], [1, 256]]), bsem, [dtk], [P.tk()])
        P.flush()


def blocks_of(g):
    d = DILS[g]
    nb = 32 // d
    return d, nb


def attention_pass(P, C, layer, x_d, oT_d, win_bf, b8):
    nc = P.nc
    with ExitStack() as es:
        C.ps = Psum(P, es, nf=7, nt=1)
        xT = es.enter_context(sbuf(nc, "a_xT", [128, 8, T], BF16))
        xT_tk = P.tk()
        oacc = es.enter_context(sbuf(nc, "a_oacc", [128, T], F32))
        dacc = es.enter_context(sbuf(nc, "a_dacc", [128, T], F32))
        oacc_tk = P.tk()
        dacc_tk = P.tk()
        ones = es.enter_context(sbuf(nc, "a_ones", [128, 2, 128], BF16))
        ones_tk = P.tk()
        qTr = Ring(P, es, "a_qT", 1, [2, T], BF16, dma=False)
        kTr = Ring(P, es, "a_kT", 2, [T], BF16, dma=False)
        vbr = Ring(P, es, "a_vb", 1, [32, 2, 128], BF16, dma=False)
        wr = Ring(P, es, "a_w", 2, [8, 384], BF16)
        w8r = Ring(P, es, "a_w8", 2, [2, 2, 128], F32)
        ssr = Ring(P, es, "a_ss", 3, [512], F32, dma=False)
        ptr = Ring(P, es, "a_pT", 4, [512], BF16, dma=False)
        C.att_i = 0
        C.att_s = 0
        onr = Ring(P, es, "a_on", 2, [1024], BF16)
        rcr = Ring(P, es, "a_rc", 2, [1024], F32, dma=False)
        xg = Ring(P, es, "a_xg", 2, [D], F32)
        xbr = Ring(P, es, "a_xb", 2, [D], BF16, dma=False)
        xv = x_d.rearrange("(n p) d -> p n d", p=128)
        P.rec("pool", lambda e: e.memset(ones[:, :, :], 0.0), [], [ones_tk])
        P.rec("pool", lambda e: e.memset(ones[:, 0, 0:64], 1.0), [ones_tk], [ones_tk])
        P.rec("pool", lambda e: e.memset(ones[:, 1, 64:128], 1.0), [ones_tk], [ones_tk])
        P.rec("pool", lambda e: e.memset(qTr.t[:, 0], 0.0), [], [qTr.tks[0]])
        P.rec("pool", lambda e: e.memset(vbr.t[:, 0], 0.0), [], [vbr.tks[0]])
        for t in range(NT):
            i = xg.next()
            P.dma("sp", xg.t[:, i], xv[:, t, :], xg.sems[i], [C.x_tk[t // 4]], [xg.tks[i]])
            bi = xbr.next()
            P.cp("pool" if t % 2 else "dve", xbr.t[:, bi], xg.t[:, i], [xg.tks[i]], [xbr.tks[bi]])
            for half in range(2):
                pt, ptk = C.ps.tbank()
                for q in range(4):
                    kc = half * 4 + q
                    P.tr(pt[:, q * 128:(q + 1) * 128], xbr.t[:, bi, kc * 128:(kc + 1) * 128], C.ident,
                         [xbr.tks[bi]], [ptk] if q == 0 else (), () if q == 0 else [ptk])
                P.cp("act" if half else "dve", xT[:, half * 4:(half + 1) * 4, t * 128:(t + 1) * 128],
                     pt.rearrange("p (k t) -> p k t", t=128), [ptk], (), [xT_tk])

        if DBG == 2:
            P.flush()
            return
        nblk = 24
        wslot = {}
        wn = {"n": 0}

        def ensure_w(upto):
            while wn["n"] <= min(upto, nblk - 1):
                b = wn["n"]
                i = wr.next()
                P.dma("sp", wr.t[:, i], win_bf[b * 128:(b + 1) * 128, :].rearrange("p (k c) -> p k c", c=384),
                      wr.sems[i], [], [wr.tks[i]])
                wslot[b] = i
                wn["n"] += 1

        ensure_w(1)
        for hp in range(8):
            for g in range(3):
                d, nb = blocks_of(g)
                b = hp * 3 + g
                ensure_w(b)
                wt, wtk = wr.t[:, wslot[b]], wr.tks[wslot[b]]
                w8i = w8r.next()
                for hd in range(2):
                    gh = g * 16 + hp * 2 + hd
                    P.dma("sp", w8r.t[:, w8i, :, hd, :], bass.AP(b8.tensor, gh * 2 * BL + 127, [[255, 128], [BL, 2], [1, 128]]),
                          w8r.sems[w8i], [], [w8r.tks[w8i]] if hd == 0 else (), () if hd == 0 else [w8r.tks[w8i]])
                w8t, w8k = w8r.t[:, w8i], w8r.tks[w8i]
                qi, ki, vi = qTr.next(), kTr.next(), vbr.next()
                qT, qk_ = qTr.t[:, qi], qTr.tks[qi]
                kT, kk_ = kTr.t[:, ki], kTr.tks[ki]
                vb, vk_ = vbr.t[:, vi], vbr.tks[vi]
                for tg in range(8):
                    pp, ppk = C.ps.bank()
                    for kc in range(8):
                        P.mm(pp, wt[:, kc, 0:128], xT[:, kc, tg * 512:(tg + 1) * 512],
                             kc == 0, kc == 7, [wtk, xT_tk], [ppk] if kc == 0 else (), () if kc == 0 else [ppk])
                    ts2 = slice(tg * 512, (tg + 1) * 512)
                    ev = "act" if tg % 2 else "dve"
                    P.cp(ev, qT[0:64, 0, ts2], pp[0:64, :], [ppk], (), [qk_])
                    P.cp(ev, qT[64:128, 1, ts2], pp[64:128, :], [ppk], (), [qk_])
                for tg in range(8):
                    pp, ppk = C.ps.bank()
                    for kc in range(8):
                        P.mm(pp, wt[:, kc, 128:256], xT[:, kc, tg * 512:(tg + 1) * 512],
                             kc == 0, kc == 7, [wtk, xT_tk], [ppk] if kc == 0 else (), () if kc == 0 else [ppk])
                    P.cp("dve" if tg % 2 else "act", kT[:, tg * 512:(tg + 1) * 512], pp, [ppk], (), [kk_])

                def tok(r, n0, cnt=1):
                    s0 = n0 * 128 * d + r
                    return slice(s0, min(s0 + cnt * 128 * d, T), d)

                blist = [(r, n) for r in range(d) for n in range(nb)]
                for b4 in range(8 if DBG not in (31, 32) else 0):
                    pv, pvk = C.ps.bank()
                    for q in range(4):
                        r, n = blist[b4 * 4 + q]
                        for kc in range(8):
                            first = (q == 0 and kc == 0)
                            P.mm(pv[:, q * 128:(q + 1) * 128], xT[:, kc, tok(r, n)], wt[:, kc, 256:384],
                                 kc == 0, kc == 7, [wtk, xT_tk], [pvk] if first else (), () if first else [pvk])
                    pv3 = pv.rearrange("p (q c) -> p q c", c=128)
                    ev = "act" if b4 % 2 else "dve"
                    P.cp(ev, vb[:, b4 * 4:(b4 + 1) * 4, 0, 0:64], pv3[:, :, 0:64], [pvk], (), [vk_])
                    P.cp(ev, vb[:, b4 * 4:(b4 + 1) * 4, 1, 64:128], pv3[:, :, 64:128], [pvk], (), [vk_])
                ensure_w(b + 1)
                if DBG in (3, 31, 32, 33, 34, 35):
                    continue
                if DBG in (4, 6, 7, 8) and g > 0:
                    continue
                if DBG == 5 and g > 1:
                    continue
                nbat = min(4, nb)
                items = []
                for r in range(d):
                    for n0 in range(0, nb, nbat):
                        pob = 0 if (C.att_i % 2 == 0) else 2
                        C.att_i += 1
                        for nn in range(nbat):
                            items.append((r, n0, nn, pob))
                LA = 2
                pend = {}

                def stage_a(it):
                    r, n0, nn, pob = it
                    n = n0 + nn
                    nch = 2 if n > 0 else 1
                    wv_ = nch * 256
                    ps_, psk = C.ps.fixed(4 + C.att_s % 3)
                    C.att_s += 1
                    for c in range(nch):
                        P.mm(ps_[:, c * 256:(c + 1) * 256], kT[:, tok(r, n - c)], qT[:, :, tok(r, n)],
                             True, True, [qk_, kk_], [psk] if c == 0 else (), () if c == 0 else [psk])
                    si, pi = ssr.next(), ptr.next()
                    P.stt(ssr.t[:, si, 0:wv_], ps_[:, 0:wv_], 600.0,
                          w8t.rearrange("p c h i -> p (c h i)")[:, 0:wv_], ALU.min, ALU.add,
                          [psk, w8k], [ssr.tks[si]])
                    P.act(ptr.t[:, pi, 0:wv_], ssr.t[:, si, 0:wv_], AF.Exp, [ssr.tks[si]], [ptr.tks[pi]],
                          scale=0.125)
                    pend[it] = pi

                def stage_b(it):
                    r, n0, nn, pob = it
                    n = n0 + nn
                    nch = 2 if n > 0 else 1
                    bc = r * nb + n
                    pi = pend.pop(it)
                    po, pok = C.ps.fixed(pob)
                    pd, pdk = C.ps.fixed(pob + 1)
                    k = 0
                    for c in range(nch):
                        for hd in range(2):
                            pTs = ptr.t[:, pi, (c * 2 + hd) * 128:(c * 2 + hd + 1) * 128]
                            fo = (nn == 0 and k == 0)
                            P.mm(po[:, nn * 128:(nn + 1) * 128], vb[:, bc - c, hd, :], pTs, k == 0,
                                 k == 2 * nch - 1, [vk_, ptr.tks[pi]], [pok] if fo else (), () if fo else [pok])
                            k += 1
                    k = 0
                    for c in range(nch):
                        for hd in range(2):
                            pTs = ptr.t[:, pi, (c * 2 + hd) * 128:(c * 2 + hd + 1) * 128]
                            fo = (nn == 0 and k == 0)
                            P.mm(pd[:, nn * 128:(nn + 1) * 128], ones[:, hd, :], pTs, k == 0,
                                 k == 2 * nch - 1, [ones_tk, ptr.tks[pi]], [pdk] if fo else (), () if fo else [pdk])
                            k += 1
                    if nn == nbat - 1:
                        ts_ = tok(r, n0, nbat)
                        w = nbat * 128
                        if g == 0:
                            P.cp("act", oacc[:, ts_], po[:, 0:w], [pok], (), [oacc_tk])
                            P.cp("act", dacc[:, ts_], pd[:, 0:w], [pdk], (), [dacc_tk])
                        else:
                            P.tt("dve", oacc[:, ts_], oacc[:, ts_], po[:, 0:w], ALU.add, [pok, oacc_tk], (), [oacc_tk])
                            P.tt("dve", dacc[:, ts_], dacc[:, ts_], pd[:, 0:w], ALU.add, [pdk, dacc_tk], (), [dacc_tk])

                for i in range(len(items) + LA):
                    if i < len(items):
                        stage_a(items[i])
                    if i >= LA:
                        stage_b(items[i - LA])
            if DBG in (3, 6, 7, 8, 31, 32, 33, 34, 35):
                continue
            for q in range(4):
                cs = slice(q * 1024, (q + 1) * 1024)
                ri, oi = rcr.next(), onr.next()
                P.rec("dve", lambda e, ri=ri, cs=cs: e.reciprocal(out=rcr.t[:, ri], in_=dacc[:, cs]), [dacc_tk],
                      [rcr.tks[ri]])
                P.tt("pool", onr.t[:, oi], oacc[:, cs], rcr.t[:, ri], ALU.mult, [oacc_tk, rcr.tks[ri]], [onr.tks[oi]])
                P.dma("sp", oT_d[hp, :, cs], onr.t[:, oi], onr.sems[oi], [onr.tks[oi]], (), [C.oT_tk])
        P.flush()


def outproj_pass(P, C, layer, x_d, x1_d, oT_d, wout_bf, lng, lnb):
    nc = P.nc
    with ExitStack() as es:
        C.ps = Psum(P, es)
        C.ln_stats.renew()
        wo = es.enter_context(sbuf(nc, "o_wo", [128, 8, D], BF16))
        wo_tk = P.tk()
        wo_sem = P.dsem()
        C.gb = es.enter_context(sbuf(nc, "o_gb", [128, 2, D], F32))
        C.gb_tk = P.tk()
        gsem = P.dsem()
        oT = Ring(P, es, "o_oT", 2, [8, 512], BF16)
        rr = Ring(P, es, "o_r", 4, [D], F32)
        ot = Ring(P, es, "o_o", 3, [D], F32, sw=True)
        load_gain_bias(P, C, lng, lnb, layer, 0, gsem)
        for q in range(2):
            P.dma("sp", wo[:, q * 4:(q + 1) * 4, :],
                  wout_bf[:, q * 4 * D:(q + 1) * 4 * D].rearrange("p (f d) -> p f d", d=D), wo_sem, [], (), [wo_tk])
        xv = x_d.rearrange("(n p) d -> p n d", p=128)
        x1v = x1_d.rearrange("(n p) d -> p n d", p=128)
        rslot = {}

        def load_res(tile):
            if tile < NT:
                ri_ = rr.next()
                P.dma("sp", rr.t[:, ri_], xv[:, tile, :], rr.sems[ri_], [C.x_tk[tile // 4]], [rr.tks[ri_]])
                rslot[tile] = ri_

        load_res(0)
        load_res(1)
        for g in range(8):
            oi_ = oT.next()
            P.dma("sp", oT.t[:, oi_], oT_d[:, :, g * 512:(g + 1) * 512].rearrange("h p t -> p h t"), oT.sems[oi_],
                  [C.oT_tk], [oT.tks[oi_]])
            for a in range(4):
                load_res(g * 4 + a + 2)
                po, pok = C.ps.bank2()
                for half in range(2):
                    for hp in range(8):
                        P.mm(po[:, half * 512:(half + 1) * 512], oT.t[:, oi_, hp, a * 128:(a + 1) * 128],
                             wo[:, hp, half * 512:(half + 1) * 512], hp == 0, hp == 7, [oT.tks[oi_], wo_tk],
                             [pok[half]] if hp == 0 else (), () if hp == 0 else [pok[half]])
                ri = rslot.pop(g * 4 + a)
                P.stt(rr.t[:, ri], rr.t[:, ri], ALPHA, po, ALU.mult, ALU.add, [rr.tks[ri]] + pok, [rr.tks[ri]])
                oi = ot.next()
                layer_norm_rows(P, C, rr.t[:, ri], rr.tks[ri], C.gb[:, 0, :], C.gb[:, 1, :], C.gb_tk,
                                ot.t[:, oi], ot.tks[oi])
                P.dma("pool", x1v[:, g * 4 + a, :], ot.t[:, oi], ot.sems[oi], [ot.tks[oi]], (), [C.x1_tk[g]])
        P.flush()


def _t5_bucket_np(dist):
    max_exact = 16
    d_f = np.maximum(dist, 1).astype(np.float32)
    large = max_exact + (np.log(d_f / np.float32(max_exact)) / np.float32(math.log(2048 / max_exact))
                         * np.float32(32 - max_exact)).astype(np.int32)
    large = np.minimum(large, 31)
    return np.where(dist < max_exact, dist, large)


def make_consts():
    c = {}
    c["ident"] = np.eye(128, dtype=np.float32)
    inv = (1.0 / (np.float32(10000.0) ** np.linspace(0.0, 1.0, 128, dtype=np.float32))).astype(np.float32)
    ang = (np.arange(T, dtype=np.float32)[:, None] * inv[None, :]).astype(np.float32)
    cos = np.cos(ang.astype(np.float64)).T
    sin = np.sin(ang.astype(np.float64)).T
    c["r_ropek"] = np.stack([cos, sin], axis=1).astype(np.float32)
    gam = np.array([1.0 - 2.0 ** (-5.0 - h) for h in range(4)], dtype=np.float64)
    il = (np.arange(T) % 128).astype(np.float64)
    rq = np.zeros((4, 128, 2, T), np.float32)
    for h in range(4):
        xi = gam[h] ** (il + 1.0)
        rq[h, :, 0, :] = cos * xi[None, :]
        rq[h, :, 1, :] = sin * xi[None, :]
    c["r_ropeq"] = rq
    j = np.arange(128)[:, None].astype(np.float64)
    i = np.arange(128)[None, :].astype(np.float64)
    msk = np.zeros((128, 4, 128), np.float32)
    zs = np.zeros((128, 4), np.float32)
    for h in range(4):
        msk[:, h, :] = np.where(i >= j, gam[h] ** (-(j + 1.0)) / 16.0, 0.0)
        zs[:, h] = gam[h] ** (127.0 - np.arange(128)) / 16.0
    c["r_msk"] = msk
    c["r_zs"] = zs
    oh = np.zeros((32, 3, 2, 256), np.float32)
    negm = np.zeros((16, 2, 256), np.float32)
    u = np.arange(255)
    for cc in range(2):
        delta = (u - 127) if cc == 0 else (u + 1)
        valid = (delta >= 0) if cc == 0 else (delta <= 128)
        negm[:, cc, :255] = np.where(valid, 0.0, NEG)[None, :]
        negm[:, cc, 255] = NEG
        for g, dil in enumerate(DILS):
            bkt = _t5_bucket_np(np.maximum(delta, 0) * dil)
            for uu in range(255):
                if valid[uu]:
                    oh[bkt[uu], g, cc, uu] = 8.0
    c["a_oh"] = oh.reshape(32, 3 * 512)
    c["a_negm"] = negm.reshape(16, 512)
    return c


def layout_weights(inp):
    w = {}
    for l in range(DEPTH):
        Wu = inp["ffn_w_up"][l]
        gt = Wu[:, :DFF].reshape(8, 128, NFC, 128)
        up = Wu[:, DFF:].reshape(8, 128, NFC, 128)
        blk = np.concatenate([gt, up], axis=3)
        w["wup%d" % l] = np.ascontiguousarray(blk.transpose(2, 1, 0, 3)).reshape(NFC * 128, 2048)
        Wd = inp["ffn_w_down"][l].reshape(NFC, 128, D)
        w["wdn%d" % l] = np.ascontiguousarray(Wd.transpose(1, 0, 2)).reshape(128 * 11, 2048)
    for j in range(2):
        Wi = inp["ret_w_in"][j]
        blocks = []
        for h in range(4):
            q = Wi[:, h * 256:(h + 1) * 256]
            k = Wi[:, 1024 + h * 256:1024 + (h + 1) * 256]
            qk = np.concatenate([q[:, 0::2], q[:, 1::2], k[:, 0::2], k[:, 1::2]], axis=1)
            v = Wi[:, 2048 + h * 512:2048 + (h + 1) * 512]
            gte = Wi[:, 4096 + h * 512:4096 + (h + 1) * 512]
            for m in (qk, v, gte):
                blocks.append(m.reshape(8, 128, 512).transpose(1, 0, 2).reshape(128, 4096))
        w["rwin%d" % j] = np.ascontiguousarray(np.concatenate(blocks, axis=0)).reshape(12 * 128 * 2, 2048)
        Wo = inp["ret_w_out"][j].reshape(16, 128, D)
        w["rwout%d" % j] = np.ascontiguousarray(Wo.transpose(1, 0, 2)).reshape(128 * 8, 2048)
        Wa = inp["attn_w_in"][j]
        blocks = []
        for hp in range(8):
            for g in range(3):
                cols = []
                for s_ in range(3):
                    c0 = ((s_ * 3 + g) * 16 + 2 * hp) * 64
                    cols.append(Wa[:, c0:c0 + 128])
                m = np.concatenate(cols, axis=1)
                blocks.append(m.reshape(8, 128, 384).transpose(1, 0, 2).reshape(128, 3072))
        w["awin%d" % j] = np.ascontiguousarray(np.concatenate(blocks, axis=0)).reshape(24 * 128 * 2, 1536)
        Wao = inp["attn_w_out"][j].reshape(8, 128, D)
        w["awout%d" % j] = np.ascontiguousarray(Wao.transpose(1, 0, 2)).reshape(128 * 4, 2048)
    return w


LAYERS_DEFAULT = (0, 1, 2, 3)


def build_program(layers=LAYERS_DEFAULT, debug_out=None):
    nc = bass.Bass("TRN2", target_bir_lowering=False)
    x_in = nc.dram_tensor("x", [T, D], F32, kind="ExternalInput").ap()
    y_out = nc.dram_tensor("y", [T, D], F32, kind="ExternalOutput").ap()
    lng = nc.dram_tensor("ln_gain", [DEPTH * 2 * D], F32, kind="ExternalInput").ap()
    lnb = nc.dram_tensor("ln_bias", [DEPTH * 2 * D], F32, kind="ExternalInput").ap()
    consts = make_consts()
    cd = {}
    for k, v in consts.items():
        cd[k] = nc.dram_tensor("c_" + k, list(v.shape), F32, kind="ExternalInput").ap()
    wsrc = {}
    wshapes = {}
    for l in layers:
        wshapes["wup%d" % l] = (NFC * 128, 2048)
        wshapes["wdn%d" % l] = (128 * 11, 2048)
        if l % 2 == 0:
            wshapes["rwin%d" % (l // 2)] = (12 * 128 * 2, 2048)
            wshapes["rwout%d" % (l // 2)] = (128 * 8, 2048)
        else:
            wshapes["awin%d" % (l // 2)] = (24 * 128 * 2, 1536)
            wshapes["awout%d" % (l // 2)] = (128 * 4, 2048)
    for k, shp in wshapes.items():
        wsrc[k] = nc.dram_tensor(k, list(shp), F32, kind="ExternalInput").ap()
    wbf = {k: nc.dram_tensor(k + "_bf", list(shp), BF16).ap() for k, shp in wshapes.items()}
    xa = nc.dram_tensor("xa", [T, D], F32).ap()
    xb_ = nc.dram_tensor("xb", [T, D], F32).ap()
    x1 = nc.dram_tensor("x1", [T, D], F32).ap()
    oT_d = nc.dram_tensor("oT_d", [8, 128, T], BF16).ap()
    w8d = nc.dram_tensor("w8d", [48, 512], F32).ap()
    b8 = nc.dram_tensor("b8", [96, BL], F32).ap()
    has_attn = any(l % 2 == 1 for l in layers)
    rel_bias_d = nc.dram_tensor("rel_bias", [48, 32], F32, kind="ExternalInput").ap() if has_attn else None

    with ExitStack() as es:
        P = Prog(nc, es)
        C = Ctx()
        C.ln_stats = Ring(P, es, "lnst", 4, [16], F32, dma=False)
        C.ident_t = es.enter_context(sbuf(nc, "ident", [128, 128], BF16))
        C.ident = C.ident_t[:, :]
        C.eps_ln = es.enter_context(sbuf(nc, "eps_ln", [128, 1], F32))
        C.eps_gn = es.enter_context(sbuf(nc, "eps_gn", [128, 1], F32))
        C.conv_sems = [SemC(es.enter_context(nc.semaphore("cv%d" % i))) for i in range(4)]
        C.conv_i = 0
        isem = SemC(es.enter_context(nc.semaphore("cv_i")))
        P.dma("pool", C.ident, cd["ident"], isem, [], [P.tk()])
        P.rec("dve", lambda e: e.memset(C.eps_ln[:], LN_EPS), [], [P.tk()])
        P.rec("dve", lambda e: e.memset(C.eps_gn[:], GN_EPS), [], [P.tk()])
        wtk = {k: [] for k in wshapes}
        pass_keys = []
        for l in layers:
            if l % 2 == 0:
                pass_keys.append(["rwin%d" % (l // 2), "rwout%d" % (l // 2)])
            else:
                pass_keys.append(["awin%d" % (l // 2)])
                pass_keys.append(["awout%d" % (l // 2)])
            pass_keys.append(["wup%d" % l, "wdn%d" % l])
        C.pass_i = 0

        def conv_for(pi):
            if pi < len(pass_keys):
                for k in pass_keys[pi]:
                    convert_weights(P, C, wsrc[k], wbf[k], wshapes[k][0], wshapes[k][1], wtk[k])

        def next_pass():
            C.pass_i += 1
            conv_for(C.pass_i)

        conv_for(0)
        P.flush()
        if has_attn:
            bias_prep_pass(P, C, rel_bias_d, w8d, b8, cd)
        if DBG == 1:
            return nc, consts, list(wshapes.keys())

        cur = x_in
        for li, l in enumerate(layers):
            last = li == len(layers) - 1
            nxt = y_out if last else (xa if cur is not xa else xb_)
            C.x_tk = P.tks(8)
            C.x1_tk = P.tks(8)
            C.xo_tk = P.tks(8)
            if l % 2 == 0:
                j = l // 2
                next_pass()
                retention_pass(P, C, es, l, cur, x1, wbf["rwin%d" % j].rearrange("(r two) c -> r (two c)", two=2),
                               wbf["rwout%d" % j].rearrange("(p e) c -> p (e c)", e=8),
                               wtk["rwin%d" % j], wtk["rwout%d" % j], lng, lnb, cd)
            else:
                j = l // 2
                C.oT_tk = P.tk()
                next_pass()
                attention_pass(P, C, l, cur, oT_d, wbf["awin%d" % j].rearrange("(r two) c -> r (two c)", two=2), b8)
                if DBG:
                    return nc, consts, list(wshapes.keys())
                C.x_tk = P.tks(8)
                C.x1_tk = P.tks(8)
                C.oT_tk = P.tk()
                next_pass()
                outproj_pass(P, C, l, cur, x1, oT_d, wbf["awout%d" % j].rearrange("(p e) c -> p (e c)", e=4),
                             lng, lnb)
            C.x_tk = P.tks(8)
            C.x1_tk = P.tks(8)
            C.xo_tk = P.tks(8)
            next_pass()
            ffn_pass(P, C, es, l, x1, nxt, wbf["wup%d" % l],
                     wbf["wdn%d" % l].rearrange("(p e) c -> p (e c)", e=11),
                     wtk["wup%d" % l], wtk["wdn%d" % l], lng, lnb)
            cur = nxt
    return nc, consts, list(wshapes.keys())


_CACHE = {}


def kernel(x, ret_w_in, ret_w_out, attn_w_in, attn_w_out, rel_bias, ffn_w_up, ffn_w_down, ln_gain, ln_bias):
    inp = dict(x=np.asarray(x), ret_w_in=np.asarray(ret_w_in), ret_w_out=np.asarray(ret_w_out),
               attn_w_in=np.asarray(attn_w_in), attn_w_out=np.asarray(attn_w_out), rel_bias=np.asarray(rel_bias),
               ffn_w_up=np.asarray(ffn_w_up), ffn_w_down=np.asarray(ffn_w_down))
    nc, consts, wkeys = build_program()
    w = layout_weights(inp)
    shared = {"ln_gain": np.ascontiguousarray(np.asarray(ln_gain, np.float32).reshape(-1)),
              "ln_bias": np.ascontiguousarray(np.asarray(ln_bias, np.float32).reshape(-1)),
              "rel_bias": np.ascontiguousarray(inp["rel_bias"])}
    for k, v in consts.items():
        shared["c_" + k] = v
    for k in wkeys:
        shared[k] = w[k]
    in_maps = []
    for b in range(NCORES):
        m = dict(shared)
        m["x"] = np.ascontiguousarray(inp["x"][b])
        in_maps.append(m)
    res = run_bass_kernel_spmd(nc, in_maps, core_ids=list(range(NCORES)))
    return np.stack([np.asarray(r["y"]) for r in res.results], axis=0).astype(np.float32)
```

```python
import math
from contextlib import ExitStack

import numpy as np
import concourse.bass as bass
import concourse.mybir as mybir
from concourse.bass_utils import run_bass_kernel_spmd

F32 = mybir.dt.float32
BF16 = mybir.dt.bfloat16
AF = mybir.ActivationFunctionType
ALU = mybir.AluOpType

D = 1024
T = 4096
NT = T // 128
DEPTH = 4
DFF = 2816
NFC = DFF // 128
ALPHA = float((2 * DEPTH) ** 0.25)
LN_EPS = 1e-5
GN_EPS = 1e-5
NCORES = 8
SAME_ENGINE_SYNC = True
NEG = -1.0e5
DILS = (1, 4, 16)
DBG = 0


class Tk:
    __slots__ = ("w", "r", "gen")

    def __init__(self):
        self.w = []
        self.r = []
        self.gen = []


class SemC:
    def __init__(self, sem):
        self.sem = sem
        self.count = 0


class Op:
    __slots__ = ("eng", "fn", "deps", "need_inc", "semc", "val", "isdma", "idx")


def _add_latest(lst, op):
    if not op.isdma:
        for i, q in enumerate(lst):
            if (not q.isdma) and q.eng == op.eng:
                lst[i] = op
                return
    lst.append(op)


class Prog:
    CE = ("pe", "act", "dve", "pool")

    def __init__(self, nc, es):
        self.nc = nc
        self.es = es
        self.csem = {e: SemC(es.enter_context(nc.semaphore("c_" + e))) for e in self.CE}
        self.lists = {e: [] for e in ("pe", "act", "dve", "pool", "sp")}
        self.n = 0
        self.tiles = []
        self.nsem = 0
        self.sempool = []
        self.nsw = 0
        self.swpool = []

    def dsem_sw(self):
        if self.nsw == len(self.swpool):
            self.swpool.append(SemC(self.es.enter_context(self.nc.semaphore("sw%d" % self.nsw))))
        c = self.swpool[self.nsw]
        self.nsw += 1
        return c

    def tk(self):
        t = Tk()
        self.tiles.append(t)
        return t

    def tks(self, n):
        return [self.tk() for _ in range(n)]

    def dsem(self):
        if self.nsem == len(self.sempool):
            self.sempool.append(SemC(self.es.enter_context(self.nc.semaphore("d%d" % self.nsem))))
        c = self.sempool[self.nsem]
        self.nsem += 1
        return c

    def rec(self, eng, fn, reads=(), writes=(), pw=(), semc=None):
        op = Op()
        op.eng = eng
        op.fn = fn
        op.need_inc = False
        op.isdma = semc is not None
        op.idx = self.n
        op.semc = None
        op.val = 0
        self.n += 1
        deps = {}

        def add(p):
            if p.isdma:
                deps[id(p)] = p
            else:
                q = deps.get(p.eng)
                if q is None or q.idx < p.idx:
                    deps[p.eng] = p

        for t in reads:
            for p in t.w:
                add(p)
        for t in writes:
            for p in t.r:
                add(p)
            for p in t.w:
                add(p)
        for t in pw:
            if t.r:
                t.gen = t.r + t.w
            for p in t.gen:
                add(p)
        for t in reads:
            _add_latest(t.r, op)
        for t in writes:
            t.w = [op]
            t.r = []
            t.gen = [op]
        for t in pw:
            if t.r:
                t.w = [op]
                t.r = []
            else:
                _add_latest(t.w, op)
        dl = []
        for p in deps.values():
            if (not p.isdma) and (not op.isdma) and p.eng == eng and (eng == "pe" or not SAME_ENGINE_SYNC):
                continue
            p.need_inc = True
            dl.append(p)
        op.deps = dl
        if semc is not None:
            semc.count += 16
            op.semc = semc
            op.val = semc.count
        self.lists[eng].append(op)
        return op

    def flush(self):
        for e in self.CE:
            c = self.csem[e]
            for op in self.lists[e]:
                if (not op.isdma) and op.need_inc:
                    c.count += 1
                    op.semc = c
                    op.val = c.count
        lists = self.lists
        with self.nc.Block() as block:
            for e, deco in (("pe", block.tensor), ("act", block.scalar), ("dve", block.vector),
                            ("pool", block.gpsimd), ("sp", block.sync)):
                ops = lists[e]

                def body(eng, ops=ops):
                    waited = {}
                    final = {}
                    for op in ops:
                        need = {}
                        for p in op.deps:
                            k = id(p.semc)
                            if waited.get(k, 0) >= p.val:
                                continue
                            if k not in need or need[k][1] < p.val:
                                need[k] = (p.semc, p.val)
                        for k, (c, v) in need.items():
                            eng.wait_ge(c.sem, v)
                            waited[k] = v
                        ins = op.fn(eng)
                        if op.isdma:
                            ins.then_inc(op.semc.sem, 16)
                            final[id(op.semc)] = (op.semc, op.val)
                        elif op.need_inc:
                            ins.then_inc(op.semc.sem, 1)
                    for k, (c, v) in final.items():
                        if waited.get(k, 0) < v:
                            eng.wait_ge(c.sem, v)

                deco(body)
        self.lists = {e: [] for e in ("pe", "act", "dve", "pool", "sp")}
        self.nsem = 0
        self.nsw = 0
        for t in self.tiles:
            t.w = []
            t.r = []
            t.gen = []
        self.tiles = []

    def mm(self, out, lhsT, rhs, start, stop, reads, writes=(), pw=()):
        return self.rec("pe", lambda e: e.matmul(out, lhsT, rhs, start=start, stop=stop), reads, writes, pw)

    def tr(self, out, in_, ident, reads, writes=(), pw=()):
        return self.rec("pe", lambda e: e.transpose(out, in_, ident), reads, writes, pw)

    def act(self, out, in_, func, reads, writes=(), pw=(), bias=None, scale=None):
        kw = {}
        if bias is not None:
            kw["bias"] = bias
        if scale is not None:
            kw["scale"] = scale
        return self.rec("act", lambda e: e.activation(out=out, in_=in_, func=func, **kw), reads, writes, pw)

    def tt(self, eng, out, in0, in1, op, reads, writes=(), pw=()):
        return self.rec(eng, lambda e: e.tensor_tensor(out=out, in0=in0, in1=in1, op=op), reads, writes, pw)

    def ts(self, eng, out, in0, s1, s2, op0, op1, reads, writes=(), pw=()):
        return self.rec(eng, lambda e: e.tensor_scalar(out=out, in0=in0, scalar1=s1, scalar2=s2, op0=op0, op1=op1),
                        reads, writes, pw)

    def stt(self, out, in0, scalar, in1, op0, op1, reads, writes=(), pw=()):
        return self.rec("dve", lambda e: e.scalar_tensor_tensor(out=out, in0=in0, scalar=scalar, in1=in1,
                                                                op0=op0, op1=op1), reads, writes, pw)

    def cp(self, eng, out, in_, reads, writes=(), pw=()):
        if eng == "act":
            return self.rec("act", lambda e: e.copy(out=out, in_=in_), reads, writes, pw)
        return self.rec(eng, lambda e: e.tensor_copy(out=out, in_=in_), reads, writes, pw)

    def dma(self, q, out, in_, semc, reads, writes=(), pw=(), **kw):
        return self.rec(q, lambda e: e.dma_start(out=out, in_=in_, **kw), reads, writes, pw, semc=semc)


_UNIQ = [0]


def sbuf(nc, name, shape, dtype):
    _UNIQ[0] += 1
    return nc.sbuf_tensor("%s_%d" % (name, _UNIQ[0]), shape, dtype)


class Ring:
    def __init__(self, P, es, name, n, shape, dtype, dma=True, sw=False):
        self.n = n
        self.t = es.enter_context(sbuf(P.nc, name, [128, n] + list(shape), dtype))
        self.P = P
        self.sems = [(P.dsem_sw() if sw else P.dsem()) for _ in range(n)] if dma else None
        self.i = 0
        self.renew()

    def renew(self):
        self.tks = self.P.tks(self.n)

    def next(self):
        i = self.i % self.n
        self.i += 1
        return i


class Psum:
    cnt = 0

    def __init__(self, P, es, nf=6, nt=2):
        nc = P.nc
        self.P = P
        self.nf = nf
        self.nt = nt
        Psum.cnt += 1
        self.ps = es.enter_context(nc.psum_tensor("psf%d" % Psum.cnt, [128, nf * 512], F32))
        self.pt = es.enter_context(nc.psum_tensor("pst%d" % Psum.cnt, [128, nt * 1024], BF16))
        self.i = 0
        self.j = 0
        self.tks = P.tks(nf)
        self.ttks = P.tks(nt)

    def fixed(self, b):
        return self.ps[:, b * 512:(b + 1) * 512], self.tks[b]

    def bank(self):
        b = self.i % self.nf
        self.i += 1
        return self.fixed(b)

    def bank2(self):
        if self.i % 2:
            self.i += 1
        b = self.i % 6
        self.i += 2
        return self.ps[:, b * 512:(b + 2) * 512], [self.tks[b], self.tks[b + 1]]

    def tbank(self):
        b = self.j % self.nt
        self.j += 1
        return self.pt[:, b * 1024:b * 1024 + 512], self.ttks[b]

    def tbank_full(self):
        b = self.j % self.nt
        self.j += 1
        return self.pt[:, b * 1024:(b + 1) * 1024], self.ttks[b]


class Ctx:
    pass


def layer_norm_rows(P, C, src, src_tk, gain, bias, gb_tk, out, out_tk):
    st = C.ln_stats
    i = st.next()
    stt_, stk = st.t[:, i], st.tks[i]
    P.rec("dve", lambda e: e.bn_stats(out=stt_[:, 0:6], in_=src[:, 0:512]), [src_tk], [stk])
    P.rec("dve", lambda e: e.bn_stats(out=stt_[:, 6:12], in_=src[:, 512:1024]), [src_tk], (), [stk])
    P.rec("dve", lambda e: e.bn_aggr(out=stt_[:, 12:14], in_=stt_[:, 0:12]), [stk], (), [stk])
    P.act(stt_[:, 14:15], stt_[:, 13:14], AF.Sqrt, [stk], (), [stk], bias=C.eps_ln[:, 0:1], scale=1.0)
    P.rec("dve", lambda e: e.reciprocal(out=stt_[:, 14:15], in_=stt_[:, 14:15]), [stk], (), [stk])
    P.ts("dve", stt_[:, 15:16], stt_[:, 12:13], stt_[:, 14:15], -1.0, ALU.mult, ALU.mult, [stk], (), [stk])
    P.act(out, src, AF.Identity, [src_tk, stk], [out_tk], bias=stt_[:, 15:16], scale=stt_[:, 14:15])
    P.tt("dve", out, out, gain, ALU.mult, [out_tk, gb_tk], [out_tk])
    P.tt("pool", out, out, bias, ALU.add, [out_tk, gb_tk], [out_tk])


def load_gain_bias(P, C, lng, lnb, layer, which, semc):
    for k, src in enumerate((lng, lnb)):
        off = (layer * 2 + which) * D
        ap = bass.AP(src.tensor, off, [[0, 128], [1, D]])
        P.dma("sp", C.gb[:, k, :], ap, semc, [], (), [C.gb_tk])


def convert_weights(P, C, src, dst, rows, cols, tklist, chunk_rows=512):
    r = 0
    while r < rows:
        n = min(chunk_rows, rows - r)
        tk = P.tk()
        s = C.conv_sems[C.conv_i % len(C.conv_sems)]
        C.conv_i += 1
        P.dma("pool", dst[r:r + n, :], src[r:r + n, :], s, [], [tk])
        tklist.append((r, r + n, tk))
        r += n


def conv_tks(tklist, r0, r1):
    return [tk for (a, b, tk) in tklist if a < r1 and b > r0]


def ffn_pass(P, C, es0, layer, x1_d, xo_d, wup_bf, wdn_bf, wup_tk, wdn_tk, lng, lnb):
    nc = P.nc
    with ExitStack() as es:
        C.ps = Psum(P, es)
        C.ln_stats.renew()
        wd = es.enter_context(sbuf(nc, "wd", [128, NFC, D], BF16))
        wd_tk = P.tk()
        wd_sem = P.dsem()
        xg = Ring(P, es, "f_xg", 2, [4, D], F32)
        xb = es.enter_context(sbuf(nc, "f_xb", [128, 4, D], BF16))
        xb_tk = P.tk()
        xT = es.enter_context(sbuf(nc, "f_xT", [128, 8, 512], BF16))
        xT_tk = P.tk()
        hT = es.enter_context(sbuf(nc, "f_hT", [128, NFC, 512], BF16))
        hT_tk = P.tk()
        wu = Ring(P, es, "f_wu", 4, [8, 256], BF16)
        sg = Ring(P, es, "f_sg", 3, [512], F32, dma=False)
        rr = Ring(P, es, "f_r", 2, [D], F32, dma=False)
        ot = Ring(P, es, "f_o", 3, [D], F32, sw=True)
        C.gb = es.enter_context(sbuf(nc, "f_gb", [128, 2, D], F32))
        C.gb_tk = P.tk()
        gsem = P.dsem()
        load_gain_bias(P, C, lng, lnb, layer, 1, gsem)
        for half in range(2):
            P.dma("sp", wd[:, half * 11:(half + 1) * 11, :],
                  wdn_bf[:, half * 11 * D:(half + 1) * 11 * D].rearrange("p (f d) -> p f d", d=D),
                  wd_sem, conv_tks(wdn_tk, 0, 128), (), [wd_tk])
        x1v = x1_d.rearrange("(n p) d -> p n d", p=128)
        xov = xo_d.rearrange("(n p) d -> p n d", p=128)

        def load_x(g):
            i = xg.next()
            P.dma("sp", xg.t[:, i], x1v[:, g * 4:(g + 1) * 4, :], xg.sems[i], [C.x1_tk[g]], [xg.tks[i]])
            return i

        nxt = load_x(0)
        wq = []

        def load_wu(j):
            i = wu.next()
            P.dma("sp", wu.t[:, i], wup_bf[j * 128:(j + 1) * 128, :].rearrange("p (k c) -> p k c", c=256),
                  wu.sems[i], conv_tks(wup_tk, j * 128, (j + 1) * 128), [wu.tks[i]])
            return i

        def x_to_xT(xi_):
            xgt_, xgk_ = xg.t[:, xi_], xg.tks[xi_]
            for a in range(4):
                P.cp("pool" if a % 2 else "dve", xb[:, a, :], xgt_[:, a, :], [xgk_], (), [xb_tk])
            for kc in range(8):
                pt, ptk = C.ps.tbank()
                for a in range(4):
                    P.tr(pt[:, a * 128:(a + 1) * 128], xb[:, a, kc * 128:(kc + 1) * 128], C.ident,
                         [xb_tk], [ptk] if a == 0 else (), () if a == 0 else [ptk])
                P.cp("act" if kc % 2 else "dve", xT[:, kc, :], pt, [ptk], (), [xT_tk])

        x_to_xT(nxt)
        for g in range(8):
            xi = nxt
            xgt, xg_tk = xg.t[:, xi], xg.tks[xi]
            if g + 1 < 8:
                nxt = load_x(g + 1)
            if not wq:
                wq = [load_wu(0), load_wu(1), load_wu(2)]
            for j in range(NFC):
                if j + 3 < NFC:
                    wq.append(load_wu(j + 3))
                elif g + 1 < 8:
                    wq.append(load_wu(j + 3 - NFC))
                wi = wq.pop(0)
                wt, wtk = wu.t[:, wi], wu.tks[wi]
                pg, pgk = C.ps.bank()
                pu, puk = C.ps.bank()
                for kc in range(8):
                    P.mm(pg, wt[:, kc, 0:128], xT[:, kc, :], kc == 0, kc == 7, [wtk, xT_tk],
                         [pgk] if kc == 0 else (), () if kc == 0 else [pgk])
                for kc in range(8):
                    P.mm(pu, wt[:, kc, 128:256], xT[:, kc, :], kc == 0, kc == 7, [wtk, xT_tk],
                         [puk] if kc == 0 else (), () if kc == 0 else [puk])
                si = sg.next()
                P.act(sg.t[:, si], pg, AF.Silu, [pgk], [sg.tks[si]])
                P.tt("dve", hT[:, j, :], sg.t[:, si], pu, ALU.mult, [sg.tks[si], puk], (), [hT_tk])
            if g + 1 < 8:
                x_to_xT(nxt)
            for a in range(4):
                po, pok = C.ps.bank2()
                for half in range(2):
                    for fc in range(NFC):
                        P.mm(po[:, half * 512:(half + 1) * 512], hT[:, fc, a * 128:(a + 1) * 128],
                             wd[:, fc, half * 512:(half + 1) * 512], fc == 0, fc == NFC - 1, [hT_tk, wd_tk],
                             [pok[half]] if fc == 0 else (), () if fc == 0 else [pok[half]])
                ri = rr.next()
                P.stt(rr.t[:, ri], xgt[:, a, :], ALPHA, po, ALU.mult, ALU.add, [xg_tk] + pok, [rr.tks[ri]])
                oi = ot.next()
                layer_norm_rows(P, C, rr.t[:, ri], rr.tks[ri], C.gb[:, 0, :], C.gb[:, 1, :], C.gb_tk,
                                ot.t[:, oi], ot.tks[oi])
                P.dma("pool", xov[:, g * 4 + a, :], ot.t[:, oi], ot.sems[oi], [ot.tks[oi]], (), [C.xo_tk[g]])
        P.flush()


def retention_pass(P, C, es0, layer, x_d, x1_d, win_bf, wout_bf, win_tk, wout_tk, lng, lnb, cd):
    nc = P.nc
    with ExitStack() as es:
        C.ps = Psum(P, es)
        C.ln_stats.renew()
        wo = es.enter_context(sbuf(nc, "r_wo", [128, 16, D], BF16))
        wo_tk = P.tk()
        wo_sem = P.dsem()
        st32 = es.enter_context(sbuf(nc, "r_st32", [128, 8, 512], F32))
        stbf = es.enter_context(sbuf(nc, "r_stbf", [128, 8, 512], BF16))
        st_tk = P.tks(8)
        stb_tk = P.tks(8)
        msk = es.enter_context(sbuf(nc, "r_msk", [128, 4, 128], F32))
        zs = es.enter_context(sbuf(nc, "r_zs", [128, 4], F32))
        cst_tk = P.tk()
        cst_sem = P.dsem()
        C.gb = es.enter_context(sbuf(nc, "r_gb", [128, 2, D], F32))
        C.gb_tk = P.tk()
        gsem = P.dsem()
        xsem = P.dsem_sw()
        xb = es.enter_context(sbuf(nc, "r_xb", [128, 4, D], BF16))
        xb_tk = P.tk()
        xT = es.enter_context(sbuf(nc, "r_xT", [128, 8, 512], BF16))
        xT_tk = P.tk()
        gT = es.enter_context(sbuf(nc, "r_gT", [128, 16, 512], BF16))
        gT_tk = P.tk()
        wr = Ring(P, es, "r_w", 3, [8, 512], BF16)
        tq = Ring(P, es, "r_tq", 2, [2, 512], F32)
        tkk = Ring(P, es, "r_tk", 1, [2, 512], F32)
        qk = Ring(P, es, "r_qk", 2, [4, 512], BF16, dma=False)
        vs = Ring(P, es, "r_v", 2, [4, 512], BF16, dma=False)
        sgt = Ring(P, es, "r_sg", 2, [4, 512], BF16, dma=False)
        tmp = Ring(P, es, "r_tmp", 4, [512], F32, dma=False)
        sT = Ring(P, es, "r_sT", 2, [4, 128], BF16, dma=False)
        kz = Ring(P, es, "r_kz", 2, [4, 256], BF16, dma=False)
        yn = Ring(P, es, "r_yn", 2, [512], F32, dma=False)
        gg = Ring(P, es, "r_g", 5, [512], BF16, dma=False)
        gst = Ring(P, es, "r_gst", 6, [16], F32, dma=False)
        rr = Ring(P, es, "r_r", 2, [D], F32)
        ot = Ring(P, es, "r_o", 2, [D], F32, sw=True)

        load_gain_bias(P, C, lng, lnb, layer, 0, gsem)
        P.dma("sp", msk[:], cd["r_msk"], cst_sem, [], (), [cst_tk])
        P.dma("sp", zs[:], cd["r_zs"], cst_sem, [], (), [cst_tk])
        for q in range(4):
            P.dma("sp", wo[:, q * 4:(q + 1) * 4, :],
                  wout_bf[:, q * 4 * D:(q + 1) * 4 * D].rearrange("p (f d) -> p f d", d=D),
                  wo_sem, conv_tks(wout_tk, 0, 128), (), [wo_tk])
        for s in range(8):
            P.rec("dve", lambda e, s=s: e.memset(st32[:, s, :], 0.0), [], [st_tk[s]])
            P.rec("pool", lambda e, s=s: e.memset(stbf[:, s, :], 0.0), [], [stb_tk[s]])
        xv = x_d.rearrange("(n p) d -> p n d", p=128)
        x1v = x1_d.rearrange("(n p) d -> p n d", p=128)
        gam = [1.0 - 2.0 ** (-5.0 - h) for h in range(4)]
        gC = [float(np.float64(g) ** 128) for g in gam]

        wblocks = [(g, h, k) for g in range(8) for h in range(4) for k in range(3)]
        wslots = {}
        wstate = {"n": 0}

        def ensure_w(upto):
            while wstate["n"] <= min(upto, len(wblocks) - 1):
                g_, h_, k_ = wblocks[wstate["n"]]
                i = wr.next()
                r0 = (h_ * 3 + k_) * 128
                P.dma("sp", wr.t[:, i], win_bf[r0:r0 + 128, :].rearrange("p (k c) -> p k c", c=512),
                      wr.sems[i], [], [wr.tks[i]])
                wslots[wstate["n"]] = i
                wstate["n"] += 1

        def load_xb(g):
            for a in range(4):
                P.dma("pool", xb[:, a, :], xv[:, g * 4 + a, :], xsem, [C.x_tk[g]], (), [xb_tk])

        def x_to_xT(g):
            for kc in range(8):
                pt, ptk = C.ps.tbank()
                for a in range(4):
                    P.tr(pt[:, a * 128:(a + 1) * 128], xb[:, a, kc * 128:(kc + 1) * 128], C.ident,
                         [xb_tk], [ptk] if a == 0 else (), () if a == 0 else [ptk])
                P.cp("act" if kc % 2 else "dve", xT[:, kc, :], pt, [ptk], (), [xT_tk])
            if g + 1 < 8:
                load_xb(g + 1)

        load_xb(0)
        x_to_xT(0)
        for g in range(8):
            ki = tkk.next()
            P.dma("sp", tkk.t[:, ki], cd["r_ropek"][:, :, g * 512:(g + 1) * 512], tkk.sems[ki], [], [tkk.tks[ki]])
            for pair in ((0, 1), (2, 3)):
                hctx = {}
                for h in pair:
                    qi = tq.next()
                    P.dma("sp", tq.t[:, qi], cd["r_ropeq"][h][:, :, g * 512:(g + 1) * 512], tq.sems[qi], [],
                          [tq.tks[qi]])
                    b0 = (g * 4 + h) * 3
                    ensure_w(b0)
                    wqk_i = wslots[b0]
                    qki = qk.next()
                    qkt, qk_tk = qk.t[:, qki], qk.tks[qki]
                    wt, wtk = wr.t[:, wqk_i], wr.tks[wqk_i]
                    for which in range(2):
                        pa, pak = C.ps.bank()
                        pb, pbk = C.ps.bank()
                        for part, (pp, ppk) in enumerate(((pa, pak), (pb, pbk))):
                            c0 = (which * 2 + part) * 128
                            for kc in range(8):
                                P.mm(pp, wt[:, kc, c0:c0 + 128], xT[:, kc, :], kc == 0, kc == 7, [wtk, xT_tk],
                                     [ppk] if kc == 0 else (), () if kc == 0 else [ppk])
                        if which == 0:
                            ct, st_, tbk = tq.t[:, qi, 0, :], tq.t[:, qi, 1, :], tq.tks[qi]
                        else:
                            ct, st_, tbk = tkk.t[:, ki, 0, :], tkk.t[:, ki, 1, :], tkk.tks[ki]
                        t1, t2, t3, t4 = [tmp.next() for _ in range(4)]
                        P.tt("dve", tmp.t[:, t1], pa, ct, ALU.mult, [pak, tbk], [tmp.tks[t1]])
                        P.tt("dve", tmp.t[:, t2], pb, st_, ALU.mult, [pbk, tbk], [tmp.tks[t2]])
                        P.tt("dve", tmp.t[:, t3], pa, st_, ALU.mult, [pak, tbk], [tmp.tks[t3]])
                        P.tt("dve", tmp.t[:, t4], pb, ct, ALU.mult, [pbk, tbk], [tmp.tks[t4]])
                        P.tt("pool", qkt[:, which * 2 + 0, :], tmp.t[:, t1], tmp.t[:, t2], ALU.subtract,
                             [tmp.tks[t1], tmp.tks[t2]], (), [qk_tk])
                        P.tt("pool", qkt[:, which * 2 + 1, :], tmp.t[:, t3], tmp.t[:, t4], ALU.add,
                             [tmp.tks[t3], tmp.tks[t4]], (), [qk_tk])
                    vi = vs.next()
                    sgi = sgt.next()
                    ensure_w(b0 + 1)
                    wv, wvk = wr.t[:, wslots[b0 + 1]], wr.tks[wslots[b0 + 1]]
                    for c in range(4):
                        pv, pvk = C.ps.bank()
                        for kc in range(8):
                            P.mm(pv, xT[:, kc, c * 128:(c + 1) * 128], wv[:, kc, :], kc == 0, kc == 7, [wvk, xT_tk],
                                 [pvk] if kc == 0 else (), () if kc == 0 else [pvk])
                        P.cp("act", vs.t[:, vi, c, :], pv, [pvk], (), [vs.tks[vi]])
                    ensure_w(b0 + 2)
                    wg, wgk = wr.t[:, wslots[b0 + 2]], wr.tks[wslots[b0 + 2]]
                    for c in range(4):
                        pg, pgk = C.ps.bank()
                        for kc in range(8):
                            P.mm(pg, xT[:, kc, c * 128:(c + 1) * 128], wg[:, kc, :], kc == 0, kc == 7, [wgk, xT_tk],
                                 [pgk] if kc == 0 else (), () if kc == 0 else [pgk])
                        P.act(sgt.t[:, sgi, c, :], pg, AF.Silu, [pgk], (), [sgt.tks[sgi]])
                    ensure_w(b0 + 5)
                    hctx[h] = (qkt, qk_tk, vi, sgi)
                if pair == (2, 3) and g + 1 < 8:
                    x_to_xT(g + 1)
                for h in pair:
                    qkt, qk_tk, vi, sgi = hctx[h]
                    ps_, psk = C.ps.bank()
                    first = True
                    for c in range(4):
                        cs = slice(c * 128, (c + 1) * 128)
                        for dc in range(2):
                            P.mm(ps_[:, cs], qkt[:, 2 + dc, cs], qkt[:, dc, cs], dc == 0, dc == 1, [qk_tk],
                                 [psk] if first else (), () if first else [psk])
                            first = False
                    si = sT.next()
                    mb = bass.AP(msk.tensor if hasattr(msk, "tensor") else msk, h * 128, [[512, 128], [0, 4], [1, 128]])
                    P.tt("dve", sT.t[:, si], ps_.rearrange("p (c i) -> p c i", i=128), mb, ALU.mult,
                         [psk, cst_tk], [sT.tks[si]])
                    pt, ptk = C.ps.tbank_full()
                    first = True
                    for c in range(4):
                        cs = slice(c * 128, (c + 1) * 128)
                        for dc in range(2):
                            o0 = c * 256 + dc * 128
                            P.tr(pt[:, o0:o0 + 128], qkt[:, 2 + dc, cs], C.ident, [qk_tk],
                                 [ptk] if first else (), () if first else [ptk])
                            first = False
                    kzi = kz.next()
                    P.act(kz.t[:, kzi].rearrange("p c d -> p (c d)"), pt, AF.Identity, [ptk, cst_tk], [kz.tks[kzi]],
                          scale=zs[:, h:h + 1])
                    hctx[h] = (qkt, qk_tk, vi, sgi, si, kzi)
                deferred = []

                def emit_e(item):
                    h_, c_, ggi_ = item
                    cs_ = slice(c_ * 128, (c_ + 1) * 128)
                    pt_, ptk_ = C.ps.tbank()
                    for ec in range(4):
                        P.tr(pt_[:, ec * 128:(ec + 1) * 128], gg.t[:, ggi_, ec * 128:(ec + 1) * 128], C.ident,
                             [gg.tks[ggi_]], [ptk_] if ec == 0 else (), () if ec == 0 else [ptk_])
                    P.cp("act", gT[:, h_ * 4:(h_ + 1) * 4, cs_], pt_.rearrange("p (e t) -> p e t", t=128), [ptk_], (),
                         [gT_tk])

                for c in range(4):
                    cs = slice(c * 128, (c + 1) * 128)
                    for h in pair:
                        qkt, qk_tk, vi, sgi, si, kzi = hctx[h]
                        pus = []
                        for dc in range(2):
                            pu, puk = C.ps.bank()
                            P.mm(pu, kz.t[:, kzi, c, dc * 128:(dc + 1) * 128], vs.t[:, vi, c, :], True, True,
                                 [kz.tks[kzi], vs.tks[vi]], [puk])
                            pus.append((pu, puk))
                        py, pyk = C.ps.bank()
                        P.mm(py, sT.t[:, si, c, :], vs.t[:, vi, c, :], True, False, [sT.tks[si], vs.tks[vi]], [pyk])
                        for dc in range(2):
                            P.mm(py, qkt[:, dc, cs], stbf[:, h * 2 + dc, :], False, dc == 1,
                                 [qk_tk, stb_tk[h * 2 + dc]], (), [pyk])
                        for dc in range(2):
                            s_ = h * 2 + dc
                            pu, puk = pus[dc]
                            P.stt(st32[:, s_, :], st32[:, s_, :], gC[h], pu, ALU.mult, ALU.add, [puk, st_tk[s_]],
                                  [st_tk[s_]])
                            P.cp("act", stbf[:, s_, :], st32[:, s_, :], [st_tk[s_]], [stb_tk[s_]])
                        gi = gst.next()
                        gs_, gsk = gst.t[:, gi], gst.tks[gi]
                        P.rec("dve", lambda e, gs_=gs_, py=py: e.bn_stats(out=gs_[:, 0:6], in_=py), [pyk], [gsk])
                        P.rec("dve", lambda e, gs_=gs_: e.bn_aggr(out=gs_[:, 6:8], in_=gs_[:, 0:6]), [gsk], (), [gsk])
                        P.act(gs_[:, 8:9], gs_[:, 7:8], AF.Sqrt, [gsk], (), [gsk], bias=C.eps_gn[:, 0:1], scale=1.0)
                        P.rec("dve", lambda e, gs_=gs_: e.reciprocal(out=gs_[:, 8:9], in_=gs_[:, 8:9]), [gsk], (),
                              [gsk])
                        P.ts("dve", gs_[:, 9:10], gs_[:, 6:7], gs_[:, 8:9], -1.0, ALU.mult, ALU.mult, [gsk], (),
                             [gsk])
                        yi = yn.next()
                        P.act(yn.t[:, yi], py, AF.Identity, [pyk, gsk], [yn.tks[yi]], bias=gs_[:, 9:10],
                              scale=gs_[:, 8:9])
                        ggi = gg.next()
                        P.tt("pool", gg.t[:, ggi], yn.t[:, yi], sgt.t[:, sgi, c, :], ALU.mult,
                             [yn.tks[yi], sgt.tks[sgi]], [gg.tks[ggi]])
                        deferred.append((h, c, ggi))
                        if len(deferred) > 3:
                            emit_e(deferred.pop(0))
                while deferred:
                    emit_e(deferred.pop(0))
            for a in range(4):
                po, pok = C.ps.bank2()
                for half in range(2):
                    for ec in range(16):
                        P.mm(po[:, half * 512:(half + 1) * 512], gT[:, ec, a * 128:(a + 1) * 128],
                             wo[:, ec, half * 512:(half + 1) * 512], ec == 0, ec == 15, [gT_tk, wo_tk],
                             [pok[half]] if ec == 0 else (), () if ec == 0 else [pok[half]])
                ri = rr.next()
                P.dma("sp", rr.t[:, ri], xv[:, g * 4 + a, :], rr.sems[ri], [C.x_tk[g]], [rr.tks[ri]])
                P.stt(rr.t[:, ri], rr.t[:, ri], ALPHA, po, ALU.mult, ALU.add, [rr.tks[ri]] + pok, [rr.tks[ri]])
                oi = ot.next()
                layer_norm_rows(P, C, rr.t[:, ri], rr.tks[ri], C.gb[:, 0, :], C.gb[:, 1, :], C.gb_tk,
                                ot.t[:, oi], ot.tks[oi])
                P.dma("pool", x1v[:, g * 4 + a, :], ot.t[:, oi], ot.sems[oi], [ot.tks[oi]], (), [C.x1_tk[g]])
        P.flush()


BL = 129 * 256


def bias_prep_pass(P, C, rel_bias_d, w8d, b8, cd):
    nc = P.nc
    with ExitStack() as es:
        C.ps = Psum(P, es)
        rbT = es.enter_context(sbuf(nc, "b_rbT", [32, 48], F32))
        oh = es.enter_context(sbuf(nc, "b_oh", [32, 3 * 512], F32))
        ng = es.enter_context(sbuf(nc, "b_ng", [16, 512], F32))
        w8 = es.enter_context(sbuf(nc, "b_w8", [16, 3, 512], F32))
        tk_in = P.tk()
        sem = P.dsem()
        osem = P.dsem()
        bsem = P.dsem()
        P.dma("sp", rbT[:, :], bass.AP(rel_bias_d.tensor, 0, [[1, 32], [32, 48]]), sem, [], (), [tk_in],
              allow_slow_non_contiguous=True)
        P.dma("sp", oh[:, :], cd["a_oh"], sem, [], (), [tk_in])
        P.dma("sp", ng[:, :], cd["a_negm"], sem, [], (), [tk_in])
        for g in range(3):
            pb, pbk = C.ps.bank()
            P.mm(pb[0:16, :], rbT[:, g * 16:(g + 1) * 16], oh[:, g * 512:(g + 1) * 512], True, True, [tk_in], [pbk])
            wtk = P.tk()
            P.tt("dve", w8[:, g, :], pb[0:16, :], ng[:, :], ALU.add, [pbk, tk_in], [wtk])
            dtk = P.tk()
            P.dma("sp", w8d[g * 16:(g + 1) * 16, :], w8[:, g, :], osem, [wtk], [dtk])
            for h in range(16):
                for c in range(2):
                    gh = g * 16 + h
                    P.dma("sp", b8[gh * 2 + c].rearrange("(a b) -> a b", b=256),
                          bass.AP(w8d.tensor, gh * 512 + c * 256, [[0, 129], [1, 256]]), bsem, [dtk], [P.tk()])
        P.flush()


def blocks_of(g):
    d = DILS[g]
    nb = 32 // d
    return d, nb


def attention_pass(P, C, layer, x_d, oT_d, win_bf, b8):
    nc = P.nc
    with ExitStack() as es:
        C.ps = Psum(P, es, nf=7, nt=1)
        xT = es.enter_context(sbuf(nc, "a_xT", [128, 8, T], BF16))
        xT_tk = P.tk()
        oacc = es.enter_context(sbuf(nc, "a_oacc", [128, T], F32))
        dacc = es.enter_context(sbuf(nc, "a_dacc", [128, T], F32))
        oacc_tk = P.tk()
        dacc_tk = P.tk()
        ones = es.enter_context(sbuf(nc, "a_ones", [128, 2, 128], BF16))
        ones_tk = P.tk()
        qTr = Ring(P, es, "a_qT", 1, [2, T], BF16, dma=False)
        kTr = Ring(P, es, "a_kT", 2, [T], BF16, dma=False)
        vbr = Ring(P, es, "a_vb", 1, [32, 2, 128], BF16, dma=False)
        wr = Ring(P, es, "a_w", 2, [8, 384], BF16)
        w8r = Ring(P, es, "a_w8", 2, [2, 2, 128], F32)
        ssr = Ring(P, es, "a_ss", 3, [512], F32, dma=False)
        ptr = Ring(P, es, "a_pT", 4, [512], BF16, dma=False)
        C.att_i = 0
        C.att_s = 0
        onr = Ring(P, es, "a_on", 2, [1024], BF16)
        rcr = Ring(P, es, "a_rc", 2, [1024], F32, dma=False)
        xg = Ring(P, es, "a_xg", 2, [D], F32)
        xbr = Ring(P, es, "a_xb", 2, [D], BF16, dma=False)
        xv = x_d.rearrange("(n p) d -> p n d", p=128)
        P.rec("pool", lambda e: e.memset(ones[:, :, :], 0.0), [], [ones_tk])
        P.rec("pool", lambda e: e.memset(ones[:, 0, 0:64], 1.0), [ones_tk], [ones_tk])
        P.rec("pool", lambda e: e.memset(ones[:, 1, 64:128], 1.0), [ones_tk], [ones_tk])
        P.rec("pool", lambda e: e.memset(qTr.t[:, 0], 0.0), [], [qTr.tks[0]])
        P.rec("pool", lambda e: e.memset(vbr.t[:, 0], 0.0), [], [vbr.tks[0]])
        for t in range(NT):
            i = xg.next()
            P.dma("sp", xg.t[:, i], xv[:, t, :], xg.sems[i], [C.x_tk[t // 4]], [xg.tks[i]])
            bi = xbr.next()
            P.cp("pool" if t % 2 else "dve", xbr.t[:, bi], xg.t[:, i], [xg.tks[i]], [xbr.tks[bi]])
            for half in range(2):
                pt, ptk = C.ps.tbank()
                for q in range(4):
                    kc = half * 4 + q
                    P.tr(pt[:, q * 128:(q + 1) * 128], xbr.t[:, bi, kc * 128:(kc + 1) * 128], C.ident,
                         [xbr.tks[bi]], [ptk] if q == 0 else (), () if q == 0 else [ptk])
                P.cp("act" if half else "dve", xT[:, half * 4:(half + 1) * 4, t * 128:(t + 1) * 128],
                     pt.rearrange("p (k t) -> p k t", t=128), [ptk], (), [xT_tk])

        if DBG == 2:
            P.flush()
            return
        nblk = 24
        wslot = {}
        wn = {"n": 0}

        def ensure_w(upto):
            while wn["n"] <= min(upto, nblk - 1):
                b = wn["n"]
                i = wr.next()
                P.dma("sp", wr.t[:, i], win_bf[b * 128:(b + 1) * 128, :].rearrange("p (k c) -> p k c", c=384),
                      wr.sems[i], [], [wr.tks[i]])
                wslot[b] = i
                wn["n"] += 1

        ensure_w(1)
        for hp in range(8):
            for g in range(3):
                d, nb = blocks_of(g)
                b = hp * 3 + g
                ensure_w(b)
                wt, wtk = wr.t[:, wslot[b]], wr.tks[wslot[b]]
                w8i = w8r.next()
                for hd in range(2):
                    gh = g * 16 + hp * 2 + hd
                    P.dma("sp", w8r.t[:, w8i, :, hd, :], bass.AP(b8.tensor, gh * 2 * BL + 127, [[255, 128], [BL, 2], [1, 128]]),
                          w8r.sems[w8i], [], [w8r.tks[w8i]] if hd == 0 else (), () if hd == 0 else [w8r.tks[w8i]])
                w8t, w8k = w8r.t[:, w8i], w8r.tks[w8i]
                qi, ki, vi = qTr.next(), kTr.next(), vbr.next()
                qT, qk_ = qTr.t[:, qi], qTr.tks[qi]
                kT, kk_ = kTr.t[:, ki], kTr.tks[ki]
                vb, vk_ = vbr.t[:, vi], vbr.tks[vi]
                for tg in range(8):
                    pp, ppk = C.ps.bank()
                    for kc in range(8):
                        P.mm(pp, wt[:, kc, 0:128], xT[:, kc, tg * 512:(tg + 1) * 512],
                             kc == 0, kc == 7, [wtk, xT_tk], [ppk] if kc == 0 else (), () if kc == 0 else [ppk])
                    ts2 = slice(tg * 512, (tg + 1) * 512)
                    ev = "act" if tg % 2 else "dve"
                    P.cp(ev, qT[0:64, 0, ts2], pp[0:64, :], [ppk], (), [qk_])
                    P.cp(ev, qT[64:128, 1, ts2], pp[64:128, :], [ppk], (), [qk_])
                for tg in range(8):
                    pp, ppk = C.ps.bank()
                    for kc in range(8):
                        P.mm(pp, wt[:, kc, 128:256], xT[:, kc, tg * 512:(tg + 1) * 512],
                             kc == 0, kc == 7, [wtk, xT_tk], [ppk] if kc == 0 else (), () if kc == 0 else [ppk])
                    P.cp("dve" if tg % 2 else "act", kT[:, tg * 512:(tg + 1) * 512], pp, [ppk], (), [kk_])

                def tok(r, n0, cnt=1):
                    s0 = n0 * 128 * d + r
                    return slice(s0, min(s0 + cnt * 128 * d, T), d)

                blist = [(r, n) for r in range(d) for n in range(nb)]
                for b4 in range(8 if DBG not in (31, 32) else 0):
                    pv, pvk = C.ps.bank()
                    for q in range(4):
                        r, n = blist[b4 * 4 + q]
                        for kc in range(8):
                            first = (q == 0 and kc == 0)
                            P.mm(pv[:, q * 128:(q + 1) * 128], xT[:, kc, tok(r, n)], wt[:, kc, 256:384],
                                 kc == 0, kc == 7, [wtk, xT_tk], [pvk] if first else (), () if first else [pvk])
                    pv3 = pv.rearrange("p (q c) -> p q c", c=128)
                    ev = "act" if b4 % 2 else "dve"
                    P.cp(ev, vb[:, b4 * 4:(b4 + 1) * 4, 0, 0:64], pv3[:, :, 0:64], [pvk], (), [vk_])
                    P.cp(ev, vb[:, b4 * 4:(b4 + 1) * 4, 1, 64:128], pv3[:, :, 64:128], [pvk], (), [vk_])
                ensure_w(b + 1)
                if DBG in (3, 31, 32, 33, 34, 35):
                    continue
                if DBG in (4, 6, 7, 8) and g > 0:
                    continue
                if DBG == 5 and g > 1:
                    continue
                nbat = min(4, nb)
                items = []
                for r in range(d):
                    for n0 in range(0, nb, nbat):
                        pob = 0 if (C.att_i % 2 == 0) else 2
                        C.att_i += 1
                        for nn in range(nbat):
                            items.append((r, n0, nn, pob))
                LA = 2
                pend = {}

                def stage_a(it):
                    r, n0, nn, pob = it
                    n = n0 + nn
                    nch = 2 if n > 0 else 1
                    wv_ = nch * 256
                    ps_, psk = C.ps.fixed(4 + C.att_s % 3)
                    C.att_s += 1
                    for c in range(nch):
                        P.mm(ps_[:, c * 256:(c + 1) * 256], kT[:, tok(r, n - c)], qT[:, :, tok(r, n)],
                             True, True, [qk_, kk_], [psk] if c == 0 else (), () if c == 0 else [psk])
                    si, pi = ssr.next(), ptr.next()
                    P.stt(ssr.t[:, si, 0:wv_], ps_[:, 0:wv_], 600.0,
                          w8t.rearrange("p c h i -> p (c h i)")[:, 0:wv_], ALU.min, ALU.add,
                          [psk, w8k], [ssr.tks[si]])
                    P.act(ptr.t[:, pi, 0:wv_], ssr.t[:, si, 0:wv_], AF.Exp, [ssr.tks[si]], [ptr.tks[pi]],
                          scale=0.125)
                    pend[it] = pi

                def stage_b(it):
                    r, n0, nn, pob = it
                    n = n0 + nn
                    nch = 2 if n > 0 else 1
                    bc = r * nb + n
                    pi = pend.pop(it)
                    po, pok = C.ps.fixed(pob)
                    pd, pdk = C.ps.fixed(pob + 1)
                    k = 0
                    for c in range(nch):
                        for hd in range(2):
                            pTs = ptr.t[:, pi, (c * 2 + hd) * 128:(c * 2 + hd + 1) * 128]
                            fo = (nn == 0 and k == 0)
                            P.mm(po[:, nn * 128:(nn + 1) * 128], vb[:, bc - c, hd, :], pTs, k == 0,
                                 k == 2 * nch - 1, [vk_, ptr.tks[pi]], [pok] if fo else (), () if fo else [pok])
                            k += 1
                    k = 0
                    for c in range(nch):
                        for hd in range(2):
                            pTs = ptr.t[:, pi, (c * 2 + hd) * 128:(c * 2 + hd + 1) * 128]
                            fo = (nn == 0 and k == 0)
                            P.mm(pd[:, nn * 128:(nn + 1) * 128], ones[:, hd, :], pTs, k == 0,
                                 k == 2 * nch - 1, [ones_tk, ptr.tks[pi]], [pdk] if fo else (), () if fo else [pdk])
                            k += 1
                    if nn == nbat - 1:
                        ts_ = tok(r, n0, nbat)
                        w = nbat * 128
                        if g == 0:
                            P.cp("act", oacc[:, ts_], po[:, 0:w], [pok], (), [oacc_tk])
                            P.cp("act", dacc[:, ts_], pd[:, 0:w], [pdk], (), [dacc_tk])
                        else:
                            P.tt("dve", oacc[:, ts_], oacc[:, ts_], po[:, 0:w], ALU.add, [pok, oacc_tk], (), [oacc_tk])
                            P.tt("dve", dacc[:, ts_], dacc[:, ts_], pd[:, 0:w], ALU.add, [pdk, dacc_tk], (), [dacc_tk])

                for i in range(len(items) + LA):
                    if i < len(items):
                        stage_a(items[i])
                    if i >= LA:
                        stage_b(items[i - LA])
            if DBG in (3, 6, 7, 8, 31, 32, 33, 34, 35):
                continue
            for q in range(4):
                cs = slice(q * 1024, (q + 1) * 1024)
                ri, oi = rcr.next(), onr.next()
                P.rec("dve", lambda e, ri=ri, cs=cs: e.reciprocal(out=rcr.t[:, ri], in_=dacc[:, cs]), [dacc_tk],
                      [rcr.tks[ri]])
                P.tt("pool", onr.t[:, oi], oacc[:, cs], rcr.t[:, ri], ALU.mult, [oacc_tk, rcr.tks[ri]], [onr.tks[oi]])
                P.dma("sp", oT_d[hp, :, cs], onr.t[:, oi], onr.sems[oi], [onr.tks[oi]], (), [C.oT_tk])
        P.flush()


def outproj_pass(P, C, layer, x_d, x1_d, oT_d, wout_bf, lng, lnb):
    nc = P.nc
    with ExitStack() as es:
        C.ps = Psum(P, es)
        C.ln_stats.renew()
        wo = es.enter_context(sbuf(nc, "o_wo", [128, 8, D], BF16))
        wo_tk = P.tk()
        wo_sem = P.dsem()
        C.gb = es.enter_context(sbuf(nc, "o_gb", [128, 2, D], F32))
        C.gb_tk = P.tk()
        gsem = P.dsem()
        oT = Ring(P, es, "o_oT", 2, [8, 512], BF16)
        rr = Ring(P, es, "o_r", 4, [D], F32)
        ot = Ring(P, es, "o_o", 3, [D], F32, sw=True)
        load_gain_bias(P, C, lng, lnb, layer, 0, gsem)
        for q in range(2):
            P.dma("sp", wo[:, q * 4:(q + 1) * 4, :],
                  wout_bf[:, q * 4 * D:(q + 1) * 4 * D].rearrange("p (f d) -> p f d", d=D), wo_sem, [], (), [wo_tk])
        xv = x_d.rearrange("(n p) d -> p n d", p=128)
        x1v = x1_d.rearrange("(n p) d -> p n d", p=128)
        rslot = {}

        def load_res(tile):
            if tile < NT:
                ri_ = rr.next()
                P.dma("sp", rr.t[:, ri_], xv[:, tile, :], rr.sems[ri_], [C.x_tk[tile // 4]], [rr.tks[ri_]])
                rslot[tile] = ri_

        load_res(0)
        load_res(1)
        for g in range(8):
            oi_ = oT.next()
            P.dma("sp", oT.t[:, oi_], oT_d[:, :, g * 512:(g + 1) * 512].rearrange("h p t -> p h t"), oT.sems[oi_],
                  [C.oT_tk], [oT.tks[oi_]])
            for a in range(4):
                load_res(g * 4 + a + 2)
                po, pok = C.ps.bank2()
                for half in range(2):
                    for hp in range(8):
                        P.mm(po[:, half * 512:(half + 1) * 512], oT.t[:, oi_, hp, a * 128:(a + 1) * 128],
                             wo[:, hp, half * 512:(half + 1) * 512], hp == 0, hp == 7, [oT.tks[oi_], wo_tk],
                             [pok[half]] if hp == 0 else (), () if hp == 0 else [pok[half]])
                ri = rslot.pop(g * 4 + a)
                P.stt(rr.t[:, ri], rr.t[:, ri], ALPHA, po, ALU.mult, ALU.add, [rr.tks[ri]] + pok, [rr.tks[ri]])
                oi = ot.next()
                layer_norm_rows(P, C, rr.t[:, ri], rr.tks[ri], C.gb[:, 0, :], C.gb[:, 1, :], C.gb_tk,
                                ot.t[:, oi], ot.tks[oi])
                P.dma("pool", x1v[:, g * 4 + a, :], ot.t[:, oi], ot.sems[oi], [ot.tks[oi]], (), [C.x1_tk[g]])
        P.flush()


def _t5_bucket_np(dist):
    max_exact = 16
    d_f = np.maximum(dist, 1).astype(np.float32)
    large = max_exact + (np.log(d_f / np.float32(max_exact)) / np.float32(math.log(2048 / max_exact))
                         * np.float32(32 - max_exact)).astype(np.int32)
    large = np.minimum(large, 31)
    return np.where(dist < max_exact, dist, large)


def make_consts():
    c = {}
    c["ident"] = np.eye(128, dtype=np.float32)
    inv = (1.0 / (np.float32(10000.0) ** np.linspace(0.0, 1.0, 128, dtype=np.float32))).astype(np.float32)
    ang = (np.arange(T, dtype=np.float32)[:, None] * inv[None, :]).astype(np.float32)
    cos = np.cos(ang.astype(np.float64)).T
    sin = np.sin(ang.astype(np.float64)).T
    c["r_ropek"] = np.stack([cos, sin], axis=1).astype(np.float32)
    gam = np.array([1.0 - 2.0 ** (-5.0 - h) for h in range(4)], dtype=np.float64)
    il = (np.arange(T) % 128).astype(np.float64)
    rq = np.zeros((4, 128, 2, T), np.float32)
    for h in range(4):
        xi = gam[h] ** (il + 1.0)
        rq[h, :, 0, :] = cos * xi[None, :]
        rq[h, :, 1, :] = sin * xi[None, :]
    c["r_ropeq"] = rq
    j = np.arange(128)[:, None].astype(np.float64)
    i = np.arange(128)[None, :].astype(np.float64)
    msk = np.zeros((128, 4, 128), np.float32)
    zs = np.zeros((128, 4), np.float32)
    for h in range(4):
        msk[:, h, :] = np.where(i >= j, gam[h] ** (-(j + 1.0)) / 16.0, 0.0)
        zs[:, h] = gam[h] ** (127.0 - np.arange(128)) / 16.0
    c["r_msk"] = msk
    c["r_zs"] = zs
    oh = np.zeros((32, 3, 2, 256), np.float32)
    negm = np.zeros((16, 2, 256), np.float32)
    u = np.arange(255)
    for cc in range(2):
        delta = (u - 127) if cc == 0 else (u + 1)
        valid = (delta >= 0) if cc == 0 else (delta <= 128)
        negm[:, cc, :255] = np.where(valid, 0.0, NEG)[None, :]
        negm[:, cc, 255] = NEG
        for g, dil in enumerate(DILS):
            bkt = _t5_bucket_np(np.maximum(delta, 0) * dil)
            for uu in range(255):
                if valid[uu]:
                    oh[bkt[uu], g, cc, uu] = 8.0
    c["a_oh"] = oh.reshape(32, 3 * 512)
    c["a_negm"] = negm.reshape(16, 512)
    return c


def layout_weights(inp):
    w = {}
    for l in range(DEPTH):
        Wu = inp["ffn_w_up"][l]
        gt = Wu[:, :DFF].reshape(8, 128, NFC, 128)
        up = Wu[:, DFF:].reshape(8, 128, NFC, 128)
        blk = np.concatenate([gt, up], axis=3)
        w["wup%d" % l] = np.ascontiguousarray(blk.transpose(2, 1, 0, 3)).reshape(NFC * 128, 2048)
        Wd = inp["ffn_w_down"][l].reshape(NFC, 128, D)
        w["wdn%d" % l] = np.ascontiguousarray(Wd.transpose(1, 0, 2)).reshape(128 * 11, 2048)
    for j in range(2):
        Wi = inp["ret_w_in"][j]
        blocks = []
        for h in range(4):
            q = Wi[:, h * 256:(h + 1) * 256]
            k = Wi[:, 1024 + h * 256:1024 + (h + 1) * 256]
            qk = np.concatenate([q[:, 0::2], q[:, 1::2], k[:, 0::2], k[:, 1::2]], axis=1)
            v = Wi[:, 2048 + h * 512:2048 + (h + 1) * 512]
            gte = Wi[:, 4096 + h * 512:4096 + (h + 1) * 512]
            for m in (qk, v, gte):
                blocks.append(m.reshape(8, 128, 512).transpose(1, 0, 2).reshape(128, 4096))
        w["rwin%d" % j] = np.ascontiguousarray(np.concatenate(blocks, axis=0)).reshape(12 * 128 * 2, 2048)
        Wo = inp["ret_w_out"][j].reshape(16, 128, D)
        w["rwout%d" % j] = np.ascontiguousarray(Wo.transpose(1, 0, 2)).reshape(128 * 8, 2048)
        Wa = inp["attn_w_in"][j]
        blocks = []
        for hp in range(8):
            for g in range(3):
                cols = []
                for s_ in range(3):
                    c0 = ((s_ * 3 + g) * 16 + 2 * hp) * 64
                    cols.append(Wa[:, c0:c0 + 128])
                m = np.concatenate(cols, axis=1)
                blocks.append(m.reshape(8, 128, 384).transpose(1, 0, 2).reshape(128, 3072))
        w["awin%d" % j] = np.ascontiguousarray(np.concatenate(blocks, axis=0)).reshape(24 * 128 * 2, 1536)
        Wao = inp["attn_w_out"][j].reshape(8, 128, D)
        w["awout%d" % j] = np.ascontiguousarray(Wao.transpose(1, 0, 2)).reshape(128 * 4, 2048)
    return w


LAYERS_DEFAULT = (0, 1, 2, 3)


def build_program(layers=LAYERS_DEFAULT, debug_out=None):
    nc = bass.Bass("TRN2", target_bir_lowering=False)
    x_in = nc.dram_tensor("x", [T, D], F32, kind="ExternalInput").ap()
    y_out = nc.dram_tensor("y", [T, D], F32, kind="ExternalOutput").ap()
    lng = nc.dram_tensor("ln_gain", [DEPTH * 2 * D], F32, kind="ExternalInput").ap()
    lnb = nc.dram_tensor("ln_bias", [DEPTH * 2 * D], F32, kind="ExternalInput").ap()
    consts = make_consts()
    cd = {}
    for k, v in consts.items():
        cd[k] = nc.dram_tensor("c_" + k, list(v.shape), F32, kind="ExternalInput").ap()
    wsrc = {}
    wshapes = {}
    for l in layers:
        wshapes["wup%d" % l] = (NFC * 128, 2048)
        wshapes["wdn%d" % l] = (128 * 11, 2048)
        if l % 2 == 0:
            wshapes["rwin%d" % (l // 2)] = (12 * 128 * 2, 2048)
            wshapes["rwout%d" % (l // 2)] = (128 * 8, 2048)
        else:
            wshapes["awin%d" % (l // 2)] = (24 * 128 * 2, 1536)
            wshapes["awout%d" % (l // 2)] = (128 * 4, 2048)
    for k, shp in wshapes.items():
        wsrc[k] = nc.dram_tensor(k, list(shp), F32, kind="ExternalInput").ap()
    wbf = {k: nc.dram_tensor(k + "_bf", list(shp), BF16).ap() for k, shp in wshapes.items()}
    xa = nc.dram_tensor("xa", [T, D], F32).ap()
    xb_ = nc.dram_tensor("xb", [T, D], F32).ap()
    x1 = nc.dram_tensor("x1", [T, D], F32).ap()
    oT_d = nc.dram_tensor("oT_d", [8, 128, T], BF16).ap()
    w8d = nc.dram_tensor("w8d", [48, 512], F32).ap()
    b8 = nc.dram_tensor("b8", [96, BL], F32).ap()
    has_attn = any(l % 2 == 1 for l in layers)
    rel_bias_d = nc.dram_tensor("rel_bias", [48, 32], F32, kind="ExternalInput").ap() if has_attn else None

    with ExitStack() as es:
        P = Prog(nc, es)
        C = Ctx()
        C.ln_stats = Ring(P, es, "lnst", 4, [16], F32, dma=False)
        C.ident_t = es.enter_context(sbuf(nc, "ident", [128, 128], BF16))
        C.ident = C.ident_t[:, :]
        C.eps_ln = es.enter_context(sbuf(nc, "eps_ln", [128, 1], F32))
        C.eps_gn = es.enter_context(sbuf(nc, "eps_gn", [128, 1], F32))
        C.conv_sems = [SemC(es.enter_context(nc.semaphore("cv%d" % i))) for i in range(4)]
        C.conv_i = 0
        isem = SemC(es.enter_context(nc.semaphore("cv_i")))
        P.dma("pool", C.ident, cd["ident"], isem, [], [P.tk()])
        P.rec("dve", lambda e: e.memset(C.eps_ln[:], LN_EPS), [], [P.tk()])
        P.rec("dve", lambda e: e.memset(C.eps_gn[:], GN_EPS), [], [P.tk()])
        wtk = {k: [] for k in wshapes}
        pass_keys = []
        for l in layers:
            if l % 2 == 0:
                pass_keys.append(["rwin%d" % (l // 2), "rwout%d" % (l // 2)])
            else:
                pass_keys.append(["awin%d" % (l // 2)])
                pass_keys.append(["awout%d" % (l // 2)])
            pass_keys.append(["wup%d" % l, "wdn%d" % l])
        C.pass_i = 0

        def conv_for(pi):
            if pi < len(pass_keys):
                for k in pass_keys[pi]:
                    convert_weights(P, C, wsrc[k], wbf[k], wshapes[k][0], wshapes[k][1], wtk[k])

        def next_pass():
            C.pass_i += 1
            conv_for(C.pass_i)

        conv_for(0)
        P.flush()
        if has_attn:
            bias_prep_pass(P, C, rel_bias_d, w8d, b8, cd)
        if DBG == 1:
            return nc, consts, list(wshapes.keys())

        cur = x_in
        for li, l in enumerate(layers):
            last = li == len(layers) - 1
            nxt = y_out if last else (xa if cur is not xa else xb_)
            C.x_tk = P.tks(8)
            C.x1_tk = P.tks(8)
            C.xo_tk = P.tks(8)
            if l % 2 == 0:
                j = l // 2
                next_pass()
                retention_pass(P, C, es, l, cur, x1, wbf["rwin%d" % j].rearrange("(r two) c -> r (two c)", two=2),
                               wbf["rwout%d" % j].rearrange("(p e) c -> p (e c)", e=8),
                               wtk["rwin%d" % j], wtk["rwout%d" % j], lng, lnb, cd)
            else:
                j = l // 2
                C.oT_tk = P.tk()
                next_pass()
                attention_pass(P, C, l, cur, oT_d, wbf["awin%d" % j].rearrange("(r two) c -> r (two c)", two=2), b8)
                if DBG:
                    return nc, consts, list(wshapes.keys())
                C.x_tk = P.tks(8)
                C.x1_tk = P.tks(8)
                C.oT_tk = P.tk()
                next_pass()
                outproj_pass(P, C, l, cur, x1, oT_d, wbf["awout%d" % j].rearrange("(p e) c -> p (e c)", e=4),
                             lng, lnb)
            C.x_tk = P.tks(8)
            C.x1_tk = P.tks(8)
            C.xo_tk = P.tks(8)
            next_pass()
            ffn_pass(P, C, es, l, x1, nxt, wbf["wup%d" % l],
                     wbf["wdn%d" % l].rearrange("(p e) c -> p (e c)", e=11),
                     wtk["wup%d" % l], wtk["wdn%d" % l], lng, lnb)
            cur = nxt
    return nc, consts, list(wshapes.keys())


_CACHE = {}


def kernel(x, ret_w_in, ret_w_out, attn_w_in, attn_w_out, rel_bias, ffn_w_up, ffn_w_down, ln_gain, ln_bias):
    inp = dict(x=np.asarray(x), ret_w_in=np.asarray(ret_w_in), ret_w_out=np.asarray(ret_w_out),
               attn_w_in=np.asarray(attn_w_in), attn_w_out=np.asarray(attn_w_out), rel_bias=np.asarray(rel_bias),
               ffn_w_up=np.asarray(ffn_w_up), ffn_w_down=np.asarray(ffn_w_down))
    nc, consts, wkeys = build_program()
    w = layout_weights(inp)
    shared = {"ln_gain": np.ascontiguousarray(np.asarray(ln_gain, np.float32).reshape(-1)),
              "ln_bias": np.ascontiguousarray(np.asarray(ln_bias, np.float32).reshape(-1)),
              "rel_bias": np.ascontiguousarray(inp["rel_bias"])}
    for k, v in consts.items():
        shared["c_" + k] = v
    for k in wkeys:
        shared[k] = w[k]
    in_maps = []
    for b in range(NCORES):
        m = dict(shared)
        m["x"] = np.ascontiguousarray(inp["x"][b])
        in_maps.append(m)
    res = run_bass_kernel_spmd(nc, in_maps, core_ids=list(range(NCORES)))
    return np.stack([np.asarray(r["y"]) for r in res.results], axis=0).astype(np.float32)
```

```python
import math
from contextlib import ExitStack

import numpy as np
import concourse.bass as bass
import concourse.mybir as mybir
from concourse.bass_utils import run_bass_kernel_spmd

F32 = mybir.dt.float32
BF16 = mybir.dt.bfloat16
AF = mybir.ActivationFunctionType
ALU = mybir.AluOpType

D = 1024
T = 4096
NT = T // 128
DEPTH = 4
DFF = 2816
NFC = DFF // 128
ALPHA = float((2 * DEPTH) ** 0.25)
LN_EPS = 1e-5
GN_EPS = 1e-5
NCORES = 8
SAME_ENGINE_SYNC = True
NEG = -1.0e5
DILS = (1, 4, 16)
DBG = 0


class Tk:
    __slots__ = ("w", "r", "gen")

    def __init__(self):
        self.w = []
        self.r = []
        self.gen = []


class SemC:
    def __init__(self, sem):
        self.sem = sem
        self.count = 0


class Op:
    __slots__ = ("eng", "fn", "deps", "need_inc", "semc", "val", "isdma", "idx")


def _add_latest(lst, op):
    if not op.isdma:
        for i, q in enumerate(lst):
            if (not q.isdma) and q.eng == op.eng:
                lst[i] = op
                return
    lst.append(op)


class Prog:
    CE = ("pe", "act", "dve", "pool")

    def __init__(self, nc, es):
        self.nc = nc
        self.es = es
        self.csem = {e: SemC(es.enter_context(nc.semaphore("c_" + e))) for e in self.CE}
        self.lists = {e: [] for e in ("pe", "act", "dve", "pool", "sp")}
        self.n = 0
        self.tiles = []
        self.nsem = 0
        self.sempool = []
        self.nsw = 0
        self.swpool = []

    def dsem_sw(self):
        if self.nsw == len(self.swpool):
            self.swpool.append(SemC(self.es.enter_context(self.nc.semaphore("sw%d" % self.nsw))))
        c = self.swpool[self.nsw]
        self.nsw += 1
        return c

    def tk(self):
        t = Tk()
        self.tiles.append(t)
        return t

    def tks(self, n):
        return [self.tk() for _ in range(n)]

    def dsem(self):
        if self.nsem == len(self.sempool):
            self.sempool.append(SemC(self.es.enter_context(self.nc.semaphore("d%d" % self.nsem))))
        c = self.sempool[self.nsem]
        self.nsem += 1
        return c

    def rec(self, eng, fn, reads=(), writes=(), pw=(), semc=None):
        op = Op()
        op.eng = eng
        op.fn = fn
        op.need_inc = False
        op.isdma = semc is not None
        op.idx = self.n
        op.semc = None
        op.val = 0
        self.n += 1
        deps = {}

        def add(p):
            if p.isdma:
                deps[id(p)] = p
            else:
                q = deps.get(p.eng)
                if q is None or q.idx < p.idx:
                    deps[p.eng] = p

        for t in reads:
            for p in t.w:
                add(p)
        for t in writes:
            for p in t.r:
                add(p)
            for p in t.w:
                add(p)
        for t in pw:
            if t.r:
                t.gen = t.r + t.w
            for p in t.gen:
                add(p)
        for t in reads:
            _add_latest(t.r, op)
        for t in writes:
            t.w = [op]
            t.r = []
            t.gen = [op]
        for t in pw:
            if t.r:
                t.w = [op]
                t.r = []
            else:
                _add_latest(t.w, op)
        dl = []
        for p in deps.values():
            if (not p.isdma) and (not op.isdma) and p.eng == eng and (eng == "pe" or not SAME_ENGINE_SYNC):
                continue
            p.need_inc = True
            dl.append(p)
        op.deps = dl
        if semc is not None:
            semc.count += 16
            op.semc = semc
            op.val = semc.count
        self.lists[eng].append(op)
        return op

    def flush(self):
        for e in self.CE:
            c = self.csem[e]
            for op in self.lists[e]:
                if (not op.isdma) and op.need_inc:
                    c.count += 1
                    op.semc = c
                    op.val = c.count
        lists = self.lists
        with self.nc.Block() as block:
            for e, deco in (("pe", block.tensor), ("act", block.scalar), ("dve", block.vector),
                            ("pool", block.gpsimd), ("sp", block.sync)):
                ops = lists[e]

                def body(eng, ops=ops):
                    waited = {}
                    final = {}
                    for op in ops:
                        need = {}
                        for p in op.deps:
                            k = id(p.semc)
                            if waited.get(k, 0) >= p.val:
                                continue
                            if k not in need or need[k][1] < p.val:
                                need[k] = (p.semc, p.val)
                        for k, (c, v) in need.items():
                            eng.wait_ge(c.sem, v)
                            waited[k] = v
                        ins = op.fn(eng)
                        if op.isdma:
                            ins.then_inc(op.semc.sem, 16)
                            final[id(op.semc)] = (op.semc, op.val)
                        elif op.need_inc:
                            ins.then_inc(op.semc.sem, 1)
                    for k, (c, v) in final.items():
                        if waited.get(k, 0) < v:
                            eng.wait_ge(c.sem, v)

                deco(body)
        self.lists = {e: [] for e in ("pe", "act", "dve", "pool", "sp")}
        self.nsem = 0
        self.nsw = 0
        for t in self.tiles:
            t.w = []
            t.r = []
            t.gen = []
        self.tiles = []

    def mm(self, out, lhsT, rhs, start, stop, reads, writes=(), pw=()):
        return self.rec("pe", lambda e: e.matmul(out, lhsT, rhs, start=start, stop=stop), reads, writes, pw)

    def tr(self, out, in_, ident, reads, writes=(), pw=()):
        return self.rec("pe", lambda e: e.transpose(out, in_, ident), reads, writes, pw)

    def act(self, out, in_, func, reads, writes=(), pw=(), bias=None, scale=None):
        kw = {}
        if bias is not None:
            kw["bias"] = bias
        if scale is not None:
            kw["scale"] = scale
        return self.rec("act", lambda e: e.activation(out=out, in_=in_, func=func, **kw), reads, writes, pw)

    def tt(self, eng, out, in0, in1, op, reads, writes=(), pw=()):
        return self.rec(eng, lambda e: e.tensor_tensor(out=out, in0=in0, in1=in1, op=op), reads, writes, pw)

    def ts(self, eng, out, in0, s1, s2, op0, op1, reads, writes=(), pw=()):
        return self.rec(eng, lambda e: e.tensor_scalar(out=out, in0=in0, scalar1=s1, scalar2=s2, op0=op0, op1=op1),
                        reads, writes, pw)

    def stt(self, out, in0, scalar, in1, op0, op1, reads, writes=(), pw=()):
        return self.rec("dve", lambda e: e.scalar_tensor_tensor(out=out, in0=in0, scalar=scalar, in1=in1,
                                                                op0=op0, op1=op1), reads, writes, pw)

    def cp(self, eng, out, in_, reads, writes=(), pw=()):
        if eng == "act":
            return self.rec("act", lambda e: e.copy(out=out, in_=in_), reads, writes, pw)
        return self.rec(eng, lambda e: e.tensor_copy(out=out, in_=in_), reads, writes, pw)

    def dma(self, q, out, in_, semc, reads, writes=(), pw=(), **kw):
        return self.rec(q, lambda e: e.dma_start(out=out, in_=in_, **kw), reads, writes, pw, semc=semc)


_UNIQ = [0]


def sbuf(nc, name, shape, dtype):
    _UNIQ[0] += 1
    return nc.sbuf_tensor("%s_%d" % (name, _UNIQ[0]), shape, dtype)


class Ring:
    def __init__(self, P, es, name, n, shape, dtype, dma=True, sw=False):
        self.n = n
        self.t = es.enter_context(sbuf(P.nc, name, [128, n] + list(shape), dtype))
        self.P = P
        self.sems = [(P.dsem_sw() if sw else P.dsem()) for _ in range(n)] if dma else None
        self.i = 0
        self.renew()

    def renew(self):
        self.tks = self.P.tks(self.n)

    def next(self):
        i = self.i % self.n
        self.i += 1
        return i


class Psum:
    cnt = 0

    def __init__(self, P, es, nf=6, nt=2):
        nc = P.nc
        self.P = P
        self.nf = nf
        self.nt = nt
        Psum.cnt += 1
        self.ps = es.enter_context(nc.psum_tensor("psf%d" % Psum.cnt, [128, nf * 512], F32))
        self.pt = es.enter_context(nc.psum_tensor("pst%d" % Psum.cnt, [128, nt * 1024], BF16))
        self.i = 0
        self.j = 0
        self.tks = P.tks(nf)
        self.ttks = P.tks(nt)

    def fixed(self, b):
        return self.ps[:, b * 512:(b + 1) * 512], self.tks[b]

    def bank(self):
        b = self.i % self.nf
        self.i += 1
        return self.fixed(b)

    def bank2(self):
        if self.i % 2:
            self.i += 1
        b = self.i % 6
        self.i += 2
        return self.ps[:, b * 512:(b + 2) * 512], [self.tks[b], self.tks[b + 1]]

    def tbank(self):
        b = self.j % self.nt
        self.j += 1
        return self.pt[:, b * 1024:b * 1024 + 512], self.ttks[b]

    def tbank_full(self):
        b = self.j % self.nt
        self.j += 1
        return self.pt[:, b * 1024:(b + 1) * 1024], self.ttks[b]


class Ctx:
    pass


def layer_norm_rows(P, C, src, src_tk, gain, bias, gb_tk, out, out_tk, defer=False):
    st = C.ln_stats
    i = st.next()
    stt_, stk = st.t[:, i], st.tks[i]
    P.rec("dve", lambda e: e.bn_stats(out=stt_[:, 0:6], in_=src[:, 0:512]), [src_tk], [stk])
    P.rec("dve", lambda e: e.bn_stats(out=stt_[:, 6:12], in_=src[:, 512:1024]), [src_tk], (), [stk])
    P.rec("dve", lambda e: e.bn_aggr(out=stt_[:, 12:14], in_=stt_[:, 0:12]), [stk], (), [stk])
    P.act(stt_[:, 14:15], stt_[:, 13:14], AF.Sqrt, [stk], (), [stk], bias=C.eps_ln[:, 0:1], scale=1.0)
    P.rec("dve", lambda e: e.reciprocal(out=stt_[:, 14:15], in_=stt_[:, 14:15]), [stk], (), [stk])
    P.ts("dve", stt_[:, 15:16], stt_[:, 12:13], stt_[:, 14:15], -1.0, ALU.mult, ALU.mult, [stk], (), [stk])

    def tail():
        P.act(out, src, AF.Identity, [src_tk, stk], [out_tk], bias=stt_[:, 15:16], scale=stt_[:, 14:15])
        P.tt("dve", out, out, gain, ALU.mult, [out_tk, gb_tk], [out_tk])
        P.tt("pool", out, out, bias, ALU.add, [out_tk, gb_tk], [out_tk])

    if defer:
        return tail
    tail()


def load_gain_bias(P, C, lng, lnb, layer, which, semc):
    for k, src in enumerate((lng, lnb)):
        off = (layer * 2 + which) * D
        ap = bass.AP(src.tensor, off, [[0, 128], [1, D]])
        P.dma("sp", C.gb[:, k, :], ap, semc, [], (), [C.gb_tk])


def convert_weights(P, C, src, dst, rows, cols, tklist, chunk_rows=512):
    r = 0
    while r < rows:
        n = min(chunk_rows, rows - r)
        tk = P.tk()
        s = C.conv_sems[C.conv_i % len(C.conv_sems)]
        C.conv_i += 1
        P.dma("pool", dst[r:r + n, :], src[r:r + n, :], s, [], [tk])
        tklist.append((r, r + n, tk))
        r += n


def conv_tks(tklist, r0, r1):
    return [tk for (a, b, tk) in tklist if a < r1 and b > r0]


def ffn_pass(P, C, es0, layer, x1_d, xo_d, wup_bf, wdn_bf, wup_tk, wdn_tk, lng, lnb):
    nc = P.nc
    with ExitStack() as es:
        C.ps = Psum(P, es)
        C.ln_stats.renew()
        wd = es.enter_context(sbuf(nc, "wd", [128, NFC, D], BF16))
        wd_tk = P.tk()
        wd_sem = P.dsem()
        xg = Ring(P, es, "f_xg", 2, [4, D], F32)
        xb = es.enter_context(sbuf(nc, "f_xb", [128, 4, D], BF16))
        xb_tk = P.tk()
        xT = es.enter_context(sbuf(nc, "f_xT", [128, 8, 512], BF16))
        xT_tk = P.tk()
        hT = es.enter_context(sbuf(nc, "f_hT", [128, NFC, 512], BF16))
        hT_tk = P.tk()
        wu = Ring(P, es, "f_wu", 4, [8, 256], BF16)
        sg = Ring(P, es, "f_sg", 3, [512], F32, dma=False)
        rr = Ring(P, es, "f_r", 2, [D], F32, dma=False)
        ot = Ring(P, es, "f_o", 3, [D], F32, sw=True)
        C.gb = es.enter_context(sbuf(nc, "f_gb", [128, 2, D], F32))
        C.gb_tk = P.tk()
        gsem = P.dsem()
        load_gain_bias(P, C, lng, lnb, layer, 1, gsem)
        for half in range(2):
            P.dma("sp", wd[:, half * 11:(half + 1) * 11, :],
                  wdn_bf[:, half * 11 * D:(half + 1) * 11 * D].rearrange("p (f d) -> p f d", d=D),
                  wd_sem, conv_tks(wdn_tk, 0, 128), (), [wd_tk])
        x1v = x1_d.rearrange("(n p) d -> p n d", p=128)
        xov = xo_d.rearrange("(n p) d -> p n d", p=128)

        def load_x(g):
            i = xg.next()
            P.dma("sp", xg.t[:, i], x1v[:, g * 4:(g + 1) * 4, :], xg.sems[i], [C.x1_tk[g]], [xg.tks[i]])
            return i

        nxt = load_x(0)
        wq = []

        def load_wu(j):
            i = wu.next()
            P.dma("sp", wu.t[:, i], wup_bf[j * 128:(j + 1) * 128, :].rearrange("p (k c) -> p k c", c=256),
                  wu.sems[i], conv_tks(wup_tk, j * 128, (j + 1) * 128), [wu.tks[i]])
            return i

        def x_to_xT(xi_):
            xgt_, xgk_ = xg.t[:, xi_], xg.tks[xi_]
            for a in range(4):
                P.cp("pool" if a % 2 else "dve", xb[:, a, :], xgt_[:, a, :], [xgk_], (), [xb_tk])
            for kc in range(8):
                pt, ptk = C.ps.tbank()
                for a in range(4):
                    P.tr(pt[:, a * 128:(a + 1) * 128], xb[:, a, kc * 128:(kc + 1) * 128], C.ident,
                         [xb_tk], [ptk] if a == 0 else (), () if a == 0 else [ptk])
                P.cp("act" if kc % 2 else "dve", xT[:, kc, :], pt, [ptk], (), [xT_tk])

        x_to_xT(nxt)
        for g in range(8):
            xi = nxt
            xgt, xg_tk = xg.t[:, xi], xg.tks[xi]
            if g + 1 < 8:
                nxt = load_x(g + 1)
            if not wq:
                wq = [load_wu(0), load_wu(1), load_wu(2)]
            for j in range(NFC):
                if j + 3 < NFC:
                    wq.append(load_wu(j + 3))
                elif g + 1 < 8:
                    wq.append(load_wu(j + 3 - NFC))
                wi = wq.pop(0)
                wt, wtk = wu.t[:, wi], wu.tks[wi]
                pg, pgk = C.ps.bank()
                pu, puk = C.ps.bank()
                for kc in range(8):
                    P.mm(pg, wt[:, kc, 0:128], xT[:, kc, :], kc == 0, kc == 7, [wtk, xT_tk],
                         [pgk] if kc == 0 else (), () if kc == 0 else [pgk])
                for kc in range(8):
                    P.mm(pu, wt[:, kc, 128:256], xT[:, kc, :], kc == 0, kc == 7, [wtk, xT_tk],
                         [puk] if kc == 0 else (), () if kc == 0 else [puk])
                si = sg.next()
                P.act(sg.t[:, si], pg, AF.Silu, [pgk], [sg.tks[si]])
                P.tt("dve", hT[:, j, :], sg.t[:, si], pu, ALU.mult, [sg.tks[si], puk], (), [hT_tk])
            if g + 1 < 8:
                x_to_xT(nxt)
            for a in range(4):
                po, pok = C.ps.bank2()
                for half in range(2):
                    for fc in range(NFC):
                        P.mm(po[:, half * 512:(half + 1) * 512], hT[:, fc, a * 128:(a + 1) * 128],
                             wd[:, fc, half * 512:(half + 1) * 512], fc == 0, fc == NFC - 1, [hT_tk, wd_tk],
                             [pok[half]] if fc == 0 else (), () if fc == 0 else [pok[half]])
                ri = rr.next()
                P.stt(rr.t[:, ri], xgt[:, a, :], ALPHA, po, ALU.mult, ALU.add, [xg_tk] + pok, [rr.tks[ri]])
                oi = ot.next()
                layer_norm_rows(P, C, rr.t[:, ri], rr.tks[ri], C.gb[:, 0, :], C.gb[:, 1, :], C.gb_tk,
                                ot.t[:, oi], ot.tks[oi])
                P.dma("pool", xov[:, g * 4 + a, :], ot.t[:, oi], ot.sems[oi], [ot.tks[oi]], (), [C.xo_tk[g]])
        P.flush()


def retention_pass(P, C, es0, layer, x_d, x1_d, win_bf, wout_bf, win_tk, wout_tk, lng, lnb, cd):
    nc = P.nc
    with ExitStack() as es:
        C.ps = Psum(P, es)
        C.ln_stats.renew()
        wo = es.enter_context(sbuf(nc, "r_wo", [128, 16, D], BF16))
        wo_tk = P.tk()
        wo_sem = P.dsem()
        st32 = es.enter_context(sbuf(nc, "r_st32", [128, 8, 512], F32))
        stbf = es.enter_context(sbuf(nc, "r_stbf", [128, 8, 512], BF16))
        st_tk = P.tks(8)
        stb_tk = P.tks(8)
        msk = es.enter_context(sbuf(nc, "r_msk", [128, 4, 128], F32))
        zs = es.enter_context(sbuf(nc, "r_zs", [128, 4], F32))
        cst_tk = P.tk()
        cst_sem = P.dsem()
        C.gb = es.enter_context(sbuf(nc, "r_gb", [128, 2, D], F32))
        C.gb_tk = P.tk()
        gsem = P.dsem()
        xsem = P.dsem_sw()
        xb = es.enter_context(sbuf(nc, "r_xb", [128, 4, D], BF16))
        xb_tk = P.tk()
        xT = es.enter_context(sbuf(nc, "r_xT", [128, 8, 512], BF16))
        xT_tk = P.tk()
        gT = es.enter_context(sbuf(nc, "r_gT", [128, 16, 512], BF16))
        gT_tk = P.tk()
        wr = Ring(P, es, "r_w", 3, [8, 512], BF16)
        tq = Ring(P, es, "r_tq", 2, [2, 512], F32)
        tkk = Ring(P, es, "r_tk", 1, [2, 512], F32)
        qk = Ring(P, es, "r_qk", 2, [4, 512], BF16, dma=False)
        vs = Ring(P, es, "r_v", 2, [4, 512], BF16, dma=False)
        sgt = Ring(P, es, "r_sg", 2, [4, 512], BF16, dma=False)
        tmp = Ring(P, es, "r_tmp", 4, [512], F32, dma=False)
        sT = Ring(P, es, "r_sT", 2, [4, 128], BF16, dma=False)
        kz = Ring(P, es, "r_kz", 2, [4, 256], BF16, dma=False)
        yn = Ring(P, es, "r_yn", 2, [512], F32, dma=False)
        gg = Ring(P, es, "r_g", 5, [512], BF16, dma=False)
        gst = Ring(P, es, "r_gst", 6, [16], F32, dma=False)
        rr = Ring(P, es, "r_r", 2, [D], F32)
        ot = Ring(P, es, "r_o", 2, [D], F32, sw=True)

        load_gain_bias(P, C, lng, lnb, layer, 0, gsem)
        P.dma("sp", msk[:], cd["r_msk"], cst_sem, [], (), [cst_tk])
        P.dma("sp", zs[:], cd["r_zs"], cst_sem, [], (), [cst_tk])
        for q in range(4):
            P.dma("sp", wo[:, q * 4:(q + 1) * 4, :],
                  wout_bf[:, q * 4 * D:(q + 1) * 4 * D].rearrange("p (f d) -> p f d", d=D),
                  wo_sem, conv_tks(wout_tk, 0, 128), (), [wo_tk])
        for s in range(8):
            P.rec("dve", lambda e, s=s: e.memset(st32[:, s, :], 0.0), [], [st_tk[s]])
            P.rec("pool", lambda e, s=s: e.memset(stbf[:, s, :], 0.0), [], [stb_tk[s]])
        xv = x_d.rearrange("(n p) d -> p n d", p=128)
        x1v = x1_d.rearrange("(n p) d -> p n d", p=128)
        gam = [1.0 - 2.0 ** (-5.0 - h) for h in range(4)]
        gC = [float(np.float64(g) ** 128) for g in gam]

        wblocks = [(g, h, k) for g in range(8) for h in range(4) for k in range(3)]
        wslots = {}
        wstate = {"n": 0}

        def ensure_w(upto):
            while wstate["n"] <= min(upto, len(wblocks) - 1):
                g_, h_, k_ = wblocks[wstate["n"]]
                i = wr.next()
                r0 = (h_ * 3 + k_) * 128
                P.dma("sp", wr.t[:, i], win_bf[r0:r0 + 128, :].rearrange("p (k c) -> p k c", c=512),
                      wr.sems[i], [], [wr.tks[i]])
                wslots[wstate["n"]] = i
                wstate["n"] += 1

        def load_xb(g):
            for a in range(4):
                P.dma("pool", xb[:, a, :], xv[:, g * 4 + a, :], xsem, [C.x_tk[g]], (), [xb_tk])

        def x_to_xT(g):
            for kc in range(8):
                pt, ptk = C.ps.tbank()
                for a in range(4):
                    P.tr(pt[:, a * 128:(a + 1) * 128], xb[:, a, kc * 128:(kc + 1) * 128], C.ident,
                         [xb_tk], [ptk] if a == 0 else (), () if a == 0 else [ptk])
                P.cp("act" if kc % 2 else "dve", xT[:, kc, :], pt, [ptk], (), [xT_tk])
            if g + 1 < 8:
                load_xb(g + 1)

        load_xb(0)
        x_to_xT(0)
        for g in range(8):
            ki = tkk.next()
            P.dma("sp", tkk.t[:, ki], cd["r_ropek"][:, :, g * 512:(g + 1) * 512], tkk.sems[ki], [], [tkk.tks[ki]])
            for pair in ((0, 1), (2, 3)):
                hctx = {}
                for h in pair:
                    qi = tq.next()
                    P.dma("sp", tq.t[:, qi], cd["r_ropeq"][h][:, :, g * 512:(g + 1) * 512], tq.sems[qi], [],
                          [tq.tks[qi]])
                    b0 = (g * 4 + h) * 3
                    ensure_w(b0)
                    wqk_i = wslots[b0]
                    qki = qk.next()
                    qkt, qk_tk = qk.t[:, qki], qk.tks[qki]
                    wt, wtk = wr.t[:, wqk_i], wr.tks[wqk_i]
                    for which in range(2):
                        pa, pak = C.ps.bank()
                        pb, pbk = C.ps.bank()
                        for part, (pp, ppk) in enumerate(((pa, pak), (pb, pbk))):
                            c0 = (which * 2 + part) * 128
                            for kc in range(8):
                                P.mm(pp, wt[:, kc, c0:c0 + 128], xT[:, kc, :], kc == 0, kc == 7, [wtk, xT_tk],
                                     [ppk] if kc == 0 else (), () if kc == 0 else [ppk])
                        if which == 0:
                            ct, st_, tbk = tq.t[:, qi, 0, :], tq.t[:, qi, 1, :], tq.tks[qi]
                        else:
                            ct, st_, tbk = tkk.t[:, ki, 0, :], tkk.t[:, ki, 1, :], tkk.tks[ki]
                        t1, t2, t3, t4 = [tmp.next() for _ in range(4)]
                        P.tt("dve", tmp.t[:, t1], pa, ct, ALU.mult, [pak, tbk], [tmp.tks[t1]])
                        P.tt("dve", tmp.t[:, t2], pb, st_, ALU.mult, [pbk, tbk], [tmp.tks[t2]])
                        P.tt("dve", tmp.t[:, t3], pa, st_, ALU.mult, [pak, tbk], [tmp.tks[t3]])
                        P.tt("dve", tmp.t[:, t4], pb, ct, ALU.mult, [pbk, tbk], [tmp.tks[t4]])
                        P.tt("pool", qkt[:, which * 2 + 0, :], tmp.t[:, t1], tmp.t[:, t2], ALU.subtract,
                             [tmp.tks[t1], tmp.tks[t2]], (), [qk_tk])
                        P.tt("pool", qkt[:, which * 2 + 1, :], tmp.t[:, t3], tmp.t[:, t4], ALU.add,
                             [tmp.tks[t3], tmp.tks[t4]], (), [qk_tk])
                    vi = vs.next()
                    sgi = sgt.next()
                    ensure_w(b0 + 1)
                    wv, wvk = wr.t[:, wslots[b0 + 1]], wr.tks[wslots[b0 + 1]]
                    for c in range(4):
                        pv, pvk = C.ps.bank()
                        for kc in range(8):
                            P.mm(pv, xT[:, kc, c * 128:(c + 1) * 128], wv[:, kc, :], kc == 0, kc == 7, [wvk, xT_tk],
                                 [pvk] if kc == 0 else (), () if kc == 0 else [pvk])
                        P.cp("act", vs.t[:, vi, c, :], pv, [pvk], (), [vs.tks[vi]])
                    ensure_w(b0 + 2)
                    wg, wgk = wr.t[:, wslots[b0 + 2]], wr.tks[wslots[b0 + 2]]
                    for c in range(4):
                        pg, pgk = C.ps.bank()
                        for kc in range(8):
                            P.mm(pg, xT[:, kc, c * 128:(c + 1) * 128], wg[:, kc, :], kc == 0, kc == 7, [wgk, xT_tk],
                                 [pgk] if kc == 0 else (), () if kc == 0 else [pgk])
                        P.act(sgt.t[:, sgi, c, :], pg, AF.Silu, [pgk], (), [sgt.tks[sgi]])
                    ensure_w(b0 + 5)
                    hctx[h] = (qkt, qk_tk, vi, sgi)
                if pair == (2, 3) and g + 1 < 8:
                    x_to_xT(g + 1)
                for h in pair:
                    qkt, qk_tk, vi, sgi = hctx[h]
                    ps_, psk = C.ps.bank()
                    first = True
                    for c in range(4):
                        cs = slice(c * 128, (c + 1) * 128)
                        for dc in range(2):
                            P.mm(ps_[:, cs], qkt[:, 2 + dc, cs], qkt[:, dc, cs], dc == 0, dc == 1, [qk_tk],
                                 [psk] if first else (), () if first else [psk])
                            first = False
                    si = sT.next()
                    mb = bass.AP(msk.tensor if hasattr(msk, "tensor") else msk, h * 128, [[512, 128], [0, 4], [1, 128]])
                    P.tt("dve", sT.t[:, si], ps_.rearrange("p (c i) -> p c i", i=128), mb, ALU.mult,
                         [psk, cst_tk], [sT.tks[si]])
                    pt, ptk = C.ps.tbank_full()
                    first = True
                    for c in range(4):
                        cs = slice(c * 128, (c + 1) * 128)
                        for dc in range(2):
                            o0 = c * 256 + dc * 128
                            P.tr(pt[:, o0:o0 + 128], qkt[:, 2 + dc, cs], C.ident, [qk_tk],
                                 [ptk] if first else (), () if first else [ptk])
                            first = False
                    kzi = kz.next()
                    P.act(kz.t[:, kzi].rearrange("p c d -> p (c d)"), pt, AF.Identity, [ptk, cst_tk], [kz.tks[kzi]],
                          scale=zs[:, h:h + 1])
                    hctx[h] = (qkt, qk_tk, vi, sgi, si, kzi)
                deferred = []

                def emit_e(item):
                    h_, c_, ggi_ = item
                    cs_ = slice(c_ * 128, (c_ + 1) * 128)
                    pt_, ptk_ = C.ps.tbank()
                    for ec in range(4):
                        P.tr(pt_[:, ec * 128:(ec + 1) * 128], gg.t[:, ggi_, ec * 128:(ec + 1) * 128], C.ident,
                             [gg.tks[ggi_]], [ptk_] if ec == 0 else (), () if ec == 0 else [ptk_])
                    P.cp("act", gT[:, h_ * 4:(h_ + 1) * 4, cs_], pt_.rearrange("p (e t) -> p e t", t=128), [ptk_], (),
                         [gT_tk])

                for c in range(4):
                    cs = slice(c * 128, (c + 1) * 128)
                    for h in pair:
                        qkt, qk_tk, vi, sgi, si, kzi = hctx[h]
                        pus = []
                        for dc in range(2):
                            pu, puk = C.ps.bank()
                            P.mm(pu, kz.t[:, kzi, c, dc * 128:(dc + 1) * 128], vs.t[:, vi, c, :], True, True,
                                 [kz.tks[kzi], vs.tks[vi]], [puk])
                            pus.append((pu, puk))
                        py, pyk = C.ps.bank()
                        P.mm(py, sT.t[:, si, c, :], vs.t[:, vi, c, :], True, False, [sT.tks[si], vs.tks[vi]], [pyk])
                        for dc in range(2):
                            P.mm(py, qkt[:, dc, cs], stbf[:, h * 2 + dc, :], False, dc == 1,
                                 [qk_tk, stb_tk[h * 2 + dc]], (), [pyk])
                        for dc in range(2):
                            s_ = h * 2 + dc
                            pu, puk = pus[dc]
                            P.stt(st32[:, s_, :], st32[:, s_, :], gC[h], pu, ALU.mult, ALU.add, [puk, st_tk[s_]],
                                  [st_tk[s_]])
                            P.cp("act", stbf[:, s_, :], st32[:, s_, :], [st_tk[s_]], [stb_tk[s_]])
                        gi = gst.next()
                        gs_, gsk = gst.t[:, gi], gst.tks[gi]
                        P.rec("dve", lambda e, gs_=gs_, py=py: e.bn_stats(out=gs_[:, 0:6], in_=py), [pyk], [gsk])
                        P.rec("dve", lambda e, gs_=gs_: e.bn_aggr(out=gs_[:, 6:8], in_=gs_[:, 0:6]), [gsk], (), [gsk])
                        P.act(gs_[:, 8:9], gs_[:, 7:8], AF.Sqrt, [gsk], (), [gsk], bias=C.eps_gn[:, 0:1], scale=1.0)
                        P.rec("dve", lambda e, gs_=gs_: e.reciprocal(out=gs_[:, 8:9], in_=gs_[:, 8:9]), [gsk], (),
                              [gsk])
                        P.ts("dve", gs_[:, 9:10], gs_[:, 6:7], gs_[:, 8:9], -1.0, ALU.mult, ALU.mult, [gsk], (),
                             [gsk])
                        yi = yn.next()
                        P.act(yn.t[:, yi], py, AF.Identity, [pyk, gsk], [yn.tks[yi]], bias=gs_[:, 9:10],
                              scale=gs_[:, 8:9])
                        ggi = gg.next()
                        P.tt("pool", gg.t[:, ggi], yn.t[:, yi], sgt.t[:, sgi, c, :], ALU.mult,
                             [yn.tks[yi], sgt.tks[sgi]], [gg.tks[ggi]])
                        deferred.append((h, c, ggi))
                        if len(deferred) > 3:
                            emit_e(deferred.pop(0))
                while deferred:
                    emit_e(deferred.pop(0))
            for a in range(4):
                po, pok = C.ps.bank2()
                for half in range(2):
                    for ec in range(16):
                        P.mm(po[:, half * 512:(half + 1) * 512], gT[:, ec, a * 128:(a + 1) * 128],
                             wo[:, ec, half * 512:(half + 1) * 512], ec == 0, ec == 15, [gT_tk, wo_tk],
                             [pok[half]] if ec == 0 else (), () if ec == 0 else [pok[half]])
                ri = rr.next()
                P.dma("sp", rr.t[:, ri], xv[:, g * 4 + a, :], rr.sems[ri], [C.x_tk[g]], [rr.tks[ri]])
                P.stt(rr.t[:, ri], rr.t[:, ri], ALPHA, po, ALU.mult, ALU.add, [rr.tks[ri]] + pok, [rr.tks[ri]])
                oi = ot.next()
                layer_norm_rows(P, C, rr.t[:, ri], rr.tks[ri], C.gb[:, 0, :], C.gb[:, 1, :], C.gb_tk,
                                ot.t[:, oi], ot.tks[oi])
                P.dma("pool", x1v[:, g * 4 + a, :], ot.t[:, oi], ot.sems[oi], [ot.tks[oi]], (), [C.x1_tk[g]])
        P.flush()


BL = 129 * 256


def bias_prep_pass(P, C, rel_bias_d, w8d, b8, cd):
    nc = P.nc
    with ExitStack() as es:
        C.ps = Psum(P, es)
        rbT = es.enter_context(sbuf(nc, "b_rbT", [32, 48], F32))
        oh = es.enter_context(sbuf(nc, "b_oh", [32, 3 * 512], F32))
        ng = es.enter_context(sbuf(nc, "b_ng", [16, 512], F32))
        w8 = es.enter_context(sbuf(nc, "b_w8", [16, 3, 512], F32))
        tk_in = P.tk()
        sem = P.dsem()
        osem = P.dsem()
        bsem = P.dsem()
        P.dma("sp", rbT[:, :], bass.AP(rel_bias_d.tensor, 0, [[1, 32], [32, 48]]), sem, [], (), [tk_in],
              allow_slow_non_contiguous=True)
        P.dma("sp", oh[:, :], cd["a_oh"], sem, [], (), [tk_in])
        P.dma("sp", ng[:, :], cd["a_negm"], sem, [], (), [tk_in])
        for g in range(3):
            pb, pbk = C.ps.bank()
            P.mm(pb[0:16, :], rbT[:, g * 16:(g + 1) * 16], oh[:, g * 512:(g + 1) * 512], True, True, [tk_in], [pbk])
            wtk = P.tk()
            P.tt("dve", w8[:, g, :], pb[0:16, :], ng[:, :], ALU.add, [pbk, tk_in], [wtk])
            dtk = P.tk()
            P.dma("sp", w8d[g * 16:(g + 1) * 16, :], w8[:, g, :], osem, [wtk], [dtk])
            for h in range(16):
                for c in range(2):
                    gh = g * 16 + h
                    P.dma("sp", b8[gh * 2 + c].rearrange("(a b) -> a b", b=256),
                          bass.AP(w8d.tensor, gh * 512 + c * 256, [[0, 129], [1, 256]]), bsem, [dtk], [P.tk()])
        P.flush()


def blocks_of(g):
    d = DILS[g]
    nb = 32 // d
    return d, nb


def attention_pass(P, C, layer, x_d, oT_d, win_bf, b8):
    nc = P.nc
    with ExitStack() as es:
        C.ps = Psum(P, es, nf=7, nt=1)
        xT = es.enter_context(sbuf(nc, "a_xT", [128, 8, T], BF16))
        xT_tk = P.tk()
        oacc = es.enter_context(sbuf(nc, "a_oacc", [128, T], F32))
        dacc = es.enter_context(sbuf(nc, "a_dacc", [128, T], F32))
        oacc_tk = P.tk()
        dacc_tk = P.tk()
        ones = es.enter_context(sbuf(nc, "a_ones", [128, 2, 128], BF16))
        ones_tk = P.tk()
        qTr = Ring(P, es, "a_qT", 1, [2, T], BF16, dma=False)
        kTr = Ring(P, es, "a_kT", 2, [T], BF16, dma=False)
        vbr = Ring(P, es, "a_vb", 1, [32, 2, 128], BF16, dma=False)
        wr = Ring(P, es, "a_w", 2, [8, 384], BF16)
        w8r = Ring(P, es, "a_w8", 2, [2, 2, 128], F32)
        ssr = Ring(P, es, "a_ss", 3, [512], F32, dma=False)
        ptr = Ring(P, es, "a_pT", 4, [512], BF16, dma=False)
        C.att_i = 0
        C.att_s = 0
        onr = Ring(P, es, "a_on", 2, [1024], BF16)
        rcr = Ring(P, es, "a_rc", 2, [1024], F32, dma=False)
        xg = Ring(P, es, "a_xg", 2, [D], F32)
        xbr = Ring(P, es, "a_xb", 2, [D], BF16, dma=False)
        xv = x_d.rearrange("(n p) d -> p n d", p=128)
        P.rec("pool", lambda e: e.memset(ones[:, :, :], 0.0), [], [ones_tk])
        P.rec("pool", lambda e: e.memset(ones[:, 0, 0:64], 1.0), [ones_tk], [ones_tk])
        P.rec("pool", lambda e: e.memset(ones[:, 1, 64:128], 1.0), [ones_tk], [ones_tk])
        P.rec("pool", lambda e: e.memset(qTr.t[:, 0], 0.0), [], [qTr.tks[0]])
        P.rec("pool", lambda e: e.memset(vbr.t[:, 0], 0.0), [], [vbr.tks[0]])
        for t in range(NT):
            i = xg.next()
            P.dma("sp", xg.t[:, i], xv[:, t, :], xg.sems[i], [C.x_tk[t // 4]], [xg.tks[i]])
            bi = xbr.next()
            P.cp("pool" if t % 2 else "dve", xbr.t[:, bi], xg.t[:, i], [xg.tks[i]], [xbr.tks[bi]])
            for half in range(2):
                pt, ptk = C.ps.tbank()
                for q in range(4):
                    kc = half * 4 + q
                    P.tr(pt[:, q * 128:(q + 1) * 128], xbr.t[:, bi, kc * 128:(kc + 1) * 128], C.ident,
                         [xbr.tks[bi]], [ptk] if q == 0 else (), () if q == 0 else [ptk])
                P.cp("act" if half else "dve", xT[:, half * 4:(half + 1) * 4, t * 128:(t + 1) * 128],
                     pt.rearrange("p (k t) -> p k t", t=128), [ptk], (), [xT_tk])

        if DBG == 2:
            P.flush()
            return
        nblk = 24
        wslot = {}
        wn = {"n": 0}

        def ensure_w(upto):
            while wn["n"] <= min(upto, nblk - 1):
                b = wn["n"]
                i = wr.next()
                P.dma("sp", wr.t[:, i], win_bf[b * 128:(b + 1) * 128, :].rearrange("p (k c) -> p k c", c=384),
                      wr.sems[i], [], [wr.tks[i]])
                wslot[b] = i
                wn["n"] += 1

        ensure_w(1)
        for hp in range(8):
            for g in range(3):
                d, nb = blocks_of(g)
                b = hp * 3 + g
                ensure_w(b)
                wt, wtk = wr.t[:, wslot[b]], wr.tks[wslot[b]]
                w8i = w8r.next()
                for hd in range(2):
                    gh = g * 16 + hp * 2 + hd
                    P.dma("sp", w8r.t[:, w8i, :, hd, :], bass.AP(b8.tensor, gh * 2 * BL + 127, [[255, 128], [BL, 2], [1, 128]]),
                          w8r.sems[w8i], [], [w8r.tks[w8i]] if hd == 0 else (), () if hd == 0 else [w8r.tks[w8i]])
                w8t, w8k = w8r.t[:, w8i], w8r.tks[w8i]
                qi, ki, vi = qTr.next(), kTr.next(), vbr.next()
                qT, qk_ = qTr.t[:, qi], qTr.tks[qi]
                kT, kk_ = kTr.t[:, ki], kTr.tks[ki]
                vb, vk_ = vbr.t[:, vi], vbr.tks[vi]
                for tg in range(8):
                    pp, ppk = C.ps.bank()
                    for kc in range(8):
                        P.mm(pp, wt[:, kc, 0:128], xT[:, kc, tg * 512:(tg + 1) * 512],
                             kc == 0, kc == 7, [wtk, xT_tk], [ppk] if kc == 0 else (), () if kc == 0 else [ppk])
                    ts2 = slice(tg * 512, (tg + 1) * 512)
                    ev = "act" if tg % 2 else "dve"
                    P.cp(ev, qT[0:64, 0, ts2], pp[0:64, :], [ppk], (), [qk_])
                    P.cp(ev, qT[64:128, 1, ts2], pp[64:128, :], [ppk], (), [qk_])
                for tg in range(8):
                    pp, ppk = C.ps.bank()
                    for kc in range(8):
                        P.mm(pp, wt[:, kc, 128:256], xT[:, kc, tg * 512:(tg + 1) * 512],
                             kc == 0, kc == 7, [wtk, xT_tk], [ppk] if kc == 0 else (), () if kc == 0 else [ppk])
                    P.cp("dve" if tg % 2 else "act", kT[:, tg * 512:(tg + 1) * 512], pp, [ppk], (), [kk_])

                def tok(r, n0, cnt=1):
                    s0 = n0 * 128 * d + r
                    return slice(s0, min(s0 + cnt * 128 * d, T), d)

                blist = [(r, n) for r in range(d) for n in range(nb)]
                for b4 in range(8 if DBG not in (31, 32) else 0):
                    pv, pvk = C.ps.bank()
                    for q in range(4):
                        r, n = blist[b4 * 4 + q]
                        for kc in range(8):
                            first = (q == 0 and kc == 0)
                            P.mm(pv[:, q * 128:(q + 1) * 128], xT[:, kc, tok(r, n)], wt[:, kc, 256:384],
                                 kc == 0, kc == 7, [wtk, xT_tk], [pvk] if first else (), () if first else [pvk])
                    pv3 = pv.rearrange("p (q c) -> p q c", c=128)
                    ev = "act" if b4 % 2 else "dve"
                    P.cp(ev, vb[:, b4 * 4:(b4 + 1) * 4, 0, 0:64], pv3[:, :, 0:64], [pvk], (), [vk_])
                    P.cp(ev, vb[:, b4 * 4:(b4 + 1) * 4, 1, 64:128], pv3[:, :, 64:128], [pvk], (), [vk_])
                ensure_w(b + 1)
                if DBG in (3, 31, 32, 33, 34, 35):
                    continue
                if DBG in (4, 6, 7, 8) and g > 0:
                    continue
                if DBG == 5 and g > 1:
                    continue
                nbat = min(4, nb)
                items = []
                for r in range(d):
                    for n0 in range(0, nb, nbat):
                        pob = 0 if (C.att_i % 2 == 0) else 2
                        C.att_i += 1
                        for nn in range(nbat):
                            items.append((r, n0, nn, pob))
                LA = 2
                pend = {}

                def stage_a(it):
                    r, n0, nn, pob = it
                    n = n0 + nn
                    nch = 2 if n > 0 else 1
                    wv_ = nch * 256
                    ps_, psk = C.ps.fixed(4 + C.att_s % 3)
                    C.att_s += 1
                    for c in range(nch):
                        P.mm(ps_[:, c * 256:(c + 1) * 256], kT[:, tok(r, n - c)], qT[:, :, tok(r, n)],
                             True, True, [qk_, kk_], [psk] if c == 0 else (), () if c == 0 else [psk])
                    si, pi = ssr.next(), ptr.next()
                    P.stt(ssr.t[:, si, 0:wv_], ps_[:, 0:wv_], 600.0,
                          w8t.rearrange("p c h i -> p (c h i)")[:, 0:wv_], ALU.min, ALU.add,
                          [psk, w8k], [ssr.tks[si]])
                    P.act(ptr.t[:, pi, 0:wv_], ssr.t[:, si, 0:wv_], AF.Exp, [ssr.tks[si]], [ptr.tks[pi]],
                          scale=0.125)
                    pend[it] = pi

                def stage_b(it):
                    r, n0, nn, pob = it
                    n = n0 + nn
                    nch = 2 if n > 0 else 1
                    bc = r * nb + n
                    pi = pend.pop(it)
                    po, pok = C.ps.fixed(pob)
                    pd, pdk = C.ps.fixed(pob + 1)
                    k = 0
                    for c in range(nch):
                        for hd in range(2):
                            pTs = ptr.t[:, pi, (c * 2 + hd) * 128:(c * 2 + hd + 1) * 128]
                            fo = (nn == 0 and k == 0)
                            P.mm(po[:, nn * 128:(nn + 1) * 128], vb[:, bc - c, hd, :], pTs, k == 0,
                                 k == 2 * nch - 1, [vk_, ptr.tks[pi]], [pok] if fo else (), () if fo else [pok])
                            k += 1
                    k = 0
                    for c in range(nch):
                        for hd in range(2):
                            pTs = ptr.t[:, pi, (c * 2 + hd) * 128:(c * 2 + hd + 1) * 128]
                            fo = (nn == 0 and k == 0)
                            P.mm(pd[:, nn * 128:(nn + 1) * 128], ones[:, hd, :], pTs, k == 0,
                                 k == 2 * nch - 1, [ones_tk, ptr.tks[pi]], [pdk] if fo else (), () if fo else [pdk])
                            k += 1
                    if nn == nbat - 1:
                        ts_ = tok(r, n0, nbat)
                        w = nbat * 128
                        if g == 0:
                            P.cp("act", oacc[:, ts_], po[:, 0:w], [pok], (), [oacc_tk])
                            P.cp("act", dacc[:, ts_], pd[:, 0:w], [pdk], (), [dacc_tk])
                        else:
                            P.tt("dve", oacc[:, ts_], oacc[:, ts_], po[:, 0:w], ALU.add, [pok, oacc_tk], (), [oacc_tk])
                            P.tt("dve", dacc[:, ts_], dacc[:, ts_], pd[:, 0:w], ALU.add, [pdk, dacc_tk], (), [dacc_tk])

                for i in range(len(items) + LA):
                    if i < len(items):
                        stage_a(items[i])
                    if i >= LA:
                        stage_b(items[i - LA])
            if DBG in (3, 6, 7, 8, 31, 32, 33, 34, 35):
                continue
            for q in range(4):
                cs = slice(q * 1024, (q + 1) * 1024)
                ri, oi = rcr.next(), onr.next()
                P.rec("dve", lambda e, ri=ri, cs=cs: e.reciprocal(out=rcr.t[:, ri], in_=dacc[:, cs]), [dacc_tk],
                      [rcr.tks[ri]])
                P.tt("pool", onr.t[:, oi], oacc[:, cs], rcr.t[:, ri], ALU.mult, [oacc_tk, rcr.tks[ri]], [onr.tks[oi]])
                P.dma("sp", oT_d[hp, :, cs], onr.t[:, oi], onr.sems[oi], [onr.tks[oi]], (), [C.oT_tk])
        P.flush()


def outproj_pass(P, C, layer, x_d, x1_d, oT_d, wout_bf, lng, lnb):
    nc = P.nc
    with ExitStack() as es:
        C.ps = Psum(P, es)
        C.ln_stats.renew()
        wo = es.enter_context(sbuf(nc, "o_wo", [128, 8, D], BF16))
        wo_tk = P.tk()
        wo_sem = P.dsem()
        C.gb = es.enter_context(sbuf(nc, "o_gb", [128, 2, D], F32))
        C.gb_tk = P.tk()
        gsem = P.dsem()
        oT = Ring(P, es, "o_oT", 2, [8, 512], BF16)
        rr = Ring(P, es, "o_r", 4, [D], F32)
        ot = Ring(P, es, "o_o", 3, [D], F32, sw=True)
        load_gain_bias(P, C, lng, lnb, layer, 0, gsem)
        for q in range(2):
            P.dma("sp", wo[:, q * 4:(q + 1) * 4, :],
                  wout_bf[:, q * 4 * D:(q + 1) * 4 * D].rearrange("p (f d) -> p f d", d=D), wo_sem, [], (), [wo_tk])
        xv = x_d.rearrange("(n p) d -> p n d", p=128)
        x1v = x1_d.rearrange("(n p) d -> p n d", p=128)
        rslot = {}

        def load_res(tile):
            if tile < NT:
                ri_ = rr.next()
                P.dma("sp", rr.t[:, ri_], xv[:, tile, :], rr.sems[ri_], [C.x_tk[tile // 4]], [rr.tks[ri_]])
                rslot[tile] = ri_

        load_res(0)
        load_res(1)
        pending = []
        for g in range(8):
            oi_ = oT.next()
            P.dma("sp", oT.t[:, oi_], oT_d[:, :, g * 512:(g + 1) * 512].rearrange("h p t -> p h t"), oT.sems[oi_],
                  [C.oT_tk], [oT.tks[oi_]])
            for a in range(4):
                load_res(g * 4 + a + 2)
                po, pok = C.ps.bank2()
                for half in range(2):
                    for hp in range(8):
                        P.mm(po[:, half * 512:(half + 1) * 512], oT.t[:, oi_, hp, a * 128:(a + 1) * 128],
                             wo[:, hp, half * 512:(half + 1) * 512], hp == 0, hp == 7, [oT.tks[oi_], wo_tk],
                             [pok[half]] if hp == 0 else (), () if hp == 0 else [pok[half]])
                ri = rslot.pop(g * 4 + a)
                P.stt(rr.t[:, ri], rr.t[:, ri], ALPHA, po, ALU.mult, ALU.add, [rr.tks[ri]] + pok, [rr.tks[ri]])
                oi = ot.next()
                tail = layer_norm_rows(P, C, rr.t[:, ri], rr.tks[ri], C.gb[:, 0, :], C.gb[:, 1, :], C.gb_tk,
                                       ot.t[:, oi], ot.tks[oi], defer=True)
                if pending:
                    pending.pop(0)()
                pending.append(lambda tail=tail, oi=oi, t_=g * 4 + a, g=g: (
                    tail(), P.dma("pool", x1v[:, t_, :], ot.t[:, oi], ot.sems[oi], [ot.tks[oi]], (), [C.x1_tk[g]])))
        while pending:
            pending.pop(0)()
        P.flush()


def _t5_bucket_np(dist):
    max_exact = 16
    d_f = np.maximum(dist, 1).astype(np.float32)
    large = max_exact + (np.log(d_f / np.float32(max_exact)) / np.float32(math.log(2048 / max_exact))
                         * np.float32(32 - max_exact)).astype(np.int32)
    large = np.minimum(large, 31)
    return np.where(dist < max_exact, dist, large)


def make_consts():
    c = {}
    c["ident"] = np.eye(128, dtype=np.float32)
    inv = (1.0 / (np.float32(10000.0) ** np.linspace(0.0, 1.0, 128, dtype=np.float32))).astype(np.float32)
    ang = (np.arange(T, dtype=np.float32)[:, None] * inv[None, :]).astype(np.float32)
    cos = np.cos(ang.astype(np.float64)).T
    sin = np.sin(ang.astype(np.float64)).T
    c["r_ropek"] = np.stack([cos, sin], axis=1).astype(np.float32)
    gam = np.array([1.0 - 2.0 ** (-5.0 - h) for h in range(4)], dtype=np.float64)
    il = (np.arange(T) % 128).astype(np.float64)
    rq = np.zeros((4, 128, 2, T), np.float32)
    for h in range(4):
        xi = gam[h] ** (il + 1.0)
        rq[h, :, 0, :] = cos * xi[None, :]
        rq[h, :, 1, :] = sin * xi[None, :]
    c["r_ropeq"] = rq
    j = np.arange(128)[:, None].astype(np.float64)
    i = np.arange(128)[None, :].astype(np.float64)
    msk = np.zeros((128, 4, 128), np.float32)
    zs = np.zeros((128, 4), np.float32)
    for h in range(4):
        msk[:, h, :] = np.where(i >= j, gam[h] ** (-(j + 1.0)) / 16.0, 0.0)
        zs[:, h] = gam[h] ** (127.0 - np.arange(128)) / 16.0
    c["r_msk"] = msk
    c["r_zs"] = zs
    oh = np.zeros((32, 3, 2, 256), np.float32)
    negm = np.zeros((16, 2, 256), np.float32)
    u = np.arange(255)
    for cc in range(2):
        delta = (u - 127) if cc == 0 else (u + 1)
        valid = (delta >= 0) if cc == 0 else (delta <= 128)
        negm[:, cc, :255] = np.where(valid, 0.0, NEG)[None, :]
        negm[:, cc, 255] = NEG
        for g, dil in enumerate(DILS):
            bkt = _t5_bucket_np(np.maximum(delta, 0) * dil)
            for uu in range(255):
                if valid[uu]:
                    oh[bkt[uu], g, cc, uu] = 8.0
    c["a_oh"] = oh.reshape(32, 3 * 512)
    c["a_negm"] = negm.reshape(16, 512)
    return c


def layout_weights(inp):
    w = {}
    for l in range(DEPTH):
        Wu = inp["ffn_w_up"][l]
        gt = Wu[:, :DFF].reshape(8, 128, NFC, 128)
        up = Wu[:, DFF:].reshape(8, 128, NFC, 128)
        blk = np.concatenate([gt, up], axis=3)
        w["wup%d" % l] = np.ascontiguousarray(blk.transpose(2, 1, 0, 3)).reshape(NFC * 128, 2048)
        Wd = inp["ffn_w_down"][l].reshape(NFC, 128, D)
        w["wdn%d" % l] = np.ascontiguousarray(Wd.transpose(1, 0, 2)).reshape(128 * 11, 2048)
    for j in range(2):
        Wi = inp["ret_w_in"][j]
        blocks = []
        for h in range(4):
            q = Wi[:, h * 256:(h + 1) * 256]
            k = Wi[:, 1024 + h * 256:1024 + (h + 1) * 256]
            qk = np.concatenate([q[:, 0::2], q[:, 1::2], k[:, 0::2], k[:, 1::2]], axis=1)
            v = Wi[:, 2048 + h * 512:2048 + (h + 1) * 512]
            gte = Wi[:, 4096 + h * 512:4096 + (h + 1) * 512]
            for m in (qk, v, gte):
                blocks.append(m.reshape(8, 128, 512).transpose(1, 0, 2).reshape(128, 4096))
        w["rwin%d" % j] = np.ascontiguousarray(np.concatenate(blocks, axis=0)).reshape(12 * 128 * 2, 2048)
        Wo = inp["ret_w_out"][j].reshape(16, 128, D)
        w["rwout%d" % j] = np.ascontiguousarray(Wo.transpose(1, 0, 2)).reshape(128 * 8, 2048)
        Wa = inp["attn_w_in"][j]
        blocks = []
        for hp in range(8):
            for g in range(3):
                cols = []
                for s_ in range(3):
                    c0 = ((s_ * 3 + g) * 16 + 2 * hp) * 64
                    cols.append(Wa[:, c0:c0 + 128])
                m = np.concatenate(cols, axis=1)
                blocks.append(m.reshape(8, 128, 384).transpose(1, 0, 2).reshape(128, 3072))
        w["awin%d" % j] = np.ascontiguousarray(np.concatenate(blocks, axis=0)).reshape(24 * 128 * 2, 1536)
        Wao = inp["attn_w_out"][j].reshape(8, 128, D)
        w["awout%d" % j] = np.ascontiguousarray(Wao.transpose(1, 0, 2)).reshape(128 * 4, 2048)
    return w


LAYERS_DEFAULT = (0, 1, 2, 3)


def build_program(layers=LAYERS_DEFAULT, debug_out=None):
    nc = bass.Bass("TRN2", target_bir_lowering=False)
    x_in = nc.dram_tensor("x", [T, D], F32, kind="ExternalInput").ap()
    y_out = nc.dram_tensor("y", [T, D], F32, kind="ExternalOutput").ap()
    lng = nc.dram_tensor("ln_gain", [DEPTH * 2 * D], F32, kind="ExternalInput").ap()
    lnb = nc.dram_tensor("ln_bias", [DEPTH * 2 * D], F32, kind="ExternalInput").ap()
    consts = make_consts()
    cd = {}
    for k, v in consts.items():
        cd[k] = nc.dram_tensor("c_" + k, list(v.shape), F32, kind="ExternalInput").ap()
    wsrc = {}
    wshapes = {}
    for l in layers:
        wshapes["wup%d" % l] = (NFC * 128, 2048)
        wshapes["wdn%d" % l] = (128 * 11, 2048)
        if l % 2 == 0:
            wshapes["rwin%d" % (l // 2)] = (12 * 128 * 2, 2048)
            wshapes["rwout%d" % (l // 2)] = (128 * 8, 2048)
        else:
            wshapes["awin%d" % (l // 2)] = (24 * 128 * 2, 1536)
            wshapes["awout%d" % (l // 2)] = (128 * 4, 2048)
    for k, shp in wshapes.items():
        wsrc[k] = nc.dram_tensor(k, list(shp), F32, kind="ExternalInput").ap()
    wbf = {k: nc.dram_tensor(k + "_bf", list(shp), BF16).ap() for k, shp in wshapes.items()}
    xa = nc.dram_tensor("xa", [T, D], F32).ap()
    xb_ = nc.dram_tensor("xb", [T, D], F32).ap()
    x1 = nc.dram_tensor("x1", [T, D], F32).ap()
    oT_d = nc.dram_tensor("oT_d", [8, 128, T], BF16).ap()
    w8d = nc.dram_tensor("w8d", [48, 512], F32).ap()
    b8 = nc.dram_tensor("b8", [96, BL], F32).ap()
    has_attn = any(l % 2 == 1 for l in layers)
    rel_bias_d = nc.dram_tensor("rel_bias", [48, 32], F32, kind="ExternalInput").ap() if has_attn else None

    with ExitStack() as es:
        P = Prog(nc, es)
        C = Ctx()
        C.ln_stats = Ring(P, es, "lnst", 4, [16], F32, dma=False)
        C.ident_t = es.enter_context(sbuf(nc, "ident", [128, 128], BF16))
        C.ident = C.ident_t[:, :]
        C.eps_ln = es.enter_context(sbuf(nc, "eps_ln", [128, 1], F32))
        C.eps_gn = es.enter_context(sbuf(nc, "eps_gn", [128, 1], F32))
        C.conv_sems = [SemC(es.enter_context(nc.semaphore("cv%d" % i))) for i in range(4)]
        C.conv_i = 0
        isem = SemC(es.enter_context(nc.semaphore("cv_i")))
        P.dma("pool", C.ident, cd["ident"], isem, [], [P.tk()])
        P.rec("dve", lambda e: e.memset(C.eps_ln[:], LN_EPS), [], [P.tk()])
        P.rec("dve", lambda e: e.memset(C.eps_gn[:], GN_EPS), [], [P.tk()])
        wtk = {k: [] for k in wshapes}
        pass_keys = []
        for l in layers:
            if l % 2 == 0:
                pass_keys.append(["rwin%d" % (l // 2), "rwout%d" % (l // 2)])
            else:
                pass_keys.append(["awin%d" % (l // 2)])
                pass_keys.append(["awout%d" % (l // 2)])
            pass_keys.append(["wup%d" % l, "wdn%d" % l])
        C.pass_i = 0

        def conv_for(pi):
            if pi < len(pass_keys):
                for k in pass_keys[pi]:
                    convert_weights(P, C, wsrc[k], wbf[k], wshapes[k][0], wshapes[k][1], wtk[k])

        def next_pass():
            C.pass_i += 1
            conv_for(C.pass_i)

        conv_for(0)
        P.flush()
        if has_attn:
            bias_prep_pass(P, C, rel_bias_d, w8d, b8, cd)
        if DBG == 1:
            return nc, consts, list(wshapes.keys())

        cur = x_in
        for li, l in enumerate(layers):
            last = li == len(layers) - 1
            nxt = y_out if last else (xa if cur is not xa else xb_)
            C.x_tk = P.tks(8)
            C.x1_tk = P.tks(8)
            C.xo_tk = P.tks(8)
            if l % 2 == 0:
                j = l // 2
                next_pass()
                retention_pass(P, C, es, l, cur, x1, wbf["rwin%d" % j].rearrange("(r two) c -> r (two c)", two=2),
                               wbf["rwout%d" % j].rearrange("(p e) c -> p (e c)", e=8),
                               wtk["rwin%d" % j], wtk["rwout%d" % j], lng, lnb, cd)
            else:
                j = l // 2
                C.oT_tk = P.tk()
                next_pass()
                attention_pass(P, C, l, cur, oT_d, wbf["awin%d" % j].rearrange("(r two) c -> r (two c)", two=2), b8)
                if DBG:
                    return nc, consts, list(wshapes.keys())
                C.x_tk = P.tks(8)
                C.x1_tk = P.tks(8)
                C.oT_tk = P.tk()
                next_pass()
                outproj_pass(P, C, l, cur, x1, oT_d, wbf["awout%d" % j].rearrange("(p e) c -> p (e c)", e=4),
                             lng, lnb)
            C.x_tk = P.tks(8)
            C.x1_tk = P.tks(8)
            C.xo_tk = P.tks(8)
            next_pass()
            ffn_pass(P, C, es, l, x1, nxt, wbf["wup%d" % l],
                     wbf["wdn%d" % l].rearrange("(p e) c -> p (e c)", e=11),
                     wtk["wup%d" % l], wtk["wdn%d" % l], lng, lnb)
            cur = nxt
    return nc, consts, list(wshapes.keys())


_CACHE = {}


def kernel(x, ret_w_in, ret_w_out, attn_w_in, attn_w_out, rel_bias, ffn_w_up, ffn_w_down, ln_gain, ln_bias):
    inp = dict(x=np.asarray(x), ret_w_in=np.asarray(ret_w_in), ret_w_out=np.asarray(ret_w_out),
               attn_w_in=np.asarray(attn_w_in), attn_w_out=np.asarray(attn_w_out), rel_bias=np.asarray(rel_bias),
               ffn_w_up=np.asarray(ffn_w_up), ffn_w_down=np.asarray(ffn_w_down))
    nc, consts, wkeys = build_program()
    w = layout_weights(inp)
    shared = {"ln_gain": np.ascontiguousarray(np.asarray(ln_gain, np.float32).reshape(-1)),
              "ln_bias": np.ascontiguousarray(np.asarray(ln_bias, np.float32).reshape(-1)),
              "rel_bias": np.ascontiguousarray(inp["rel_bias"])}
    for k, v in consts.items():
        shared["c_" + k] = v
    for k in wkeys:
        shared[k] = w[k]
    in_maps = []
    for b in range(NCORES):
        m = dict(shared)
        m["x"] = np.ascontiguousarray(inp["x"][b])
        in_maps.append(m)
    res = run_bass_kernel_spmd(nc, in_maps, core_ids=list(range(NCORES)))
    return np.stack([np.asarray(r["y"]) for r in res.results], axis=0).astype(np.float32)
```
